# Optimizing a Trainium2 kernel written in Bass

```python
import jax
import jax.numpy as jnp
from jax import lax
import numpy as np

D_MODEL = 2048
BATCH = 2
SEQ = 8192
DEPTH = 1

CTX_LEN = 256
GRID_W = 64
NORM_EPS = 1e-6

MLA_HEADS = 8
QK_NOPE_DIM = 128
QK_ROPE_DIM = 64
V_HEAD_DIM = 128
Q_LORA_RANK = 512
KV_LORA_RANK = 256
MLA_WIDTH = MLA_HEADS * V_HEAD_DIM
MLA_SCALE = (QK_NOPE_DIM + QK_ROPE_DIM) ** -0.5
ROPE_THETA = 10000.0
ROPE_AXIS_DIM = QK_ROPE_DIM // 2
Q_BLOCK = 128

RWKV_HEAD_DIM = 64
RWKV_WIDTH = D_MODEL - MLA_WIDTH
RWKV_HEADS = RWKV_WIDTH // RWKV_HEAD_DIM
DECAY_LORA = 64
ICLR_LORA = 64
GATE_LORA = 160
LNX_EPS = 64e-5

N_GROUPS = 4
EXPERTS_PER_GROUP = 8
N_EXPERTS = N_GROUPS * EXPERTS_PER_GROUP
TOP_K = 2
D_EXPERT = 512
MOE_BLOCK = 128

MLA_COLS = (Q_LORA_RANK, KV_LORA_RANK, QK_ROPE_DIM)
RWKV_COLS = (RWKV_WIDTH, RWKV_WIDTH, RWKV_WIDTH, DECAY_LORA, ICLR_LORA, GATE_LORA)
MLA_IN = sum(MLA_COLS)
RWKV_IN = sum(RWKV_COLS)
IN_COLS = MLA_IN + RWKV_IN

kernel_name = 'hybrid_mla_rwkv7_hmoe_dit_block'


def rms_norm(x, g):
    xf = x.astype(jnp.float32)
    y = xf * lax.rsqrt(jnp.mean(xf * xf, axis=-1, keepdims=True) + NORM_EPS)
    return (y * g).astype(x.dtype)


def modulate(x, g, shift, scale):
    return rms_norm(x, g) * (1 + scale) + shift


def split_cols(t, sizes):
    cuts = [int(i) for i in np.cumsum(sizes)[:-1]]
    return jnp.split(t, cuts, axis=-1)


def centred_shift(p, mu):
    zero = jnp.zeros_like(p[:, :1])
    prev = jnp.concatenate([zero, p[:, :-1]], axis=1)
    nxt = jnp.concatenate([p[:, 1:], zero], axis=1)
    return p + mu[0] * (prev - p) + mu[1] * (nxt - p)


def project_in(h, w_in, mu):
    p = h @ w_in
    p_mla, p_rwkv = p[..., :MLA_IN], p[..., MLA_IN:]
    return split_cols(p_mla, MLA_COLS) + split_cols(centred_shift(p_rwkv, mu), RWKV_COLS)


def axial_rope_tables(T):
    rows = T // GRID_W
    row, col = jnp.meshgrid(jnp.arange(rows), jnp.arange(GRID_W), indexing='ij')
    inv_freq = ROPE_THETA ** (-jnp.arange(0, ROPE_AXIS_DIM, 2, dtype=jnp.float32) / ROPE_AXIS_DIM)
    ang_r = row.reshape(-1)[:, None].astype(jnp.float32) * inv_freq
    ang_c = col.reshape(-1)[:, None].astype(jnp.float32) * inv_freq
    return (jnp.cos(ang_r), jnp.sin(ang_r), jnp.cos(ang_c), jnp.sin(ang_c))


def rotate_pair(x, cos, sin):
    x1, x2 = jnp.split(x, 2, axis=-1)
    return jnp.concatenate([x1 * cos - x2 * sin, x1 * sin + x2 * cos], axis=-1)


def rope_2d(x, tables):
    cr, sr, cc, sc = tables
    xr, xc = jnp.split(x, 2, axis=-1)
    return jnp.concatenate([rotate_pair(xr, cr, sr), rotate_pair(xc, cc, sc)], axis=-1).astype(x.dtype)


def mla_queries(c_q, q_norm_g, w_uq):
    B, T = c_q.shape[:2]
    q = (rms_norm(c_q, q_norm_g) @ w_uq).reshape(B, T, MLA_HEADS, QK_NOPE_DIM + QK_ROPE_DIM)
    return q[..., :QK_NOPE_DIM], q[..., QK_NOPE_DIM:]


def mla_keys_values(c_kv, kv_norm_g, w_ukv):
    B, T = c_kv.shape[:2]
    kv = (rms_norm(c_kv, kv_norm_g) @ w_ukv).reshape(B, T, MLA_HEADS, QK_NOPE_DIM + V_HEAD_DIM)
    return kv[..., :QK_NOPE_DIM], kv[..., QK_NOPE_DIM:]


def mla_attend(q_nope, q_rope, k_nope, k_rope, v):
    s = (jnp.einsum('bqhd,bkhd->bhqk', q_nope, k_nope, preferred_element_type=jnp.float32)
         + jnp.einsum('bqhd,bkd->bhqk', q_rope, k_rope, preferred_element_type=jnp.float32))
    p = jax.nn.softmax(s * MLA_SCALE, axis=-1)
    o = jnp.einsum('bhqk,bkhd->bqhd', p.astype(v.dtype), v)
    return o.reshape(o.shape[0], o.shape[1], MLA_WIDTH)


def mla_latent_attention(q_nope, q_rope, k_nope, k_rope, v):
    B, T = q_nope.shape[:2]
    nb = T // Q_BLOCK

    def blockify(t):
        return jnp.moveaxis(t.reshape(B, nb, Q_BLOCK, *t.shape[2:]), 1, 0)

    out = lax.map(lambda qs: mla_attend(qs[0], qs[1], k_nope, k_rope, v),
                  (blockify(q_nope), blockify(q_rope)))
    return jnp.moveaxis(out, 0, 1).reshape(B, T, MLA_WIDTH)


def heads(t):
    return t.reshape(*t.shape[:2], RWKV_HEADS, RWKV_HEAD_DIM)


def rwkv_direction(k, wl, al, w0, w_up, a0, a_up, key_a):
    w_raw = (w0 + jnp.tanh(wl) @ w_up).astype(jnp.float32)
    decay = jnp.exp(-jnp.exp(-jax.nn.softplus(-w_raw) - 0.5))
    a = jax.nn.sigmoid((a0 + al @ a_up).astype(jnp.float32))
    k_mod = k.astype(jnp.float32) * (1 + (a - 1) * key_a)
    return decay, a, k_mod


def rwkv_step(S, inp):
    r, w, k, v, kk, a = inp
    sa = jnp.einsum('bhvk,bhk->bhv', S, -kk)
    S = S * w[:, :, None, :] + sa[..., None] * (kk * a)[:, :, None, :] + v[..., None] * k[:, :, None, :]
    return S, jnp.einsum('bhvk,bhk->bhv', S, r)


def rwkv_scan(S0, r, w, k, v, kk, a, reverse):
    xs = tuple(jnp.moveaxis(heads(t), 1, 0) for t in (r, w, k, v, kk, a))
    S, ys = lax.scan(rwkv_step, S0, xs, reverse=reverse)
    return S, jnp.moveaxis(ys, 0, 1)


def rwkv_bidirectional(r, k, v, wl, al, S0_f, S0_b, params):
    w0, w_up, a0, a_up, key_k, key_a = params
    r32 = r.astype(jnp.float32)
    v32 = v.astype(jnp.float32)
    kk = heads(k.astype(jnp.float32) * key_k)
    kk = (kk * lax.rsqrt(jnp.sum(kk * kk, axis=-1, keepdims=True) + 1e-12)).reshape(k.shape)
    outs = []
    for d, (S0, rev) in enumerate(((S0_f, False), (S0_b, True))):
        decay, a, k_d = rwkv_direction(k, wl, al, w0[d], w_up[d], a0[d], a_up[d], key_a)
        S, y = rwkv_scan(S0, r32, decay, k_d, v32, kk, a, rev)
        outs.append((S, y, k_d))
    (s_f, y_f, k_f), (s_b, y_b, k_b) = outs
    return y_f + y_b, k_f + k_b, s_f, s_b


def rwkv_readout(y, r, k_sum, v, gl, gate_up, r_k, lnx_g, lnx_b):
    B, T = r.shape[:2]
    mu = jnp.mean(y, axis=-1, keepdims=True)
    var = jnp.mean(jnp.square(y - mu), axis=-1, keepdims=True)
    yn = ((y - mu) * lax.rsqrt(var + LNX_EPS)).reshape(B, T, RWKV_WIDTH) * lnx_g + lnx_b
    bonus = jnp.sum(heads(r.astype(jnp.float32)) * heads(k_sum) * r_k, axis=-1, keepdims=True) * heads(v.astype(jnp.float32))
    g = jax.nn.sigmoid(gl) @ gate_up
    return ((yn + bonus.reshape(B, T, RWKV_WIDTH)) * g).astype(r.dtype)


def hierarchical_route(x, w_grp, b_grp, w_exp, b_exp):
    N = x.shape[0]
    grp_prob = jax.nn.softmax((x @ w_grp + b_grp).astype(jnp.float32), axis=-1)
    g_val, g_idx = lax.top_k(grp_prob, 1)
    exp_logits = (x @ w_exp + b_exp).astype(jnp.float32).reshape(N, N_GROUPS, EXPERTS_PER_GROUP)
    within = exp_logits[jnp.arange(N), g_idx[:, 0]]
    e_val, e_idx = lax.top_k(jax.nn.softmax(within, axis=-1), TOP_K)
    gates = g_val * e_val / jnp.sum(e_val, axis=-1, keepdims=True)
    return g_idx * EXPERTS_PER_GROUP + e_idx, gates


def moe_ffn(h, w_grp, b_grp, w_exp, b_exp, w1, w3, w2):
    B, T, D = h.shape
    x = h.reshape(B * T, D)
    N = x.shape[0]
    expert_idx, gates = hierarchical_route(x, w_grp, b_grp, w_exp, b_exp)
    A = N * TOP_K
    n_blocks = (A + N_EXPERTS * (MOE_BLOCK - 1) + MOE_BLOCK - 1) // MOE_BLOCK
    P = n_blocks * MOE_BLOCK
    flat_e = expert_idx.reshape(-1)
    flat_tok = jnp.repeat(jnp.arange(N, dtype=jnp.int32), TOP_K)
    flat_gate = gates.reshape(-1)
    order = jnp.argsort(flat_e)
    sorted_e = flat_e[order]
    counts = jnp.bincount(flat_e, length=N_EXPERTS)
    padded = (counts + MOE_BLOCK - 1) // MOE_BLOCK * MOE_BLOCK
    start = jnp.cumsum(counts) - counts
    pad_end = jnp.cumsum(padded)
    pad_start = pad_end - padded
    dest = pad_start[sorted_e] + jnp.arange(A) - start[sorted_e]
    slot_tok = jnp.full((P,), N, jnp.int32).at[dest].set(flat_tok[order])
    slot_gate = jnp.zeros((P,), jnp.float32).at[dest].set(flat_gate[order])
    block_expert = jnp.minimum(
        jnp.searchsorted(pad_end, jnp.arange(n_blocks) * MOE_BLOCK, side='right'), N_EXPERTS - 1)
    x_pad = jnp.concatenate([x, jnp.zeros((1, D), x.dtype)], axis=0)

    def expert_block(args):
        tok, gate, e = args
        xb = x_pad[tok]
        yb = (jax.nn.silu(xb @ w1[e]) * (xb @ w3[e])) @ w2[e]
        return (yb * gate[:, None]).astype(x.dtype)

    y = lax.map(expert_block, (slot_tok.reshape(n_blocks, MOE_BLOCK),
                               slot_gate.reshape(n_blocks, MOE_BLOCK), block_expert))
    out = jnp.zeros((N + 1, D), x.dtype).at[slot_tok].add(y.reshape(P, D))[:N]
    return out.reshape(B, T, D)


def setup_inputs(seed: int = 0) -> dict:
    key = jax.random.key(seed)
    ks = iter(jax.random.split(key, 40))
    L, D = DEPTH, D_MODEL

    def nrm(shape, scale):
        return scale * jax.random.normal(next(ks), shape, jnp.float32)

    def uni(shape, lo, hi):
        return jax.random.uniform(next(ks), shape, jnp.float32, lo, hi)

    return {
        'x': nrm((BATCH, SEQ, D), 1.0),
        'c': nrm((BATCH, D), 1.0),
        'ctx': nrm((BATCH, CTX_LEN, D), 1.0),
        'c_ctx': nrm((D,), 1.0),
        'w_mod': nrm((L, D, 6 * D), 0.5 * D ** -0.5),
        'b_mod': nrm((L, 6 * D), 0.02),
        'norm_attn_g': 1.0 + nrm((L, D), 0.05),
        'norm_ffn_g': 1.0 + nrm((L, D), 0.05),
        'w_in': nrm((L, D, IN_COLS), D ** -0.5),
        'shift_mu': uni((L, 2, RWKV_IN), 0.0, 0.5),
        'q_norm_g': 1.0 + nrm((L, Q_LORA_RANK), 0.05),
        'w_uq': nrm((L, Q_LORA_RANK, MLA_HEADS * (QK_NOPE_DIM + QK_ROPE_DIM)), Q_LORA_RANK ** -0.5),
        'kv_norm_g': 1.0 + nrm((L, KV_LORA_RANK), 0.05),
        'w_ukv': nrm((L, KV_LORA_RANK, MLA_HEADS * (QK_NOPE_DIM + V_HEAD_DIM)), KV_LORA_RANK ** -0.5),
        'decay_w0': nrm((L, 2, RWKV_WIDTH), 0.5),
        'decay_up': nrm((L, 2, DECAY_LORA, RWKV_WIDTH), 0.5 * DECAY_LORA ** -0.5),
        'iclr_a0': nrm((L, 2, RWKV_WIDTH), 0.5),
        'iclr_up': nrm((L, 2, ICLR_LORA, RWKV_WIDTH), 0.5 * ICLR_LORA ** -0.5),
        'gate_up': nrm((L, GATE_LORA, RWKV_WIDTH), GATE_LORA ** -0.5),
        'key_k': 0.85 + nrm((L, RWKV_WIDTH), 0.05),
        'key_a': 1.0 + nrm((L, RWKV_WIDTH), 0.05),
        'bonus_r_k': nrm((L, RWKV_HEADS, RWKV_HEAD_DIM), 0.1),
        'lnx_g': 1.0 + nrm((L, RWKV_WIDTH), 0.05),
        'lnx_b': nrm((L, RWKV_WIDTH), 0.02),
        'w_out': nrm((L, D, D), D ** -0.5),
        'w_grp': nrm((L, D, N_GROUPS), D ** -0.5),
        'b_grp': nrm((L, N_GROUPS), 0.01),
        'w_exp': nrm((L, D, N_EXPERTS), D ** -0.5),
        'b_exp': nrm((L, N_EXPERTS), 0.01),
        'w1': nrm((L, N_EXPERTS, D, D_EXPERT), D ** -0.5),
        'w3': nrm((L, N_EXPERTS, D, D_EXPERT), D ** -0.5),
        'w2': nrm((L, N_EXPERTS, D_EXPERT, D), D_EXPERT ** -0.5),
        'final_norm_g': 1.0 + nrm((D,), 0.05),
    }


def reference(x, c, ctx, c_ctx, w_mod, b_mod, norm_attn_g, norm_ffn_g, w_in, shift_mu,
              q_norm_g, w_uq, kv_norm_g, w_ukv, decay_w0, decay_up, iclr_a0, iclr_up,
              gate_up, key_k, key_a, bonus_r_k, lnx_g, lnx_b, w_out,
              w_grp, b_grp, w_exp, b_exp, w1, w3, w2, final_norm_g):
    B, T, _ = x.shape
    rope_k = axial_rope_tables(T)
    rope_q = tuple(t[:, None, :] for t in rope_k)
    for l in range(DEPTH):
        mod = (jax.nn.silu(c) @ w_mod[l] + b_mod[l])[:, None, :]
        mod_c = jax.nn.silu(c_ctx) @ w_mod[l] + b_mod[l]
        sh1, sc1, g1, sh2, sc2, g2 = jnp.split(mod, 6, axis=-1)
        csh1, csc1, cg1, csh2, csc2, cg2 = jnp.split(mod_c, 6, axis=-1)

        h = modulate(x, norm_attn_g[l], sh1, sc1)
        hc = modulate(ctx, norm_attn_g[l], csh1, csc1)
        cq, ckv, kr, r, k, v, wl, al, gl = project_in(h, w_in[l], shift_mu[l])
        ccq, cckv, ckr, cr, ck, cv, cwl, cal, cgl = project_in(hc, w_in[l], shift_mu[l])

        qn, qr = mla_queries(cq, q_norm_g[l], w_uq[l])
        kn, vh = mla_keys_values(ckv, kv_norm_g[l], w_ukv[l])
        ckn, cvh = mla_keys_values(cckv, kv_norm_g[l], w_ukv[l])
        keys_nope = jnp.concatenate([kn, ckn], axis=1)
        keys_rope = jnp.concatenate([rope_2d(kr, rope_k), ckr], axis=1)
        values = jnp.concatenate([vh, cvh], axis=1)
        attn = mla_latent_attention(qn, rope_2d(qr, rope_q), keys_nope, keys_rope, values)

        rw = (decay_w0[l], decay_up[l], iclr_a0[l], iclr_up[l], key_k[l], key_a[l])
        zero = jnp.zeros((B, RWKV_HEADS, RWKV_HEAD_DIM, RWKV_HEAD_DIM), jnp.float32)
        cy, ck_sum, s_f, s_b = rwkv_bidirectional(cr, ck, cv, cwl, cal, zero, zero, rw)
        y, k_sum, _, _ = rwkv_bidirectional(r, k, v, wl, al, s_f, s_b, rw)
        rwkv = rwkv_readout(y, r, k_sum, v, gl, gate_up[l], bonus_r_k[l], lnx_g[l], lnx_b[l])

        x = x + g1 * (jnp.concatenate([attn, rwkv], axis=-1) @ w_out[l])
        x = x + g2 * moe_ffn(modulate(x, norm_ffn_g[l], sh2, sc2),
                             w_grp[l], b_grp[l], w_exp[l], b_exp[l], w1[l], w3[l], w2[l])

        if l < DEPTH - 1:
            cqn, cqr = mla_queries(ccq, q_norm_g[l], w_uq[l])
            cattn = mla_attend(cqn, cqr, ckn, ckr, cvh)
            crwkv = rwkv_readout(cy, cr, ck_sum, cv, cgl, gate_up[l], bonus_r_k[l], lnx_g[l], lnx_b[l])
            ctx = ctx + cg1 * (jnp.concatenate([cattn, crwkv], axis=-1) @ w_out[l])
            ctx = ctx + cg2 * moe_ffn(modulate(ctx, norm_ffn_g[l], csh2, csc2),
                                      w_grp[l], b_grp[l], w_exp[l], b_exp[l], w1[l], w3[l], w2[l])
    return rms_norm(x, final_norm_g)
```

```python
from contextlib import ExitStack
import numpy as np
import ml_dtypes
import concourse.bass as bass
import concourse.mybir as mybir
from concourse.bass_utils import run_bass_kernel_spmd

F32 = mybir.dt.float32
BF16 = mybir.dt.bfloat16
AF = mybir.ActivationFunctionType
ALU = mybir.AluOpType

D = 2048
T = 8192
TC = 256
TQ = 2048
NKV = T + TC
GRID_W = 64
NEXP = 32
DEXP = 512
EPS = 1e-6
LNX_EPS = 64e-5
MLA_SCALE = 192.0 ** -0.5
CH = 64

PCH = []
_o = 0
for _n, _m in ([("cq%d" % i, 128) for i in range(4)] + [("ckv0", 128), ("ckv1", 128), ("kr", 64), ("krsw", 64)]
               + [("r%d" % i, 64) for i in range(4)] + [("k%d" % i, 64) for i in range(4)]
               + [("v%d" % i, 64) for i in range(4)] + [("wl", 64), ("al", 64), ("gl0", 64), ("gl1", 64), ("gl2", 32)]):
    PCH.append((_n, _m, _o))
    _o += _m
NP1 = _o
RW_NAMES = [n for n, _, _ in PCH[8:]]
NRW = len(RW_NAMES)

PV_BMOD, PV_GATTN, PV_GFFN, PV_GFIN, PV_QNG, PV_KVNG, PV_N = 0, 96, 112, 128, 144, 148, 150


class Res:
    __slots__ = ("name", "lw", "rd", "sem", "dcount")

    def __init__(self, name):
        self.name = name
        self.lw = None
        self.rd = {}
        self.sem = None
        self.dcount = 0


class Prog:
    def __init__(self, nc):
        self.nc = nc
        self.engs = {"pe": nc.tensor, "act": nc.scalar, "dve": nc.vector, "pool": nc.gpsimd, "sp": nc.sync}
        self.sem = {}
        self.cnt = {}
        self.known = {}
        for e in self.engs:
            self.sem[e] = nc.alloc_semaphore("s_" + e)
            self.cnt[e] = 0
            self.known[e] = {}
        self.semown = {id(self.sem[e]): e for e in self.engs}
        self.ninst = 0
        self.all_dma = []
        self.retired = []

    def res(self, name):
        return Res(name)

    def _deps(self, e, reads, writes):
        deps = {}

        def add(tok):
            s, v = tok
            k = id(s)
            if k not in deps or deps[k][1] < v:
                deps[k] = (s, v)

        for r in reads:
            if r.lw is not None:
                add(r.lw)
        for w in writes:
            if w.lw is not None:
                add(w.lw)
            for t in w.rd.values():
                add(t)
        eng = self.engs[e]
        for k, (s, v) in deps.items():
            if e == "pe" and self.semown.get(k) == "pe":
                continue
            if self.known[e].get(k, 0) < v:
                eng.wait_ge(s, v)
                self.known[e][k] = v
                self.ninst += 1

    def _post(self, tok, reads, writes):
        k = id(tok[0])
        for r in reads:
            if k not in r.rd or r.rd[k][1] < tok[1]:
                r.rd[k] = tok
        for w in writes:
            w.lw = tok
            w.rd = {}

    def op(self, e, fn, reads=(), writes=()):
        self._deps(e, reads, writes)
        inst = fn(self.engs[e])
        self.cnt[e] += 1
        inst.then_inc(self.sem[e], 1)
        self.ninst += 1
        self._post((self.sem[e], self.cnt[e]), reads, writes)

    def dma(self, q, out, in_, reads=(), writes=(), **kw):
        self._deps(q, reads, writes)
        w = writes[0]
        if w.sem is None:
            w.sem = self.nc.alloc_semaphore("d_" + w.name)
            self.all_dma.append(w)
        inst = self.engs[q].dma_start(out=out, in_=in_, **kw)
        w.dcount += 16
        inst.then_inc(w.sem, 16)
        self.ninst += 1
        self._post((w.sem, w.dcount), reads, writes)

    def barrier(self):
        for e in self.engs:
            eng = self.engs[e]
            for f in self.engs:
                if f == e:
                    continue
                k = id(self.sem[f])
                if self.cnt[f] > 0 and self.known[e].get(k, 0) < self.cnt[f]:
                    eng.wait_ge(self.sem[f], self.cnt[f])
                    self.known[e][k] = self.cnt[f]
                    self.ninst += 1
            for r in self.all_dma:
                k = id(r.sem)
                if self.known[e].get(k, 0) < r.dcount:
                    eng.wait_ge(r.sem, r.dcount)
                    self.known[e][k] = r.dcount
                    self.ninst += 1
        for f in self.engs:
            if self.cnt[f] > 20000:
                self.retired.append(self.sem[f])
                self.sem[f] = self.nc.alloc_semaphore("s_%s_%d" % (f, len(self.retired)))
                self.semown[id(self.sem[f])] = f
                self.cnt[f] = 0

    def wait_all(self, e, resources):
        self._deps(e, resources, [])


class Tl:
    def __init__(self, P, t, name):
        self.t = t
        self.r = P.res(name)

    def __getitem__(self, idx):
        return self.t[idx]


class Ctx:
    pass


def sb(cx, stack, name, shape, dt=F32):
    t = stack.enter_context(cx.nc.sbuf_tensor("sb_" + name, shape, dt))
    return Tl(cx.P, t, name)


def dram(cx, name, shape, dt, kind="Internal", **kw):
    t = cx.nc.dram_tensor(name, shape, dt, kind=kind, **kw).ap()
    tl = Tl(cx.P, t, name)
    return tl


def mm(cx, out, lhsT, rhs, start, stop, reads, writes):
    cx.P.op("pe", lambda e: e.matmul(out, lhsT=lhsT, rhs=rhs, start=start, stop=stop), reads=reads, writes=writes)


def act(cx, out, in_, func, reads, writes, eng="act", **kw):
    cx.P.op("act", lambda e: e.activation(out=out, in_=in_, func=func, **kw), reads=reads, writes=writes)


def tt(cx, out, in0, in1, op, reads, writes, eng="dve"):
    cx.P.op(eng, lambda e: e.tensor_tensor(out=out, in0=in0, in1=in1, op=op), reads=reads, writes=writes)


def ts(cx, out, in0, s1, s2, op0, op1, reads, writes, eng="dve"):
    if op1 is None and eng == "pool":
        op1, s2 = (ALU.mult, 1.0) if op0 == ALU.add else (ALU.add, 0.0)
    if op1 is None:
        cx.P.op(eng, lambda e: e.tensor_scalar(out=out, in0=in0, scalar1=s1, scalar2=None, op0=op0), reads=reads, writes=writes)
    else:
        cx.P.op(eng, lambda e: e.tensor_scalar(out=out, in0=in0, scalar1=s1, scalar2=s2, op0=op0, op1=op1), reads=reads, writes=writes)


def stt(cx, out, in0, scalar, in1, op0, op1, reads, writes):
    cx.P.op("dve", lambda e: e.scalar_tensor_tensor(out=out, in0=in0, scalar=scalar, in1=in1, op0=op0, op1=op1), reads=reads, writes=writes)


def cp(cx, out, in_, reads, writes, eng="dve"):
    cx.P.op(eng, lambda e: e.tensor_copy(out=out, in_=in_), reads=reads, writes=writes)


def recip(cx, out, in_, reads, writes):
    cx.P.op("dve", lambda e: e.reciprocal(out=out, in_=in_), reads=reads, writes=writes)


def rsqrt_inplace(cx, tl, ap, scale, bias):
    act(cx, ap, ap, AF.Ln, [tl.r, cx.epsb.r], [tl.r], scale=scale, bias=bias)
    act(cx, ap, ap, AF.Exp, [tl.r], [tl.r], scale=-0.5)


def phase0(cx):
    nc, P, I = cx.nc, cx.P, cx.I
    st = cx.gstack
    cx.ident = sb(cx, st, "ident", [128, 128])
    P.dma("sp", cx.ident[:], I["ident"][:, :], writes=[cx.ident.r])
    cx.ones_bf = sb(cx, st, "ones_bf", [128, 128], BF16)
    P.op("pool", lambda e: e.memset(cx.ones_bf[:], 1.0), writes=[cx.ones_bf.r])
    cx.ones_f = sb(cx, st, "ones_f", [128, 128])
    P.op("pool", lambda e: e.memset(cx.ones_f[:], 1.0), writes=[cx.ones_f.r])
    cx.epsb = sb(cx, st, "epsb", [128, 4])
    P.op("pool", lambda e: e.memset(cx.epsb[:, 0:1], EPS), writes=[cx.epsb.r])
    P.op("pool", lambda e: e.memset(cx.epsb[:, 1:2], LNX_EPS), writes=[cx.epsb.r])
    P.op("pool", lambda e: e.memset(cx.epsb[:, 2:3], 1e-12), writes=[cx.epsb.r])
    P.op("pool", lambda e: e.memset(cx.epsb[:, 3:4], 0.0), writes=[cx.epsb.r])
    cx.pvec = sb(cx, st, "pvec", [128, PV_N])
    P.dma("sp", cx.pvec[:], I["pvec"][:, :], writes=[cx.pvec.r])
    cx.modv = sb(cx, st, "modv", [128, 96, 2])
    cx.dv = sb(cx, st, "dv", [128, 8, 16])

    with ExitStack() as ps:
        cT = sb(cx, ps, "cT", [128, 16, 2])
        sg = sb(cx, ps, "sg", [128, 16, 2])
        P.dma("sp", cT[:], I["cT"][:, :, :], writes=[cT.r])
        act(cx, sg[:], cT[:], AF.Exp, [cT.r], [sg.r], scale=-1.0)
        ts(cx, sg[:], sg[:], 1.0, None, ALU.add, None, [sg.r], [sg.r])
        recip(cx, sg[:], sg[:], [sg.r], [sg.r])
        tt(cx, sg[:], sg[:], cT[:], ALU.mult, [sg.r, cT.r], [sg.r])
        wbuf = [sb(cx, ps, "wmod%d" % i, [128, 16, 512]) for i in range(2)]
        pm = cx.psum[0]
        wsrc = I["w_mod"].rearrange("(k p) n -> p k n", p=128)
        for blk in range(0 if not cx.flags.get("fast0") else 24, 24):
            wb = wbuf[blk % 2]
            for kh in range(4):
                P.dma("sp", wb[:, kh * 4:(kh + 1) * 4, :], wsrc[:, kh * 4:(kh + 1) * 4, blk * 512:(blk + 1) * 512], writes=[wb.r])
            for jj in range(4):
                j = blk * 4 + jj
                for k in range(16):
                    mm(cx, pm[:, j * 2:j * 2 + 2], wb[:, k, jj * 128:(jj + 1) * 128], sg[:, k, :], k == 0, k == 15,
                       [wb.r, sg.r], [pm.r])
        for col in range(2):
            tt(cx, cx.modv[:, :, col], pm[:, 0:192].rearrange("p (j c) -> p j c", c=2)[:, :, col], cx.pvec[:, PV_BMOD:PV_BMOD + 96],
               ALU.add, [pm.r, cx.pvec.r], [cx.modv.r])
    dv, mv, pv = cx.dv, cx.modv, cx.pvec
    for col in range(2):
        stt(cx, dv[:, col, :], mv[:, 16:32, col], 1.0, pv[:, PV_GATTN:PV_GATTN + 16], ALU.add, ALU.mult, [mv.r, pv.r], [dv.r])
        cp(cx, dv[:, 2 + col, :], mv[:, 0:16, col], [mv.r], [dv.r])
    cp(cx, dv[:, 4, :], mv[:, 32:48, 0], [mv.r], [dv.r])
    stt(cx, dv[:, 5, :], mv[:, 64:80, 0], 1.0, pv[:, PV_GFFN:PV_GFFN + 16], ALU.add, ALU.mult, [mv.r, pv.r], [dv.r])
    cp(cx, dv[:, 6, :], mv[:, 48:64, 0], [mv.r], [dv.r])
    cp(cx, dv[:, 7, :], mv[:, 80:96, 0], [mv.r], [dv.r])


def phase1(cx):
    nc, P, I, S = cx.nc, cx.P, cx.I, cx.S
    G = 256
    q0 = cx.q0
    with ExitStack() as ps:
        w1p = sb(cx, ps, "w1p", [128, 16, NP1], BF16)
        wsrc = I["w1p"].rearrange("(k p) n -> p k n", p=128)
        for k in range(16):
            P.dma("pool", w1p[:, k, :], wsrc[:, k, :], writes=[w1p.r])
        xb = [sb(cx, ps, "xg%d" % i, [128, 16, G]) for i in range(2)]
        sq = sb(cx, ps, "sq", [128, 16, G], BF16)
        hT = sb(cx, ps, "hT", [128, 16, G], BF16)
        rstd = sb(cx, ps, "rstd", [128, G])
        tmp = [sb(cx, ps, "tmp%d" % i, [128, G]) for i in range(2)]
        stg = [sb(cx, ps, "stg%d" % i, [64, NRW, G]) for i in range(2)]
        kvs = [sb(cx, ps, "kvs%d" % i, [128, 2, G]) for i in range(2)]
        kvq = sb(cx, ps, "kvq", [128, 2, G], BF16)
        kvr = sb(cx, ps, "kvr", [128, G])
        kvo = [sb(cx, ps, "kvo%d" % i, [128, 2, G], BF16) for i in range(2)]
        krs = [sb(cx, ps, "krs%d" % i, [64, 2, G]) for i in range(2)]
        kro = [sb(cx, ps, "kro%d" % i, [64, G], BF16) for i in range(2)]
        rp = [sb(cx, ps, "rp%d" % i, [64, 2, G]) for i in range(2)]
        cqs = sb(cx, ps, "cqs", [128, 4, G])
        cqq = sb(cx, ps, "cqq", [128, 4, G], BF16)
        cqo = [sb(cx, ps, "cqo%d" % i, [128, 4, G], BF16) for i in range(2)]
        zt = sb(cx, ps, "zt", [64, NRW, 1])
        P.op("pool", lambda e: e.memset(zt[:], 0.0), writes=[zt.r])
        for tl_, n in ((S["rawp"], T), (S["rawc"], TC)):
            v = tl_.t.rearrange("(m p) t -> p m t", p=64)
            P.dma("sp", v[:, :, 0:1], zt[:], reads=[zt.r], writes=[tl_.r], allow_slow_non_contiguous=True)
            P.dma("sp", v[:, :, n + 1:n + 2], zt[:], reads=[zt.r], writes=[tl_.r], allow_slow_non_contiguous=True)

        groups = [("c", i) for i in range(TC // G)] + [("x", i) for i in range(T // G)] + [("o", i) for i in range(TQ // G)]
        pp = cx.psum
        for gi, (kind, i) in enumerate(groups):
            xg = xb[gi % 2]
            t0 = i * G
            if kind == "c":
                src = I["ctxT"].rearrange("(k p) t -> p k t", p=128)[:, :, t0:t0 + G]
                col = 1
            elif kind == "x":
                src = I["xT"].rearrange("(k p) t -> p k t", p=128)[:, :, t0:t0 + G]
                col = 0
            else:
                src = I["xoT"].rearrange("(k p) t -> p k t", p=128)[:, :, t0:t0 + G]
                col = 0
            for kh in range(2):
                P.dma("sp", xg[:, kh * 8:(kh + 1) * 8, :], src[:, kh * 8:(kh + 1) * 8, :], writes=[xg.r])
            act(cx, sq[:], xg[:], AF.Square, [xg.r], [sq.r])
            pss = pp[gi % 2]
            for k in range(16):
                mm(cx, pss[:, 0:G], cx.ones_bf[:], sq[:, k, :], k == 0, k == 15, [cx.ones_bf.r, sq.r], [pss.r])
            act(cx, rstd[:], pss[:, 0:G], AF.Ln, [pss.r, cx.epsb.r], [rstd.r], scale=1.0 / D, bias=cx.epsb[:, 0:1])
            act(cx, rstd[:], rstd[:], AF.Exp, [rstd.r], [rstd.r], scale=-0.5)
            for k in range(16):
                tm = tmp[k % 2]
                stt(cx, tm[:], xg[:, k, :], cx.dv[:, col, k:k + 1], rstd[:], ALU.mult, ALU.mult, [xg.r, cx.dv.r, rstd.r], [tm.r])
                if k % 2 == 0:
                    act(cx, hT[:, k, :], tm[:], AF.Identity, [tm.r, cx.dv.r], [hT.r], bias=cx.dv[:, 2 + col, k:k + 1], scale=1.0)
                else:
                    ts(cx, hT[:, k, :], tm[:], cx.dv[:, 2 + col, k:k + 1], None, ALU.add, None, [tm.r, cx.dv.r], [hT.r], eng="pool")
            if kind == "o":
                for m in range(4):
                    pc = pp[2 + m % 2]
                    for k in range(16):
                        mm(cx, pc[:, 0:G], w1p[:, k, m * 128:(m + 1) * 128], hT[:, k, :], k == 0, k == 15, [w1p.r, hT.r], [pc.r])
                    cp(cx, cqs[:, m, :], pc[:, 0:G], [pc.r], [cqs.r])
                act(cx, cqq[:], cqs[:], AF.Square, [cqs.r], [cqq.r])
                pq = pp[4]
                for m in range(4):
                    mm(cx, pq[:, 0:G], cx.ones_bf[:], cqq[:, m, :], m == 0, m == 3, [cx.ones_bf.r, cqq.r], [pq.r])
                act(cx, kvr[:], pq[:, 0:G], AF.Ln, [pq.r, cx.epsb.r], [kvr.r], scale=1.0 / 512, bias=cx.epsb[:, 0:1])
                act(cx, kvr[:], kvr[:], AF.Exp, [kvr.r], [kvr.r], scale=-0.5)
                co = cqo[i % 2]
                for m in range(4):
                    stt(cx, co[:, m, :], cqs[:, m, :], cx.pvec[:, PV_QNG + m:PV_QNG + m + 1], kvr[:], ALU.mult, ALU.mult,
                        [cqs.r, cx.pvec.r, kvr.r], [co.r])
                P.dma("sp", S["cqnT"].t.rearrange("(m p) t -> p m t", p=128)[:, :, t0:t0 + G], co[:], reads=[co.r], writes=[S["cqnT"].r])
                continue
            kv = kvs[gi % 2]
            for m in range(2):
                pc = pp[2 + m]
                c0 = 512 + m * 128
                for k in range(16):
                    mm(cx, pc[:, 0:G], w1p[:, k, c0:c0 + 128], hT[:, k, :], k == 0, k == 15, [w1p.r, hT.r], [pc.r])
                cp(cx, kv[:, m, :], pc[:, 0:G], [pc.r], [kv.r])
            act(cx, kvq[:], kv[:], AF.Square, [kv.r], [kvq.r])
            pq = pp[4]
            for m in range(2):
                mm(cx, pq[:, 0:G], cx.ones_bf[:], kvq[:, m, :], m == 0, m == 1, [cx.ones_bf.r, kvq.r], [pq.r])
            act(cx, kvr[:], pq[:, 0:G], AF.Ln, [pq.r, cx.epsb.r], [kvr.r], scale=1.0 / 256, bias=cx.epsb[:, 0:1])
            act(cx, kvr[:], kvr[:], AF.Exp, [kvr.r], [kvr.r], scale=-0.5)
            ko = kvo[gi % 2]
            for m in range(2):
                stt(cx, ko[:, m, :], kv[:, m, :], cx.pvec[:, PV_KVNG + m:PV_KVNG + m + 1], kvr[:], ALU.mult, ALU.mult,
                    [kv.r, cx.pvec.r, kvr.r], [ko.r])
            kvoff = T + t0 if kind == "c" else t0
            P.dma("sp", S["ckvnT"].t.rearrange("(m p) t -> p m t", p=128)[:, :, kvoff:kvoff + G], ko[:], reads=[ko.r], writes=[S["ckvnT"].r])
            kr_ = krs[gi % 2]
            pc = pp[5]
            for m in range(2):
                c0 = 768 + m * 64
                for k in range(16):
                    mm(cx, pc[0:64, m * G:(m + 1) * G], w1p[:, k, c0:c0 + 64], hT[:, k, :], k == 0, k == 15, [w1p.r, hT.r], [pc.r])
            o_ = kro[gi % 2]
            if kind == "c":
                cp(cx, o_[:], pc[0:64, 0:G], [pc.r], [o_.r])
            else:
                r_ = rp[gi % 2]
                P.dma("sp", r_[:], I["rope"][:, :, t0:t0 + G], writes=[r_.r])
                tt(cx, kr_[:], pc[0:64, 0:2 * G].rearrange("p (m t) -> p m t", m=2), r_[:], ALU.mult, [pc.r, r_.r], [kr_.r])
                tt(cx, o_[:], kr_[:, 0, :], kr_[:, 1, :], ALU.add, [kr_.r], [o_.r])
            P.dma("sp", S["krotT"].t[:, kvoff:kvoff + G], o_[:], reads=[o_.r], writes=[S["krotT"].r])
            sg_ = stg[gi % 2]
            for m, (nm, M, c0) in enumerate(PCH[8:]):
                pc = pp[6 + m % 2]
                for k in range(16):
                    mm(cx, pc[0:M, 0:G], w1p[:, k, c0:c0 + M], hT[:, k, :], k == 0, k == 15, [w1p.r, hT.r], [pc.r])
                if m % 2 == 0:
                    cp(cx, sg_[0:M, m, :], pc[0:M, 0:G], [pc.r], [sg_.r])
                else:
                    act(cx, sg_[0:M, m, :], pc[0:M, 0:G], AF.Copy, [pc.r], [sg_.r])
            dst = S["rawc"] if kind == "c" else S["rawp"]
            P.dma("sp", dst.t.rearrange("(m p) t -> p m t", p=64)[:, :, 1 + t0:1 + t0 + G], sg_[:], reads=[sg_.r], writes=[dst.r])


NCK = (TC + T) // CH
RV_W0, RV_A0, RV_KK, RV_KA, RV_RK, RV_LG, RV_LB, RV_N = 0, 8, 16, 20, 24, 28, 32, 36


def phase2(cx, ps_):
    nc, P, I, S = cx.nc, cx.P, cx.I, cx.S
    pp = cx.psum
    X = mybir.AxisListType.X
    rv = sb(cx, ps_, "rv", [64, RV_N])
    P.dma("sp", rv[:], I["rvec"][:, :], writes=[rv.r])
    nrv = sb(cx, ps_, "nrv", [64, 16])
    ts(cx, nrv[:], rv[:, 0:16], -1.0, None, ALU.mult, None, [rv.r], [nrv.r])
    omka = sb(cx, ps_, "omka", [64, 4])
    ts(cx, omka[:], rv[:, RV_KA:RV_KA + 4], -1.0, 1.0, ALU.mult, ALU.add, [rv.r], [omka.r])
    mux = sb(cx, ps_, "mux", [64, 3, NRW, 64])
    P.dma("sp", mux[:, 0:2], I["mux"][:, :, :, :], writes=[mux.r])
    tt(cx, mux[:, 2], mux[:, 0], mux[:, 1], ALU.add, [mux.r], [mux.r])
    ts(cx, mux[:, 2], mux[:, 2], -1.0, 1.0, ALU.mult, ALU.add, [mux.r], [mux.r])
    upw = sb(cx, ps_, "upw", [64, 2, 2, 256])
    P.dma("sp", upw[:], I["upw"][:, :, :, :], writes=[upw.r])
    gup = sb(cx, ps_, "gup", [64, 3, 256])
    P.dma("sp", gup[:], I["gup"][:, :, :], writes=[gup.r])
    m1mask = sb(cx, ps_, "m1mask", [128, 2, 128])
    P.dma("sp", m1mask[:], I["m1mask"][:, :, :], writes=[m1mask.r])
    p0mask = sb(cx, ps_, "p0mask", [64, 2, 64])
    P.dma("sp", p0mask[:], I["p0mask"][:, :, :], writes=[p0mask.r])
    zer = sb(cx, ps_, "zer", [64, 64])
    P.op("pool", lambda e: e.memset(zer[:], 0.0), writes=[zer.r])
    ones64 = cx.ones_f
    idn = cx.ident
    unit_v = S["unit"].t.rearrange("(g p) n -> g p n", p=64)
    st_v = S["states"].t.rearrange("(g p) n -> g p n", p=64)
    rest_v = S["rest"].t.rearrange("(g p) n -> g p n", p=64)

    with ExitStack() as st:
        Wn = [sb(cx, st, "Wn%d" % i, [64, NRW, 66]) for i in range(2)]
        sh = sb(cx, st, "sh", [64, NRW, 64])
        t17 = sb(cx, st, "t17", [64, NRW, 64])
        kk = sb(cx, st, "kk", [64, 4, 64])
        t4 = sb(cx, st, "t4", [64, 4, 64])
        kkn = sb(cx, st, "kkn", [64, 4, 64])
        th = sb(cx, st, "th", [64, 64])
        wd = sb(cx, st, "wd", [64, 8, 64])
        aa = sb(cx, st, "aa", [64, 8, 64])
        kd = sb(cx, st, "kd", [64, 8, 64])
        pref = sb(cx, st, "pref", [64, 8, 65])
        rp = sb(cx, st, "rp", [64, 8, 65])
        tot = sb(cx, st, "tot", [64, 8])
        rtot = sb(cx, st, "rtot", [64, 8])
        GIN = sb(cx, st, "GIN", [64, 8, 64])
        GEX = sb(cx, st, "GEX", [64, 8, 64])
        GINV = sb(cx, st, "GINV", [64, 8, 64])
        BKs = [sb(cx, st, "BK%d" % i, [64, 8, 2, 64]) for i in range(2)]
        ARs = [sb(cx, st, "AR%d" % i, [64, 8, 2, 64]) for i in range(2)]
        AVs = [sb(cx, st, "AV%d" % i, [64, 8, 2, 64]) for i in range(2)]
        BKgs = [sb(cx, st, "BKg%d" % i, [64, 8, 2, 64]) for i in range(2)]
        t8 = sb(cx, st, "t8", [64, 8, 64])
        M1m = [sb(cx, st, "M1m%d" % u, [128, 128]) for u in range(8)]
        Z = [sb(cx, st, "Z%d" % u, [128, 128]) for u in range(8)]
        T2s = [sb(cx, st, "T2s%d" % u, [128, 64]) for u in range(8)]
        PQ = [sb(cx, st, "PQ%d" % u, [64, 2, 2, 64]) for u in range(8)]
        dgs = [[sb(cx, st, "dg%d_%d" % (i, u), [64, 64]) for u in range(8)] for i in range(2)]
        Ou = [sb(cx, st, "Ou%d" % i, [64, 8, 4, 64]) for i in range(2)]
        rst = [sb(cx, st, "rst%d" % i, [64, 2, 4, 64]) for i in range(2)]
        sg = sb(cx, st, "sgl", [64, 3, 64])
        P.op("pool", lambda e: e.memset(pref[:], 1.0), writes=[pref.r])

        def prep_gen(gc):
            par = gc % 2
            BK_, AR_, AV_, BKg_, dg_ = BKs[par], ARs[par], AVs[par], BKgs[par], dgs[par]
            lat = gc >= 4
            ci = gc - 4 if lat else gc
            src = (S["rawp"] if lat else S["rawc"])
            W = Wn[gc % 2]
            P.dma("sp", W[:], src.t.rearrange("(m p) t -> p m t", p=64)[:, :, ci * 64:ci * 64 + 66], reads=[src.r], writes=[W.r])
            yield
            tt(cx, sh[:], W[:, :, 1:65], mux[:, 2], ALU.mult, [W.r, mux.r], [sh.r])
            tt(cx, t17[:], W[:, :, 0:64], mux[:, 0], ALU.mult, [W.r, mux.r], [t17.r])
            tt(cx, sh[:], sh[:], t17[:], ALU.add, [sh.r, t17.r], [sh.r])
            tt(cx, t17[:], W[:, :, 2:66], mux[:, 1], ALU.mult, [W.r, mux.r], [t17.r], eng="pool")
            tt(cx, sh[:], sh[:], t17[:], ALU.add, [sh.r, t17.r], [sh.r])
            rs, ks, vs = sh[:, 0:4], sh[:, 4:8], sh[:, 8:12]
            yield
            for h in range(4):
                ts(cx, kk[:, h, :], sh[:, 4 + h, :], rv[:, RV_KK + h:RV_KK + h + 1], None, ALU.mult, None, [sh.r, rv.r], [kk.r], eng="pool")
            tt(cx, t4[:], kk[:], kk[:], ALU.mult, [kk.r], [t4.r])
            pa = pp[7]
            mm(cx, pa[0:64, 0:256], ones64[0:64, 0:64], t4[:].rearrange("p a b -> p (a b)"), True, True, [ones64.r, t4.r], [pa.r])
            act(cx, t4[:].rearrange("p a b -> p (a b)"), pa[0:64, 0:256], AF.Ln, [pa.r, cx.epsb.r], [t4.r], bias=cx.epsb[0:64, 2:3], scale=1.0)
            act(cx, t4[:], t4[:], AF.Exp, [t4.r], [t4.r], scale=-0.5)
            tt(cx, kkn[:], kk[:], t4[:], ALU.mult, [kk.r, t4.r], [kkn.r])
            yield
            act(cx, th[:], sh[:, 12, :], AF.Exp, [sh.r], [th.r], scale=-2.0)
            ts(cx, th[:], th[:], 1.0, None, ALU.add, None, [th.r], [th.r])
            recip(cx, th[:], th[:], [th.r], [th.r])
            ts(cx, th[:], th[:], 2.0, -1.0, ALU.mult, ALU.add, [th.r], [th.r])
            pw, pq = pp[7], pp[6]
            for u in range(8):
                d, h = u // 4, u % 4
                mm(cx, pw[0:64, u * 64:(u + 1) * 64], upw[:, 0, d, h * 64:(h + 1) * 64], th[:], True, True, [upw.r, th.r], [pw.r])
                mm(cx, pq[0:64, u * 64:(u + 1) * 64], upw[:, 1, d, h * 64:(h + 1) * 64], sh[:, 13, :], True, True, [upw.r, sh.r], [pq.r])
            for u in range(8):
                act(cx, wd[:, u, :], pw[0:64, u * 64:(u + 1) * 64], AF.Exp, [pw.r, nrv.r], [wd.r], scale=-1.0, bias=nrv[:, u:u + 1])
                act(cx, aa[:, u, :], pq[0:64, u * 64:(u + 1) * 64], AF.Exp, [pq.r, nrv.r], [aa.r], scale=-1.0, bias=nrv[:, 8 + u:9 + u])
            for tl_ in (wd, aa):
                ts(cx, tl_[:], tl_[:], 1.0, None, ALU.add, None, [tl_.r], [tl_.r])
                recip(cx, tl_[:], tl_[:], [tl_.r], [tl_.r])
            act(cx, wd[:], wd[:], AF.Exp, [wd.r], [wd.r], scale=-0.6065306597126334)
            yield
            for u in range(8):
                h = u % 4
                ts(cx, kd[:, u, :], aa[:, u, :], rv[:, RV_KA + h:RV_KA + h + 1], omka[:, h:h + 1], ALU.mult, ALU.add, [aa.r, rv.r, omka.r], [kd.r], eng="pool")
            yield
            for d in range(2):
                tt(cx, kd[:, d * 4:(d + 1) * 4], kd[:, d * 4:(d + 1) * 4], ks, ALU.mult, [kd.r, sh.r], [kd.r])
            yield
            for u in range(8):
                P.op("dve", lambda e: e.tensor_tensor_scan(out=pref[:, u, 1:65], data0=wd[:, u, :], data1=zer[:], initial=1.0, op0=ALU.mult, op1=ALU.add),
                     reads=[wd.r, zer.r], writes=[pref.r])
            yield
            recip(cx, rp[:], pref[:], [pref.r], [rp.r])
            cp(cx, tot[:], pref[:, :, 64], [pref.r], [tot.r])
            cp(cx, rtot[:], rp[:, :, 64], [rp.r], [rtot.r])
            cp(cx, GIN[:, 0:4], pref[:, 0:4, 1:65], [pref.r], [GIN.r], eng="pool")
            cp(cx, GEX[:, 0:4], pref[:, 0:4, 0:64], [pref.r], [GEX.r], eng="pool")
            cp(cx, GINV[:, 0:4], rp[:, 0:4, 1:65], [rp.r], [GINV.r], eng="pool")
            for u in range(4, 8):
                ts(cx, GIN[:, u, :], rp[:, u, 0:64], tot[:, u:u + 1], None, ALU.mult, None, [rp.r, tot.r], [GIN.r], eng="pool")
                ts(cx, GEX[:, u, :], rp[:, u, 1:65], tot[:, u:u + 1], None, ALU.mult, None, [rp.r, tot.r], [GEX.r], eng="pool")
                ts(cx, GINV[:, u, :], pref[:, u, 0:64], rtot[:, u:u + 1], None, ALU.mult, None, [pref.r, rtot.r], [GINV.r], eng="pool")
            yield
            for d in range(2):
                sl = slice(d * 4, (d + 1) * 4)
                stt(cx, AR_[:, sl, 0, :], kkn[:], -1.0, GEX[:, sl], ALU.mult, ALU.mult, [kkn.r, GEX.r], [AR_.r])
                tt(cx, AR_[:, sl, 1, :], rs, GIN[:, sl], ALU.mult, [sh.r, GIN.r], [AR_.r])
                tt(cx, t8[:, sl], kkn[:], aa[:, sl], ALU.mult, [kkn.r, aa.r], [t8.r])
                cp(cx, AV_[:, sl, 1, :], vs, [sh.r], [AV_.r], eng="pool")
            yield
            tt(cx, BK_[:, :, 0, :], t8[:], GINV[:], ALU.mult, [t8.r, GINV.r], [BK_.r])
            tt(cx, BK_[:, :, 1, :], kd[:], GINV[:], ALU.mult, [kd.r, GINV.r], [BK_.r])
            cp(cx, AV_[:, :, 0, :], AR_[:, :, 0, :], [AR_.r], [AV_.r], eng="pool")
            for u in range(8):
                ts(cx, BKg_[:, u], BK_[:, u], tot[:, u:u + 1], None, ALU.mult, None, [BK_.r, tot.r], [BKg_.r], eng="pool")
                ts(cx, dg_[u][:], idn[0:64, 0:64], tot[:, u:u + 1], None, ALU.mult, None, [idn.r, tot.r], [dg_[u].r], eng="pool")
            yield
            if lat:
                R_ = rst[gc % 2]
                tt(cx, t4[:], kd[:, 0:4], kd[:, 4:8], ALU.add, [kd.r], [t4.r])
                tt(cx, t4[:], t4[:], rs, ALU.mult, [t4.r, sh.r], [t4.r])
                for h in range(4):
                    ts(cx, t4[:, h, :], t4[:, h, :], rv[:, RV_RK + h:RV_RK + h + 1], None, ALU.mult, None, [t4.r, rv.r], [t4.r])
                mm(cx, pa[0:64, 256:512], ones64[0:64, 0:64], t4[:].rearrange("p a b -> p (a b)"), True, True, [ones64.r, t4.r], [pa.r])
                tt(cx, R_[:, 0].rearrange("p a b -> p (a b)"), pa[0:64, 256:512], sh[:, 8:12].rearrange("p a b -> p (a b)"), ALU.mult, [pa.r, sh.r], [R_.r])
                act(cx, sg[:], sh[:, 14:17], AF.Exp, [sh.r], [sg.r], scale=-1.0)
                ts(cx, sg[:], sg[:], 1.0, None, ALU.add, None, [sg.r], [sg.r])
                recip(cx, sg[:], sg[:], [sg.r], [sg.r])
                pg = pp[7]
                for h in range(4):
                    for j, kj in enumerate((64, 64, 32)):
                        mm(cx, pg[0:64, h * 64:(h + 1) * 64], gup[0:kj, j, h * 64:(h + 1) * 64], sg[0:kj, j, :], j == 0, j == 2, [gup.r, sg.r], [pg.r])
                cp(cx, R_[:, 1].rearrange("p a b -> p (a b)"), pg[0:64, 0:256], [pg.r], [R_.r])
                P.dma("sp", rest_v[ci].rearrange("p (a n) -> p a n", a=2), R_[:].rearrange("p a h t -> p a (h t)"), reads=[R_.r], writes=[S["rest"].r])

            yield

        def units(gc, pg_):
            par = gc % 2
            BK_, AR_, AV_, BKg_, dg_ = BKs[par], ARs[par], AVs[par], BKgs[par], dgs[par]
            O = Ou[gc % 2]

            def tick():
                if pg_ is not None:
                    next(pg_, None)

            waves = (range(0, 6), range(6, 8))
            for wave in waves:
                tick()
                for u in wave:
                    pu = pp[u % 6]
                    mm(cx, pu[:, 0:128], BK_[:, u].rearrange("p a b -> p (a b)"), AR_[:, u].rearrange("p a b -> p (a b)"), True, True, [BK_.r, AR_.r], [pu.r])
                    mm(cx, pu[0:64, 128:192], AR_[:, u, 0, :], BK_[:, u, 0, :], True, True, [BK_.r, AR_.r], [pu.r])
                    P.op("pe", lambda e: e.transpose(pu[:, 192:256], AV_[:, u].rearrange("p a b -> p (a b)"), idn[0:64, 0:64]), reads=[AV_.r, idn.r], writes=[pu.r])
                    P.op("pe", lambda e: e.transpose(pu[:, 256:320], BKg_[:, u].rearrange("p a b -> p (a b)"), idn[0:64, 0:64]), reads=[BKg_.r, idn.r], writes=[pu.r])
                tick()
                for u in wave:
                    d = u // 4
                    pu = pp[u % 6]
                    tt(cx, M1m[u][:], pu[:, 0:128], m1mask[:, d, :], ALU.mult, [pu.r, m1mask.r], [M1m[u].r])
                    tt(cx, PQ[u][:, 0, 0, :], pu[0:64, 128:192], p0mask[:, d, :], ALU.mult, [pu.r, p0mask.r], [PQ[u].r])
                    cp(cx, Z[u][0:64, 0:64], pu[0:64, 192:256], [pu.r], [Z[u].r])
                    cp(cx, Z[u][64:128, 64:128], pu[64:128, 192:256], [pu.r], [Z[u].r])
                    cp(cx, T2s[u][:], pu[:, 256:320], [pu.r], [T2s[u].r])
                    cp(cx, PQ[u][:, 0, 1, :], M1m[u][0:64, 0:64], [M1m[u].r], [PQ[u].r], eng="pool")
            for wave in waves:
                tick()
                for u in wave:
                    pu = pp[u % 6]
                    mm(cx, pu[0:64, 320:384], M1m[u][64:128, 0:64], Z[u][64:128, 64:128], True, True, [M1m[u].r, Z[u].r], [pu.r])
                for u in wave:
                    pu = pp[u % 6]
                    cp(cx, Z[u][0:64, 64:128], pu[0:64, 320:384], [pu.r], [Z[u].r])
            for j in range(6):
                b0, b1 = j % 2, (j + 1) % 2
                for wave in waves:
                    tick()
                    for u in wave:
                        pu = pp[u % 6]
                        mm(cx, pu[0:64, 384:512], PQ[u][:, b0, 1, :], Z[u][0:64, :], True, True, [PQ[u].r, Z[u].r], [pu.r])
                        if j < 5:
                            mm(cx, pu[0:64, 128:192], PQ[u][:, b0, 1, :], PQ[u][:, b0, 0, :], True, True, [PQ[u].r], [pu.r])
                            mm(cx, pu[0:64, 192:256], PQ[u][:, b0, 0, :], PQ[u][:, b0, 1, :], True, True, [PQ[u].r], [pu.r])
                    for u in wave:
                        pu = pp[u % 6]
                        tt(cx, Z[u][0:64, :], Z[u][0:64, :], pu[0:64, 384:512], ALU.add, [Z[u].r, pu.r], [Z[u].r])
                        if j < 5:
                            cp(cx, PQ[u][:, b1].rearrange("p a b -> p (a b)"), pu[0:64, 128:256], [pu.r], [PQ[u].r])
            for wave in waves:
                tick()
                for u in wave:
                    pu = pp[u % 6]
                    mm(cx, pu[0:64, 0:64], Z[u][0:64, 0:64], M1m[u][0:64, 64:128], True, True, [Z[u].r, M1m[u].r], [pu.r])
                    mm(cx, pu[0:64, 64:128], Z[u][0:64, 0:64], T2s[u][0:64, :], True, True, [Z[u].r, T2s[u].r], [pu.r])
                    mm(cx, pu[0:64, 128:192], T2s[u][:], Z[u][:, 64:128], True, True, [Z[u].r, T2s[u].r], [pu.r])
                    mm(cx, pu[0:64, 192:256], Z[u][:, 64:128], M1m[u][:, 64:128], True, True, [Z[u].r, M1m[u].r], [pu.r])
                for u in wave:
                    pu = pp[u % 6]
                    tt(cx, O[:, u, 2, :], pu[0:64, 0:64], AR_[:, u, 1, :], ALU.add, [pu.r, AR_.r], [O.r])
                    tt(cx, O[:, u, 0, :], pu[0:64, 64:128], dg_[u][:], ALU.add, [pu.r, dg_[u].r], [O.r])
                    cp(cx, O[:, u, 1, :], pu[0:64, 128:192], [pu.r], [O.r])
                    cp(cx, O[:, u, 3, :], pu[0:64, 192:256], [pu.r], [O.r])
            P.dma("sp", unit_v[gc], O[:].rearrange("p u a t -> p (u a t)"), reads=[O.r], writes=[S["unit"].r])

        chunks = list(range(NCK) if "nchunks" not in cx.flags else cx.flags["nchunks"])
        g0 = prep_gen(chunks[0])
        for _ in g0:
            pass
        for ii, gc in enumerate(chunks):
            nxt = prep_gen(chunks[ii + 1]) if ii + 1 < len(chunks) else None
            units(gc, nxt)
            if nxt is not None:
                for _ in nxt:
                    pass
    P.barrier()
    if cx.flags.get("stopA"):
        return

    with ExitStack() as st:
        ST = [sb(cx, st, "ST%d" % i, [64, 8, 64]) for i in range(3)]
        GH = [sb(cx, st, "GH%d" % i, [64, 8, 2, 64]) for i in range(3)]
        P.op("pool", lambda e: e.memset(ST[0][:], 0.0), writes=[ST[0].r])
        order_f = list(range(NCK))
        order_b = [3, 2, 1, 0] + [4 + i for i in range(127, -1, -1)]
        uv = S["unit"].t.rearrange("(g p) (u a t) -> g p u a t", p=64, u=8, a=4)
        sv = S["states"].t.rearrange("(g p) (u t) -> g p u t", p=64, u=8)
        for s_ in range(NCK):
            gf, gb = order_f[s_], order_b[s_]
            cur, nxt = ST[s_ % 3], ST[(s_ + 1) % 3]
            g_ = GH[s_ % 3]
            P.dma("sp", g_[:, 0:4], uv[gf][:, 0:4, 0:2, :], reads=[S["unit"].r], writes=[g_.r])
            P.dma("sp", g_[:, 4:8], uv[gb][:, 4:8, 0:2, :], reads=[S["unit"].r], writes=[g_.r])
            P.dma("sp", sv[gf][:, 0:4, :], cur[:, 0:4, :], reads=[cur.r], writes=[S["states"].r])
            P.dma("sp", sv[gb][:, 4:8, :], cur[:, 4:8, :], reads=[cur.r], writes=[S["states"].r])
            pb_ = pp[s_ % 2]
            for u in range(8):
                mm(cx, pb_[0:64, u * 64:(u + 1) * 64], g_[:, u, 0, :], cur[:, u, :], True, True, [g_.r, cur.r], [pb_.r])
            tt(cx, nxt[:], pb_[0:64, 0:512].rearrange("p (u t) -> p u t", u=8), g_[:, :, 1, :], ALU.add, [pb_.r, g_.r], [nxt.r])
    P.barrier()
    if cx.flags.get("stopB"):
        return

    with ExitStack() as st:
        S0 = [sb(cx, st, "S0_%d" % i, [64, 8, 64]) for i in range(2)]
        RY = [sb(cx, st, "RY%d" % i, [64, 8, 2, 64]) for i in range(2)]
        RS = [sb(cx, st, "RS%d" % i, [64, 2, 4, 64]) for i in range(2)]
        y8 = sb(cx, st, "y8", [64, 8, 64])
        y = sb(cx, st, "y", [64, 4, 64])
        ysq = sb(cx, st, "ysq", [64, 4, 64])
        mu = sb(cx, st, "mu", [64, 4, 64])
        var = sb(cx, st, "var", [64, 4, 64])
        ob = [sb(cx, st, "ob%d" % i, [64, 4, 512], BF16) for i in range(2)]
        uv = S["unit"].t.rearrange("(g p) (u a t) -> g p u a t", p=64, u=8, a=4)
        sv = S["states"].t.rearrange("(g p) (u t) -> g p u t", p=64, u=8)
        rv4 = S["rest"].t.rearrange("(g p) (a h t) -> g p a h t", p=64, a=2, h=4)
        for ci in range(cx.flags.get("cchunks", T // CH)):
            gc = 4 + ci
            s0, ry, rs_ = S0[ci % 2], RY[ci % 2], RS[ci % 2]
            P.dma("sp", s0[:], sv[gc], reads=[S["states"].r], writes=[s0.r])
            P.dma("sp", ry[:], uv[gc][:, :, 2:4, :], reads=[S["unit"].r], writes=[ry.r])
            P.dma("sp", rs_[:], rv4[ci], reads=[S["rest"].r], writes=[rs_.r])
            pc_ = pp[ci % 2]
            for u in range(8):
                mm(cx, pc_[0:64, u * 64:(u + 1) * 64], s0[:, u, :], ry[:, u, 0, :], True, True, [s0.r, ry.r], [pc_.r])
            tt(cx, y8[:], pc_[0:64, 0:512].rearrange("p (u t) -> p u t", u=8), ry[:, :, 1, :], ALU.add, [pc_.r, ry.r], [y8.r])
            tt(cx, y[:], y8[:, 0:4], y8[:, 4:8], ALU.add, [y8.r], [y.r])
            if cx.flags.get("ccut") == 1:
                continue
            tt(cx, ysq[:], y[:], y[:], ALU.mult, [y.r], [ysq.r], eng="pool")
            pm_ = pp[2 + ci % 2]
            mm(cx, pm_[0:64, 0:256], ones64[0:64, 0:64], y[:].rearrange("p a b -> p (a b)"), True, True, [ones64.r, y.r], [pm_.r])
            mm(cx, pm_[0:64, 256:512], ones64[0:64, 0:64], ysq[:].rearrange("p a b -> p (a b)"), True, True, [ones64.r, ysq.r], [pm_.r])
            if cx.flags.get("ccut") == 2:
                continue
            muf, varf = mu[:].rearrange("p a b -> p (a b)"), var[:].rearrange("p a b -> p (a b)")
            ts(cx, muf, pm_[0:64, 0:256], 1.0 / 64, None, ALU.mult, None, [pm_.r], [mu.r])
            tt(cx, ysq[:], mu[:], mu[:], ALU.mult, [mu.r], [ysq.r])
            stt(cx, varf, pm_[0:64, 256:512], 1.0 / 64, ysq[:].rearrange("p a b -> p (a b)"), ALU.mult, ALU.subtract, [pm_.r, ysq.r], [var.r])
            act(cx, varf, varf, AF.Ln, [var.r, cx.epsb.r], [var.r], bias=cx.epsb[0:64, 1:2], scale=1.0)
            act(cx, varf, varf, AF.Exp, [var.r], [var.r], scale=-0.5)
            if cx.flags.get("ccut") == 3:
                continue
            tt(cx, y[:], y[:], mu[:], ALU.subtract, [y.r, mu.r], [y.r])
            tt(cx, y[:], y[:], var[:], ALU.mult, [y.r, var.r], [y.r])
            for h in range(4):
                ts(cx, y[:, h, :], y[:, h, :], rv[:, RV_LG + h:RV_LG + h + 1], rv[:, RV_LB + h:RV_LB + h + 1], ALU.mult, ALU.add, [y.r, rv.r], [y.r])
            tt(cx, y[:], y[:], rs_[:, 0], ALU.add, [y.r, rs_.r], [y.r])
            if cx.flags.get("ccut") == 4:
                continue
            o_ = ob[(ci // 8) % 2]
            tt(cx, o_[:, :, (ci % 8) * 64:(ci % 8 + 1) * 64], y[:], rs_[:, 1], ALU.mult, [y.r, rs_.r], [o_.r])
            if cx.flags.get("ccut") == 5:
                continue
            if ci % 8 == 7:
                t0 = (ci // 8) * 512
                rin = S["rwkv_in%d" % (t0 // TQ)]
                P.dma("pool", rin.t.rearrange("(h p) t -> p h t", p=64)[:, :, t0 % TQ:t0 % TQ + 512], o_[:], reads=[o_.r], writes=[rin.r])
                if "rwkv_dbg" in S:
                    P.dma("pool", S["rwkv_dbg"].t.rearrange("(h p) t -> p h t", p=64)[:, :, t0:t0 + 512], o_[:], reads=[o_.r], writes=[S["rwkv_dbg"].r])
    P.barrier()
    if cx.flags.get("stopC"):
        return
    for j in range(4):
        rin, ra = S["rwkv_in%d" % j], S["rwkv_all%d" % j].r
        P._deps("pool", [rin.r], [ra])
        inst = nc.gpsimd.collective_compute("AllGather", ALU.bypass, replica_groups=[[0, 1, 2, 3], [4, 5, 6, 7]],
                                            ins=[rin.t], outs=[S["rwkv_all%d" % j].t])
        if ra.sem is None:
            ra.sem = nc.alloc_semaphore("d_rwkv_all%d" % j)
            P.all_dma.append(ra)
        ra.dcount += 1
        inst.then_inc(ra.sem, 1)
        P.ninst += 1
        P._post((ra.sem, ra.dcount), [rin.r], [ra])


def phase3(cx, ps_):
    nc, P, I, S = cx.nc, cx.P, cx.I, cx.S
    pp = cx.psum
    NT = NKV // 128
    ckv = sb(cx, ps_, "a_ckv", [128, 2, NKV], BF16)
    krot = sb(cx, ps_, "a_krot", [64, NKV], BF16)
    cqn = sb(cx, ps_, "a_cqn", [128, 4, TQ], BF16)
    wq = sb(cx, ps_, "a_wq", [128, 4, 8 * 256], BF16)
    wk = sb(cx, ps_, "a_wk", [128, 2, 8 * 128], BF16)
    wv = sb(cx, ps_, "a_wv", [128, 2, 8 * 128], BF16)
    rqs = [sb(cx, ps_, "a_rq%d" % i, [64, 2, 512]) for i in range(2)]
    otl = [sb(cx, ps_, "a_ot%d" % i, [128, 512], BF16) for i in range(2)]
    knT = sb(cx, ps_, "a_knT", [128, NKV], BF16)
    vh = sb(cx, ps_, "a_vh", [128, NT, 128], BF16)
    qnT = sb(cx, ps_, "a_qnT", [128, TQ], BF16)
    qrT = sb(cx, ps_, "a_qrT", [64, TQ], BF16)
    qtmp = sb(cx, ps_, "a_qtmp", [64, 2, 512])
    pT = [sb(cx, ps_, "a_pT%d" % i, [128, 512], BF16) for i in range(3)]
    rden = sb(cx, ps_, "a_rden", [128, 512])
    P.dma("sp", ckv[:], S["ckvnT"].t.rearrange("(m p) t -> p m t", p=128), reads=[S["ckvnT"].r], writes=[ckv.r])
    P.dma("sp", krot[:], S["krotT"].t[:, :], reads=[S["krotT"].r], writes=[krot.r])
    P.dma("sp", cqn[:], S["cqnT"].t.rearrange("(m p) t -> p m t", p=128), reads=[S["cqnT"].r], writes=[cqn.r])
    for k in range(4):
        P.dma("pool", wq[:, k, :], I["wq"].rearrange("(k p) n -> p k n", p=128)[:, k, :], writes=[wq.r])
    for k in range(2):
        P.dma("pool", wk[:, k, :], I["wk"].rearrange("(k p) n -> p k n", p=128)[:, k, :], writes=[wk.r])
        P.dma("pool", wv[:, k, :], I["wv"].rearrange("(k p) n -> p k n", p=128)[:, k, :], writes=[wv.r])
    ev = 0
    for h in range(8):
        for g in range((NKV + 511) // 512):
            t0 = g * 512
            n = min(512, NKV - t0)
            pc = pp[g % 2]
            for k in range(2):
                mm(cx, pc[:, 0:n], wk[:, k, h * 128:(h + 1) * 128], ckv[:, k, t0:t0 + n], k == 0, k == 1, [wk.r, ckv.r], [pc.r])
            if g % 2 == 0:
                cp(cx, knT[:, t0:t0 + n], pc[:, 0:n], [pc.r], [knT.r])
            else:
                act(cx, knT[:, t0:t0 + n], pc[:, 0:n], AF.Copy, [pc.r], [knT.r])
        for g in range((NT + 3) // 4):
            nt = min(4, NT - g * 4)
            pc = pp[2 + g % 2]
            for j in range(nt):
                t0 = (g * 4 + j) * 128
                for k in range(2):
                    mm(cx, pc[:, j * 128:(j + 1) * 128], ckv[:, k, t0:t0 + 128], wv[:, k, h * 128:(h + 1) * 128], k == 0, k == 1,
                       [wv.r, ckv.r], [pc.r])
            dst = vh[:, g * 4:g * 4 + nt, :].rearrange("p a b -> p (a b)")
            if g % 2 == 0:
                act(cx, dst, pc[:, 0:nt * 128], AF.Copy, [pc.r], [vh.r])
            else:
                cp(cx, dst, pc[:, 0:nt * 128], [pc.r], [vh.r])
        for g in range(4):
            t0 = g * 512
            pc = pp[g % 2]
            for k in range(4):
                mm(cx, pc[:, :], wq[:, k, h * 256:h * 256 + 128], cqn[:, k, t0:t0 + 512], k == 0, k == 3, [wq.r, cqn.r], [pc.r])
            act(cx, qnT[:, t0:t0 + 512], pc[:, :], AF.Copy, [pc.r], [qnT.r], scale=MLA_SCALE)
            pa, pb = pp[4], pp[5]
            for m, pdst in ((0, pa), (1, pb)):
                c0 = h * 256 + 128 + m * 64
                for k in range(4):
                    mm(cx, pdst[0:64, :], wq[:, k, c0:c0 + 64], cqn[:, k, t0:t0 + 512], k == 0, k == 3, [wq.r, cqn.r], [pdst.r])
            rq = rqs[g % 2]
            P.dma("sp", rq[:], I["ropeq"][:, :, t0:t0 + 512], writes=[rq.r])
            tt(cx, qtmp[:, 0, :], pa[0:64, :], rq[:, 0, :], ALU.mult, [pa.r, rq.r], [qtmp.r])
            tt(cx, qtmp[:, 1, :], pb[0:64, :], rq[:, 1, :], ALU.mult, [pb.r, rq.r], [qtmp.r])
            tt(cx, qtmp[:, 0, :], qtmp[:, 0, :], qtmp[:, 1, :], ALU.add, [qtmp.r], [qtmp.r])
            ts(cx, qrT[:, t0:t0 + 512], qtmp[:, 0, :], MLA_SCALE, None, ALU.mult, None, [qtmp.r], [qrT.r])
        for g in range(4):
            t0 = g * 512
            po, pd = pp[6], pp[7]
            def qk(j):
                k0 = j * 128
                pc = pp[j % 4]
                mm(cx, pc[:, :], knT[:, k0:k0 + 128], qnT[:, t0:t0 + 512], True, False, [knT.r, qnT.r], [pc.r])
                mm(cx, pc[:, :], krot[:, k0:k0 + 128], qrT[:, t0:t0 + 512], False, True, [krot.r, qrT.r], [pc.r])

            qk(0)
            qk(1)
            for j in range(NT):
                if j + 2 < NT:
                    qk(j + 2)
                pc = pp[j % 4]
                pt = pT[j % 3]
                act(cx, pt[:], pc[:, :], AF.Exp, [pc.r], [pt.r])
                mm(cx, po[:, :], vh[:, j, :], pt[:], j == 0, j == NT - 1, [vh.r, pt.r], [po.r])
                mm(cx, pd[:, :], cx.ones_bf[:], pt[:], j == 0, j == NT - 1, [cx.ones_bf.r, pt.r], [pd.r])
            recip(cx, rden[:], pd[:, :], [pd.r], [rden.r])
            ot_ = otl[g % 2]
            tt(cx, ot_[:], po[:, :], rden[:], ALU.mult, [po.r, rden.r], [ot_.r])
            P.dma("sp", S["catA"].t[h * 128:(h + 1) * 128, t0:t0 + 512], ot_[:], reads=[ot_.r], writes=[S["catA"].r])


def phase4(cx, ps_):
    nc, P, I, S = cx.nc, cx.P, cx.I, cx.S
    pp = cx.psum
    dv, pv = cx.dv, cx.pvec
    x2T = S["x2T"]
    x2v = x2T.t.rearrange("(m p) t -> p m t", p=128)
    xov = I["xoT"].rearrange("(m p) t -> p m t", p=128)
    x2n = sb(cx, ps_, "x2n", [128, 16, TQ], BF16)
    gateT = sb(cx, ps_, "gateT", [32, TQ])
    with ExitStack() as st:
        rstd2 = sb(cx, st, "rstd2", [128, TQ])
        cx.catT = sb(cx, st, "catT", [128, 16, TQ], BF16)
        P.dma("sp", cx.catT[:, 0:8, :], S["catA"].t.rearrange("(m p) t -> p m t", p=128), reads=[S["catA"].r], writes=[cx.catT.r])
        if cx.have_rwkv:
            selq = sb(cx, st, "selq", [128, 4])
            P.dma("sp", selq[:], I["selq"][:, :], writes=[selq.r])
            stg_ = sb(cx, st, "rstage", [128, 4, TQ], BF16)
            for mh in range(2):
                dst = cx.catT[:, 8 + mh * 4:12 + mh * 4, :]
                for j in range(4):
                    raj = S["rwkv_all%d" % j]
                    P.dma("sp", stg_[:], raj.t.rearrange("(m p) t -> p m t", p=128)[:, mh * 4:(mh + 1) * 4, :], reads=[raj.r], writes=[stg_.r])
                    if j == 0:
                        ts(cx, dst, stg_[:], selq[:, 0:1], None, ALU.mult, None, [stg_.r, selq.r], [cx.catT.r])
                    else:
                        stt(cx, dst, stg_[:], selq[:, j:j + 1], dst, ALU.mult, ALU.add, [stg_.r, selq.r, cx.catT.r], [cx.catT.r])
        else:
            P.op("pool", lambda e: e.memset(cx.catT[:, 8:16, :], 0.0), writes=[cx.catT.r])
        wo = [sb(cx, st, "wo%d" % i, [128, 16, 128], BF16) for i in range(2)]
        xt = [sb(cx, st, "xt%d" % i, [128, 512]) for i in range(3)]
        x2t = [sb(cx, st, "x2t%d" % i, [128, 512]) for i in range(3)]
        sqt = [sb(cx, st, "sqt%d" % i, [128, 512], BF16) for i in range(2)]
        wsrc = I["w_out"].rearrange("(k p) n -> p k n", p=128)
        it = 0
        for m in range(16):
            w = wo[m % 2]
            for kh in range(4):
                P.dma("pool", w[:, kh * 4:(kh + 1) * 4, :], wsrc[:, kh * 4:(kh + 1) * 4, m * 128:(m + 1) * 128], writes=[w.r])
            for g in range(4):
                t0 = g * 512
                pc = pp[it % 2]
                for k in range(16):
                    mm(cx, pc[:, :], w[:, k, :], cx.catT[:, k, t0:t0 + 512], k == 0, k == 15, [w.r, cx.catT.r], [pc.r])
                x_ = xt[it % 3]
                P.dma("sp", x_[:], xov[:, m, t0:t0 + 512], writes=[x_.r])
                o_ = x2t[it % 3]
                stt(cx, o_[:], pc[:, :], dv[:, 4, m:m + 1], x_[:], ALU.mult, ALU.add, [pc.r, dv.r, x_.r], [o_.r])
                P.dma("sp", x2v[:, m, t0:t0 + 512], o_[:], reads=[o_.r], writes=[x2T.r])
                q_ = sqt[it % 2]
                act(cx, q_[:], o_[:], AF.Square, [o_.r], [q_.r])
                mm(cx, pp[4 + g][:, :], cx.ones_bf[:], q_[:], m == 0, m == 15, [cx.ones_bf.r, q_.r], [pp[4 + g].r])
                it += 1
        for g in range(4):
            act(cx, rstd2[:, g * 512:(g + 1) * 512], pp[4 + g][:, :], AF.Ln, [pp[4 + g].r, cx.epsb.r], [rstd2.r], scale=1.0 / D, bias=cx.epsb[:, 0:1])
        act(cx, rstd2[:], rstd2[:], AF.Exp, [rstd2.r], [rstd2.r], scale=-0.5)
        wr = sb(cx, st, "wr", [128, 16, 36])
        P.dma("sp", wr[:], I["wr"].rearrange("(k p) n -> p k n", p=128), writes=[wr.r])
        brt = sb(cx, st, "brt", [36, 1])
        P.dma("sp", brt[:], I["br"][:, :], writes=[brt.r])
        xnf = [sb(cx, st, "xnf%d" % i, [128, 512]) for i in range(2)]
        it = 0
        for m in range(16):
            for g in range(4):
                t0 = g * 512
                x_ = xt[it % 3]
                P.dma("sp", x_[:], x2v[:, m, t0:t0 + 512], reads=[x2T.r], writes=[x_.r])
                f_ = xnf[it % 2]
                stt(cx, f_[:], x_[:], dv[:, 5, m:m + 1], rstd2[:, t0:t0 + 512], ALU.mult, ALU.mult, [x_.r, dv.r, rstd2.r], [f_.r])
                ts(cx, f_[:], f_[:], dv[:, 6, m:m + 1], None, ALU.add, None, [f_.r, dv.r], [f_.r])
                act(cx, x2n[:, m, t0:t0 + 512], f_[:], AF.Copy, [f_.r], [x2n.r])
                mm(cx, pp[4 + g][0:36, :], wr[:, m, :], f_[:], m == 0, m == 15, [wr.r, f_.r], [pp[4 + g].r])
                it += 1
        lgT = sb(cx, st, "lgT", [36, TQ])
        for g in range(4):
            ts(cx, lgT[:, g * 512:(g + 1) * 512], pp[4 + g][0:36, :], brt[:, 0:1], None, ALU.add, None, [pp[4 + g].r, brt.r], [lgT.r])
        L = sb(cx, st, "L", [128, 36])
        sc = sb(cx, st, "rsc", [128, 16])
        ohg = sb(cx, st, "ohg", [128, 4])
        eg = sb(cx, st, "eg", [128, 4])
        wi = sb(cx, st, "wi", [128, 8])
        oh1 = sb(cx, st, "oh1", [128, 8])
        oh2 = sb(cx, st, "oh2", [128, 8])
        msk = sb(cx, st, "msk", [128, 8])
        gw = sb(cx, st, "gw", [128, 8])
        g32 = sb(cx, st, "g32", [128, 32])
        for tt_ in range(16):
            pl = pp[tt_ % 2]
            P.op("pe", lambda e: e.transpose(pl[:, 0:36], lgT[:, tt_ * 128:(tt_ + 1) * 128], cx.ident[0:36, 0:36]), reads=[lgT.r, cx.ident.r], writes=[pl.r])
            cp(cx, L[:], pl[:, 0:36], [pl.r], [L.r])
            R_, W_ = [L.r, sc.r, ohg.r, eg.r, wi.r, oh1.r, oh2.r, msk.r, gw.r], None
            P.op("dve", lambda e: e.tensor_reduce(out=sc[:, 0:1], in_=L[:, 0:4], axis=mybir.AxisListType.X, op=ALU.max), reads=[L.r], writes=[sc.r])
            ts(cx, ohg[:], L[:, 0:4], sc[:, 0:1], None, ALU.is_ge, None, [L.r, sc.r], [ohg.r])
            ts(cx, sc[:, 1:2], sc[:, 0:1], -1.0, None, ALU.mult, None, [sc.r], [sc.r])
            act(cx, eg[:], L[:, 0:4], AF.Exp, [L.r, sc.r], [eg.r], bias=sc[:, 1:2], scale=1.0)
            P.op("dve", lambda e: e.tensor_reduce(out=sc[:, 2:3], in_=eg[:], axis=mybir.AxisListType.X, op=ALU.add), reads=[eg.r], writes=[sc.r])
            recip(cx, sc[:, 3:4], sc[:, 2:3], [sc.r], [sc.r])
            ts(cx, wi[:], L[:, 4:12], ohg[:, 0:1], None, ALU.mult, None, [L.r, ohg.r], [wi.r])
            for gi in range(1, 4):
                stt(cx, wi[:], L[:, 4 + gi * 8:12 + gi * 8], ohg[:, gi:gi + 1], wi[:], ALU.mult, ALU.add, [L.r, ohg.r, wi.r], [wi.r])
            P.op("dve", lambda e: e.tensor_reduce(out=sc[:, 4:5], in_=wi[:], axis=mybir.AxisListType.X, op=ALU.max), reads=[wi.r], writes=[sc.r])
            ts(cx, oh1[:], wi[:], sc[:, 4:5], None, ALU.is_ge, None, [wi.r, sc.r], [oh1.r])
            stt(cx, msk[:], oh1[:], -1e30, wi[:], ALU.mult, ALU.add, [oh1.r, wi.r], [msk.r])
            P.op("dve", lambda e: e.tensor_reduce(out=sc[:, 5:6], in_=msk[:], axis=mybir.AxisListType.X, op=ALU.max), reads=[msk.r], writes=[sc.r])
            ts(cx, oh2[:], msk[:], sc[:, 5:6], None, ALU.is_ge, None, [msk.r, sc.r], [oh2.r])
            tt(cx, sc[:, 6:7], sc[:, 5:6], sc[:, 4:5], ALU.subtract, [sc.r], [sc.r])
            act(cx, sc[:, 7:8], sc[:, 6:7], AF.Exp, [sc.r], [sc.r])
            ts(cx, sc[:, 8:9], sc[:, 7:8], 1.0, None, ALU.add, None, [sc.r], [sc.r])
            recip(cx, sc[:, 9:10], sc[:, 8:9], [sc.r], [sc.r])
            tt(cx, sc[:, 10:11], sc[:, 9:10], sc[:, 3:4], ALU.mult, [sc.r], [sc.r])
            tt(cx, sc[:, 11:12], sc[:, 10:11], sc[:, 7:8], ALU.mult, [sc.r], [sc.r])
            ts(cx, gw[:], oh1[:], sc[:, 10:11], None, ALU.mult, None, [oh1.r, sc.r], [gw.r])
            stt(cx, gw[:], oh2[:], sc[:, 11:12], gw[:], ALU.mult, ALU.add, [oh2.r, sc.r, gw.r], [gw.r])
            for gi in range(4):
                ts(cx, g32[:, gi * 8:(gi + 1) * 8], gw[:], ohg[:, gi:gi + 1], None, ALU.mult, None, [gw.r, ohg.r], [g32.r])
            pg = pp[2 + tt_ % 2]
            P.op("pe", lambda e: e.transpose(pg[0:32, 0:128], g32[:], cx.ident[:]), reads=[g32.r, cx.ident.r], writes=[pg.r])
            cp(cx, gateT[:, tt_ * 128:(tt_ + 1) * 128], pg[0:32, 0:128], [pg.r], [gateT.r])
    if "gateT" in cx.debug:
        P.dma("sp", S["gateT"].t[:, :], gateT[:], reads=[gateT.r], writes=[S["gateT"].r])
    P.barrier()
    with ExitStack() as st:
        HT = TQ // 2
        yacc = sb(cx, st, "yacc", [128, 16, HT])
        w1e = sb(cx, st, "w1e", [128, 16, 512], BF16)
        w3e = sb(cx, st, "w3e", [128, 16, 512], BF16)
        w2e = sb(cx, st, "w2e", [128, 4, D // 2], BF16)
        selr = [sb(cx, st, "selr%d" % i, [32, 128]) for i in range(2)]
        wstg = [sb(cx, st, "wstg%d" % i, [128, 512]) for i in range(3)]
        wsi = 0
        actg = sb(cx, st, "actg", [128, 4, HT], BF16)
        e1 = [sb(cx, st, "e1_%d" % i, [128, 512]) for i in range(2)]
        xt = [sb(cx, st, "mxt%d" % i, [128, 512]) for i in range(2)]
        sqt = [sb(cx, st, "msq%d" % i, [128, 512], BF16) for i in range(1)] * 2
        rstdf = sb(cx, st, "rstdf", [128, HT])
        for hf in range(2):
            h0 = hf * HT
            P.op("pool", lambda e: e.memset(yacc[:], 0.0), writes=[yacc.r])
            for ex in range(NEXP):
                w1s = I["w1"][ex].rearrange("(k p) n -> p k n", p=128)
                w3s = I["w3"][ex].rearrange("(k p) n -> p k n", p=128)
                w2s = I["w2"][ex].rearrange("(j p) n -> p j n", p=128)
                for kh in range(4):
                    P.dma("pool", w1e[:, kh * 4:(kh + 1) * 4, :], w1s[:, kh * 4:(kh + 1) * 4, :], writes=[w1e.r])
                for kh in range(16):
                    sg_ = wstg[wsi % len(wstg)]
                    P.dma("sp", sg_[:], w3s[:, kh, :], writes=[sg_.r])
                    act(cx, w3e[:, kh, :], sg_[:], AF.Copy, [sg_.r], [w3e.r])
                    wsi += 1
                it = 0
                sel = selr[ex % 2]
                P.op("pool", lambda e: e.memset(sel[:], 1.0), writes=[sel.r])
                P.op("pool", lambda e: e.affine_select(out=sel[:], in_=sel[:], pattern=[[0, 128]], compare_op=ALU.is_equal, fill=0.0,
                                                       base=-ex, channel_multiplier=1), reads=[sel.r], writes=[sel.r])
                for tg in range(HT // 512):
                    t0 = h0 + tg * 512
                    pgb = pp[6 + tg % 2]
                    mm(cx, pgb[:, :], sel[:], gateT[:, t0:t0 + 512], True, True, [sel.r, gateT.r], [pgb.r])
                    for j in range(4):
                        p1, p3 = pp[(it % 2) * 2], pp[(it % 2) * 2 + 1]
                        for k in range(16):
                            mm(cx, p1[:, :], w1e[:, k, j * 128:(j + 1) * 128], x2n[:, k, t0:t0 + 512], k == 0, k == 15, [w1e.r, x2n.r], [p1.r])
                        for k in range(16):
                            mm(cx, p3[:, :], w3e[:, k, j * 128:(j + 1) * 128], x2n[:, k, t0:t0 + 512], k == 0, k == 15, [w3e.r, x2n.r], [p3.r])
                        e_ = e1[it % 2]
                        act(cx, e_[:], p1[:, :], AF.Silu, [p1.r], [e_.r])
                        tt(cx, e_[:], e_[:], p3[:, :], ALU.mult, [e_.r, p3.r], [e_.r])
                        tt(cx, actg[:, j, tg * 512:(tg + 1) * 512], e_[:], pgb[:, :], ALU.mult, [e_.r, pgb.r], [actg.r])
                        it += 1
                it = 0
                for m in range(16):
                    if m % 8 == 0:
                        for j in range(4):
                            P.dma("pool", w2e[:, j, :], w2s[:, j, (m // 8) * 1024:(m // 8 + 1) * 1024], writes=[w2e.r])
                    for tg in range(HT // 512):
                        py = pp[4 + it % 2]
                        for j in range(4):
                            mm(cx, py[:, :], w2e[:, j, (m % 8) * 128:(m % 8 + 1) * 128], actg[:, j, tg * 512:(tg + 1) * 512], j == 0, j == 3, [w2e.r, actg.r], [py.r])
                        tt(cx, yacc[:, m, tg * 512:(tg + 1) * 512], yacc[:, m, tg * 512:(tg + 1) * 512], py[:, :], ALU.add, [yacc.r, py.r], [yacc.r])
                        it += 1
            it = 0
            for m in range(16):
                for tg in range(HT // 512):
                    t0 = h0 + tg * 512
                    x_ = xt[it % 2]
                    P.dma("sp", x_[:], x2v[:, m, t0:t0 + 512], reads=[x2T.r], writes=[x_.r])
                    o_ = e1[it % 2]
                    stt(cx, o_[:], yacc[:, m, tg * 512:(tg + 1) * 512], dv[:, 7, m:m + 1], x_[:], ALU.mult, ALU.add, [yacc.r, dv.r, x_.r], [o_.r])
                    P.dma("sp", x2v[:, m, t0:t0 + 512], o_[:], reads=[o_.r], writes=[x2T.r])
                    q_ = sqt[it % 2]
                    act(cx, q_[:], o_[:], AF.Square, [o_.r], [q_.r])
                    mm(cx, pp[6 + tg][:, :], cx.ones_bf[:], q_[:], m == 0, m == 15, [cx.ones_bf.r, q_.r], [pp[6 + tg].r])
                    it += 1
            for tg in range(HT // 512):
                act(cx, rstdf[:, tg * 512:(tg + 1) * 512], pp[6 + tg][:, :], AF.Ln, [pp[6 + tg].r, cx.epsb.r], [rstdf.r], scale=1.0 / D, bias=cx.epsb[:, 0:1])
            act(cx, rstdf[:], rstdf[:], AF.Exp, [rstdf.r], [rstdf.r], scale=-0.5)
            ov = cx.outT.t.rearrange("(m p) t -> p m t", p=128)
            it = 0
            for m in range(16):
                for tg in range(HT // 512):
                    t0 = h0 + tg * 512
                    x_ = xt[it % 2]
                    P.dma("sp", x_[:], x2v[:, m, t0:t0 + 512], reads=[x2T.r], writes=[x_.r])
                    o_ = e1[it % 2]
                    stt(cx, o_[:], x_[:], pv[:, PV_GFIN + m:PV_GFIN + m + 1], rstdf[:, tg * 512:(tg + 1) * 512], ALU.mult, ALU.mult,
                        [x_.r, pv.r, rstdf.r], [o_.r])
                    P.dma("sp", ov[:, m, t0:t0 + 512], o_[:], reads=[o_.r], writes=[cx.outT.r])
                    it += 1


def build_nc(debug=(), upto=99, rwkv=True, flags=None):
    nc = bass.Bass("TRN2", target_bir_lowering=False)
    cx = Ctx()
    cx.nc = nc
    cx.P = Prog(nc)
    cx.I = {}
    cx.S = {}
    cx.debug = debug
    cx.flags = flags or {}

    def inp(name, shape, dt=F32):
        cx.I[name] = nc.dram_tensor(name, list(shape), dt, kind="ExternalInput").ap()

    inp("xT", [D, T]); inp("xoT", [D, TQ]); inp("ctxT", [D, TC]); inp("cT", [128, 16, 2]); inp("w_mod", [D, 6 * D])
    inp("pvec", [128, PV_N]); inp("w1p", [D, NP1]); inp("rope", [64, 2, T]); inp("ropeq", [64, 2, TQ])
    inp("ident", [128, 128])
    if upto >= 3:
        inp("wq", [512, 8 * 256]); inp("wk", [256, 8 * 128]); inp("wv", [256, 8 * 128])
    if upto >= 4:
        inp("w_out", [D, D]); inp("wr", [D, 36]); inp("br", [36, 1])
    inp("rvec", [64, RV_N]); inp("mux", [64, 2, NRW, 64]); inp("upw", [64, 2, 2, 256]); inp("gup", [64, 3, 256])
    inp("m1mask", [128, 2, 128]); inp("p0mask", [64, 2, 64]); inp("selq", [128, 4])
    if upto >= 4:
        inp("w1", [NEXP, D, DEXP]); inp("w3", [NEXP, D, DEXP]); inp("w2", [NEXP, DEXP, D])

    def scr(name, shape, dt):
        kind = "ExternalOutput" if name in debug else "Internal"
        cx.S[name] = dram(cx, name, shape, dt, kind=kind)

    scr("rawp", [NRW * 64, T + 2], F32)
    scr("rawc", [NRW * 64, TC + 2], F32)
    scr("ckvnT", [256, NKV], BF16)
    scr("krotT", [64, NKV], BF16)
    scr("cqnT", [512, TQ], BF16)
    scr("x2T", [D, TQ], F32)
    scr("catA", [1024, TQ], BF16)
    scr("unit", [NCK * 64, 8 * 4 * 64], F32)
    scr("states", [NCK * 64, 8 * 64], F32)
    scr("rest", [(T // CH) * 64, 2 * 4 * 64], F32)
    for j in range(4):
        cx.S["rwkv_in%d" % j] = dram(cx, "rwkv_in%d" % j, [256, TQ], BF16, kind="Internal")
        cx.S["rwkv_all%d" % j] = dram(cx, "rwkv_all%d" % j, [1024, TQ], BF16, kind="Internal", addr_space="Local")
    if "rwkv_dbg" in debug:
        scr("rwkv_dbg", [256, T], BF16)
    scr("gateT", [32, TQ], F32)
    cx.outT = dram(cx, "outT", [D, TQ], F32, kind="ExternalOutput")

    with ExitStack() as gstack:
        cx.gstack = gstack
        cx.psum = []
        for i in range(8):
            t = gstack.enter_context(nc.psum_tensor("ps%d" % i, [128, 512], F32))
            cx.psum.append(Tl(cx.P, t, "ps%d" % i))
        cx.q0 = None
        phase0(cx)
        if "modv" in debug:
            cx.S["modv"] = dram(cx, "modv", [128, 192], F32, kind="ExternalOutput")
            cx.P.dma("sp", cx.S["modv"].t[:, :], cx.modv[:].rearrange("p j c -> p (j c)"), reads=[cx.modv.r], writes=[cx.S["modv"].r])
        cx.P.barrier()
        if upto >= 1 and not cx.flags.get("skip1"):
            phase1(cx)
            cx.P.barrier()
        cx.have_rwkv = False
        if upto >= 2 and rwkv:
            with ExitStack() as st2:
                phase2(cx, st2)
            cx.P.barrier()
            cx.have_rwkv = True
        if upto >= 3:
            with ExitStack() as st3:
                phase3(cx, st3)
            cx.P.barrier()
            if upto >= 4:
                with ExitStack() as st4:
                    phase4(cx, st4)
                cx.P.barrier()
        finals = [cx.outT] + [cx.S[n] for n in debug if n in cx.S]
        cx.P.wait_all("sp", [f.r for f in finals])
    cx.nc_ninst = cx.P.ninst
    return nc, cx


def rope_tables():
    rows = T // GRID_W
    row, col = np.meshgrid(np.arange(rows), np.arange(GRID_W), indexing="ij")
    inv_freq = (10000.0 ** (-np.arange(0, 32, 2, dtype=np.float32) / 32)).astype(np.float32)
    ang_r = row.reshape(-1)[:, None].astype(np.float32) * inv_freq
    ang_c = col.reshape(-1)[:, None].astype(np.float32) * inv_freq
    Ct = np.concatenate([np.cos(ang_r), np.cos(ang_r), np.cos(ang_c), np.cos(ang_c)], axis=1).T
    St = np.concatenate([-np.sin(ang_r), np.sin(ang_r), -np.sin(ang_c), np.sin(ang_c)], axis=1).T
    return np.ascontiguousarray(np.stack([Ct, St], axis=1).astype(np.float32))


ROPE_PERM = np.concatenate([np.arange(16, 32), np.arange(0, 16), np.arange(48, 64), np.arange(32, 48)])


def fm(v, p=128):
    return np.ascontiguousarray(v.reshape(-1, p).T)


def prep_core(inp, c, shared):
    b, q = c // 4, c % 4
    m = {}
    m["xT"] = shared["xT"][b]
    m["xoT"] = np.ascontiguousarray(shared["xT"][b][:, q * TQ:(q + 1) * TQ])
    m["ctxT"] = shared["ctxT"][b]
    m["cT"] = np.ascontiguousarray(np.stack([fm(inp["c"][b]), fm(inp["c_ctx"])], axis=-1))
    m["w_mod"] = shared["w_mod"]
    m["pvec"] = shared["pvec"]
    w_in = inp["w_in"][0]
    cols = list(range(0, 832)) + list(768 + ROPE_PERM)
    for base in (832, 1856, 2880):
        for i in range(4):
            h = 4 * q + i
            cols += list(range(base + h * 64, base + (h + 1) * 64))
    cols += list(range(3904, 4192))
    m["w1p"] = np.ascontiguousarray(w_in[:, cols])
    assert m["w1p"].shape[1] == NP1
    m["rope"] = shared["rope"]
    m["ropeq"] = np.ascontiguousarray(shared["rope"][:, :, q * TQ:(q + 1) * TQ])
    m["ident"] = shared["ident"]
    for k in ("wq", "wk", "wv", "w_out", "wr", "br", "w1", "w2", "w3", "m1mask", "p0mask"):
        m[k] = shared[k]
    hs = [4 * q + i for i in range(4)]
    rvv = np.zeros((64, RV_N), np.float32)
    for i, h in enumerate(hs):
        cs = slice(h * 64, (h + 1) * 64)
        for d in range(2):
            rvv[:, RV_W0 + d * 4 + i] = inp["decay_w0"][0][d][cs]
            rvv[:, RV_A0 + d * 4 + i] = inp["iclr_a0"][0][d][cs]
        rvv[:, RV_KK + i] = inp["key_k"][0][cs]
        rvv[:, RV_KA + i] = inp["key_a"][0][cs]
        rvv[:, RV_RK + i] = inp["bonus_r_k"][0][h]
        rvv[:, RV_LG + i] = inp["lnx_g"][0][cs]
        rvv[:, RV_LB + i] = inp["lnx_b"][0][cs]
    m["rvec"] = rvv
    smu = inp["shift_mu"][0]
    mux = np.zeros((64, 2, NRW, 64), np.float32)
    mi = 0
    for base in (0, 1024, 2048):
        for h in hs:
            for a in range(2):
                mux[:, a, mi, :] = smu[a][base + h * 64: base + (h + 1) * 64][:, None]
            mi += 1
    for c0, n in ((3072, 64), (3136, 64), (3200, 64), (3264, 64), (3328, 32)):
        for a in range(2):
            mux[:n, a, mi, :] = smu[a][c0:c0 + n][:, None]
        mi += 1
    m["mux"] = mux
    cols = slice(4 * q * 64, 4 * q * 64 + 256)
    upw = np.zeros((64, 2, 2, 256), np.float32)
    for d in range(2):
        upw[:, 0, d, :] = inp["decay_up"][0][d][:, cols]
        upw[:, 1, d, :] = inp["iclr_up"][0][d][:, cols]
    m["upw"] = upw
    gup = np.zeros((64, 3, 256), np.float32)
    gu = inp["gate_up"][0]
    gup[:, 0, :] = gu[0:64, cols]; gup[:, 1, :] = gu[64:128, cols]; gup[:32, 2, :] = gu[128:160, cols]
    m["gup"] = gup
    sq_ = np.zeros((128, 4), np.float32); sq_[:, q] = 1.0
    m["selq"] = sq_
    return m


def prep_shared(inp):
    sh = {}
    sh["xT"] = [np.ascontiguousarray(inp["x"][b].T) for b in range(2)]
    sh["ctxT"] = [np.ascontiguousarray(inp["ctx"][b].T) for b in range(2)]
    sh["w_mod"] = np.ascontiguousarray(inp["w_mod"][0])
    pv = np.zeros((128, PV_N), np.float32)
    pv[:, PV_BMOD:PV_BMOD + 96] = fm(inp["b_mod"][0])
    pv[:, PV_GATTN:PV_GATTN + 16] = fm(inp["norm_attn_g"][0])
    pv[:, PV_GFFN:PV_GFFN + 16] = fm(inp["norm_ffn_g"][0])
    pv[:, PV_GFIN:PV_GFIN + 16] = fm(inp["final_norm_g"])
    pv[:, PV_QNG:PV_QNG + 4] = fm(inp["q_norm_g"][0])
    pv[:, PV_KVNG:PV_KVNG + 2] = fm(inp["kv_norm_g"][0])
    sh["pvec"] = pv
    sh["rope"] = rope_tables()
    sh["ident"] = np.eye(128, dtype=np.float32)
    wuq = inp["w_uq"][0].reshape(512, 8, 192)
    sh["wq"] = np.ascontiguousarray(np.concatenate([wuq, wuq[:, :, 128 + ROPE_PERM]], axis=2).reshape(512, 8 * 256))
    wukv = inp["w_ukv"][0].reshape(256, 8, 256)
    sh["wk"] = np.ascontiguousarray(wukv[:, :, :128].reshape(256, 1024))
    sh["wv"] = np.ascontiguousarray(wukv[:, :, 128:].reshape(256, 1024))
    sh["w_out"] = np.ascontiguousarray(inp["w_out"][0])
    sh["wr"] = np.ascontiguousarray(np.concatenate([inp["w_grp"][0], inp["w_exp"][0]], axis=1))
    sh["br"] = np.ascontiguousarray(np.concatenate([inp["b_grp"][0], inp["b_exp"][0]])[:, None])
    lo_s = np.tril(np.ones((64, 64), np.float32), -1)
    lo_i = np.tril(np.ones((64, 64), np.float32), 0)
    m1 = np.zeros((128, 2, 128), np.float32)
    p0 = np.zeros((64, 2, 64), np.float32)
    for d, (ts_, ti_) in enumerate(((lo_s, lo_i), (lo_s.T, lo_i.T))):
        blk = np.block([[ts_.T, ti_.T], [ts_.T, ti_.T]])
        m1[:, d, :] = blk
        p0[:, d, :] = ts_
    sh["m1mask"] = m1; sh["p0mask"] = p0
    sh["w1"] = np.ascontiguousarray(inp["w1"][0]); sh["w3"] = np.ascontiguousarray(inp["w3"][0]); sh["w2"] = np.ascontiguousarray(inp["w2"][0])
    return sh


def kernel(**inputs):
    inp = {k: np.asarray(v) for k, v in inputs.items()}
    shared = prep_shared(inp)
    nc, cx = build_nc()
    in_maps = [prep_core(inp, c, shared) for c in range(8)]
    res = run_bass_kernel_spmd(nc, in_maps, core_ids=list(range(8)))
    out = np.zeros((2, T, D), np.float32)
    for c in range(8):
        b, q = c // 4, c % 4
        out[b, q * TQ:(q + 1) * TQ, :] = res.results[c]["outT"].T
    return out
```

```python
from contextlib import ExitStack
import numpy as np
import ml_dtypes
import concourse.bass as bass
import concourse.mybir as mybir
from concourse.bass_utils import run_bass_kernel_spmd

F32 = mybir.dt.float32
BF16 = mybir.dt.bfloat16
AF = mybir.ActivationFunctionType
ALU = mybir.AluOpType

D = 2048
T = 8192
TC = 256
TQ = 2048
NKV = T + TC
GRID_W = 64
NEXP = 32
DEXP = 512
EPS = 1e-6
LNX_EPS = 64e-5
MLA_SCALE = 192.0 ** -0.5
CH = 64

PCH = []
_o = 0
for _n, _m in ([("cq%d" % i, 128) for i in range(4)] + [("ckv0", 128), ("ckv1", 128), ("kr", 64), ("krsw", 64)]
               + [("r%d" % i, 64) for i in range(4)] + [("k%d" % i, 64) for i in range(4)]
               + [("v%d" % i, 64) for i in range(4)] + [("wl", 64), ("al", 64), ("gl0", 64), ("gl1", 64), ("gl2", 32)]):
    PCH.append((_n, _m, _o))
    _o += _m
NP1 = _o
RW_NAMES = [n for n, _, _ in PCH[8:]]
NRW = len(RW_NAMES)

PV_BMOD, PV_GATTN, PV_GFFN, PV_GFIN, PV_QNG, PV_KVNG, PV_N = 0, 96, 112, 128, 144, 148, 150


class Res:
    __slots__ = ("name", "lw", "rd", "sem", "dcount")

    def __init__(self, name):
        self.name = name
        self.lw = None
        self.rd = {}
        self.sem = None
        self.dcount = 0


class Prog:
    def __init__(self, nc):
        self.nc = nc
        self.engs = {"pe": nc.tensor, "act": nc.scalar, "dve": nc.vector, "pool": nc.gpsimd, "sp": nc.sync}
        self.sem = {}
        self.cnt = {}
        self.known = {}
        for e in self.engs:
            self.sem[e] = nc.alloc_semaphore("s_" + e)
            self.cnt[e] = 0
            self.known[e] = {}
        self.semown = {id(self.sem[e]): e for e in self.engs}
        self.ninst = 0
        self.all_dma = []
        self.retired = []

    def res(self, name):
        return Res(name)

    def _deps(self, e, reads, writes):
        deps = {}

        def add(tok):
            s, v = tok
            k = id(s)
            if k not in deps or deps[k][1] < v:
                deps[k] = (s, v)

        for r in reads:
            if r.lw is not None:
                add(r.lw)
        for w in writes:
            if w.lw is not None:
                add(w.lw)
            for t in w.rd.values():
                add(t)
        eng = self.engs[e]
        for k, (s, v) in deps.items():
            if e == "pe" and self.semown.get(k) == "pe":
                continue
            if self.known[e].get(k, 0) < v:
                eng.wait_ge(s, v)
                self.known[e][k] = v
                self.ninst += 1

    def _post(self, tok, reads, writes):
        k = id(tok[0])
        for r in reads:
            if k not in r.rd or r.rd[k][1] < tok[1]:
                r.rd[k] = tok
        for w in writes:
            w.lw = tok
            w.rd = {}

    def op(self, e, fn, reads=(), writes=(), sig=True):
        self._deps(e, reads, writes)
        inst = fn(self.engs[e])
        self.ninst += 1
        if sig:
            self.cnt[e] += 1
            inst.then_inc(self.sem[e], 1)
            self._post((self.sem[e], self.cnt[e]), reads, writes)
        else:
            self._post((self.sem[e], self.cnt[e] + 1), reads, writes)

    def dma(self, q, out, in_, reads=(), writes=(), **kw):
        self._deps(q, reads, writes)
        w = writes[0]
        if w.sem is None:
            w.sem = self.nc.alloc_semaphore("d_" + w.name)
            self.all_dma.append(w)
        inst = self.engs[q].dma_start(out=out, in_=in_, **kw)
        w.dcount += 16
        inst.then_inc(w.sem, 16)
        self.ninst += 1
        self._post((w.sem, w.dcount), reads, writes)

    def barrier(self):
        for e in self.engs:
            eng = self.engs[e]
            for f in self.engs:
                if f == e:
                    continue
                k = id(self.sem[f])
                if self.cnt[f] > 0 and self.known[e].get(k, 0) < self.cnt[f]:
                    eng.wait_ge(self.sem[f], self.cnt[f])
                    self.known[e][k] = self.cnt[f]
                    self.ninst += 1
            for r in self.all_dma:
                k = id(r.sem)
                if self.known[e].get(k, 0) < r.dcount:
                    eng.wait_ge(r.sem, r.dcount)
                    self.known[e][k] = r.dcount
                    self.ninst += 1
        for f in self.engs:
            if self.cnt[f] > 20000:
                self.retired.append(self.sem[f])
                self.sem[f] = self.nc.alloc_semaphore("s_%s_%d" % (f, len(self.retired)))
                self.semown[id(self.sem[f])] = f
                self.cnt[f] = 0

    def wait_all(self, e, resources):
        self._deps(e, resources, [])


class Tl:
    def __init__(self, P, t, name):
        self.t = t
        self.r = P.res(name)

    def __getitem__(self, idx):
        return self.t[idx]


class Ctx:
    pass


def sb(cx, stack, name, shape, dt=F32):
    t = stack.enter_context(cx.nc.sbuf_tensor("sb_" + name, shape, dt))
    return Tl(cx.P, t, name)


def dram(cx, name, shape, dt, kind="Internal", **kw):
    t = cx.nc.dram_tensor(name, shape, dt, kind=kind, **kw).ap()
    tl = Tl(cx.P, t, name)
    return tl


def mm(cx, out, lhsT, rhs, start, stop, reads, writes, sig=None):
    cx.P.op("pe", lambda e: e.matmul(out, lhsT=lhsT, rhs=rhs, start=start, stop=stop), reads=reads, writes=writes,
            sig=bool(stop) if sig is None else sig)


def act(cx, out, in_, func, reads, writes, eng="act", **kw):
    cx.P.op("act", lambda e: e.activation(out=out, in_=in_, func=func, **kw), reads=reads, writes=writes)


def tt(cx, out, in0, in1, op, reads, writes, eng="dve"):
    cx.P.op(eng, lambda e: e.tensor_tensor(out=out, in0=in0, in1=in1, op=op), reads=reads, writes=writes)


def ts(cx, out, in0, s1, s2, op0, op1, reads, writes, eng="dve"):
    if op1 is None and eng == "pool":
        op1, s2 = (ALU.mult, 1.0) if op0 == ALU.add else (ALU.add, 0.0)
    if op1 is None:
        cx.P.op(eng, lambda e: e.tensor_scalar(out=out, in0=in0, scalar1=s1, scalar2=None, op0=op0), reads=reads, writes=writes)
    else:
        cx.P.op(eng, lambda e: e.tensor_scalar(out=out, in0=in0, scalar1=s1, scalar2=s2, op0=op0, op1=op1), reads=reads, writes=writes)


def stt(cx, out, in0, scalar, in1, op0, op1, reads, writes):
    cx.P.op("dve", lambda e: e.scalar_tensor_tensor(out=out, in0=in0, scalar=scalar, in1=in1, op0=op0, op1=op1), reads=reads, writes=writes)


def cp(cx, out, in_, reads, writes, eng="dve"):
    cx.P.op(eng, lambda e: e.tensor_copy(out=out, in_=in_), reads=reads, writes=writes)


def recip(cx, out, in_, reads, writes):
    cx.P.op("dve", lambda e: e.reciprocal(out=out, in_=in_), reads=reads, writes=writes)


def rsqrt_inplace(cx, tl, ap, scale, bias):
    act(cx, ap, ap, AF.Ln, [tl.r, cx.epsb.r], [tl.r], scale=scale, bias=bias)
    act(cx, ap, ap, AF.Exp, [tl.r], [tl.r], scale=-0.5)


def phase0(cx):
    nc, P, I = cx.nc, cx.P, cx.I
    st = cx.gstack
    cx.ident = sb(cx, st, "ident", [128, 128])
    P.dma("sp", cx.ident[:], I["ident"][:, :], writes=[cx.ident.r])
    cx.ones_bf = sb(cx, st, "ones_bf", [128, 128], BF16)
    P.op("pool", lambda e: e.memset(cx.ones_bf[:], 1.0), writes=[cx.ones_bf.r])
    cx.ones_f = sb(cx, st, "ones_f", [128, 128])
    P.op("pool", lambda e: e.memset(cx.ones_f[:], 1.0), writes=[cx.ones_f.r])
    cx.epsb = sb(cx, st, "epsb", [128, 4])
    P.op("pool", lambda e: e.memset(cx.epsb[:, 0:1], EPS), writes=[cx.epsb.r])
    P.op("pool", lambda e: e.memset(cx.epsb[:, 1:2], LNX_EPS), writes=[cx.epsb.r])
    P.op("pool", lambda e: e.memset(cx.epsb[:, 2:3], 1e-12), writes=[cx.epsb.r])
    P.op("pool", lambda e: e.memset(cx.epsb[:, 3:4], 0.0), writes=[cx.epsb.r])
    cx.pvec = sb(cx, st, "pvec", [128, PV_N])
    P.dma("sp", cx.pvec[:], I["pvec"][:, :], writes=[cx.pvec.r])
    cx.modv = sb(cx, st, "modv", [128, 96, 2])
    cx.dv = sb(cx, st, "dv", [128, 8, 16])

    with ExitStack() as ps:
        cT = sb(cx, ps, "cT", [128, 16, 2])
        sg = sb(cx, ps, "sg", [128, 16, 2])
        P.dma("sp", cT[:], I["cT"][:, :, :], writes=[cT.r])
        act(cx, sg[:], cT[:], AF.Exp, [cT.r], [sg.r], scale=-1.0)
        ts(cx, sg[:], sg[:], 1.0, None, ALU.add, None, [sg.r], [sg.r])
        recip(cx, sg[:], sg[:], [sg.r], [sg.r])
        tt(cx, sg[:], sg[:], cT[:], ALU.mult, [sg.r, cT.r], [sg.r])
        wbuf = [sb(cx, ps, "wmod%d" % i, [128, 16, 512]) for i in range(2)]
        pm = cx.psum[0]
        wsrc = I["w_mod"].rearrange("(k p) n -> p k n", p=128)
        for blk in range(0 if not cx.flags.get("fast0") else 24, 24):
            wb = wbuf[blk % 2]
            for kh in range(4):
                P.dma("sp", wb[:, kh * 4:(kh + 1) * 4, :], wsrc[:, kh * 4:(kh + 1) * 4, blk * 512:(blk + 1) * 512], writes=[wb.r])
            for jj in range(4):
                j = blk * 4 + jj
                for k in range(16):
                    mm(cx, pm[:, j * 2:j * 2 + 2], wb[:, k, jj * 128:(jj + 1) * 128], sg[:, k, :], k == 0, k == 15,
                       [wb.r, sg.r], [pm.r])
        for col in range(2):
            tt(cx, cx.modv[:, :, col], pm[:, 0:192].rearrange("p (j c) -> p j c", c=2)[:, :, col], cx.pvec[:, PV_BMOD:PV_BMOD + 96],
               ALU.add, [pm.r, cx.pvec.r], [cx.modv.r])
    dv, mv, pv = cx.dv, cx.modv, cx.pvec
    for col in range(2):
        stt(cx, dv[:, col, :], mv[:, 16:32, col], 1.0, pv[:, PV_GATTN:PV_GATTN + 16], ALU.add, ALU.mult, [mv.r, pv.r], [dv.r])
        cp(cx, dv[:, 2 + col, :], mv[:, 0:16, col], [mv.r], [dv.r])
    cp(cx, dv[:, 4, :], mv[:, 32:48, 0], [mv.r], [dv.r])
    stt(cx, dv[:, 5, :], mv[:, 64:80, 0], 1.0, pv[:, PV_GFFN:PV_GFFN + 16], ALU.add, ALU.mult, [mv.r, pv.r], [dv.r])
    cp(cx, dv[:, 6, :], mv[:, 48:64, 0], [mv.r], [dv.r])
    cp(cx, dv[:, 7, :], mv[:, 80:96, 0], [mv.r], [dv.r])


def phase1(cx):
    nc, P, I, S = cx.nc, cx.P, cx.I, cx.S
    G = 256
    q0 = cx.q0
    with ExitStack() as ps:
        w1p = sb(cx, ps, "w1p", [128, 16, NP1], BF16)
        wsrc = I["w1p"].rearrange("(k p) n -> p k n", p=128)
        for k in range(16):
            P.dma("pool", w1p[:, k, :], wsrc[:, k, :], writes=[w1p.r])
        xb = [sb(cx, ps, "xg%d" % i, [128, 16, G]) for i in range(2)]
        sq = sb(cx, ps, "sq", [128, 16, G], BF16)
        hT = sb(cx, ps, "hT", [128, 16, G], BF16)
        rstd = sb(cx, ps, "rstd", [128, G])
        tmp = [sb(cx, ps, "tmp%d" % i, [128, G]) for i in range(2)]
        stg = [sb(cx, ps, "stg%d" % i, [64, NRW, G]) for i in range(2)]
        kvs = [sb(cx, ps, "kvs%d" % i, [128, 2, G]) for i in range(2)]
        kvq = sb(cx, ps, "kvq", [128, 2, G], BF16)
        kvr = sb(cx, ps, "kvr", [128, G])
        kvo = [sb(cx, ps, "kvo%d" % i, [128, 2, G], BF16) for i in range(2)]
        krs = [sb(cx, ps, "krs%d" % i, [64, 2, G]) for i in range(2)]
        kro = [sb(cx, ps, "kro%d" % i, [64, G], BF16) for i in range(2)]
        rp = [sb(cx, ps, "rp%d" % i, [64, 2, G]) for i in range(2)]
        cqs = sb(cx, ps, "cqs", [128, 4, G])
        cqq = sb(cx, ps, "cqq", [128, 4, G], BF16)
        cqo = [sb(cx, ps, "cqo%d" % i, [128, 4, G], BF16) for i in range(2)]
        zt = sb(cx, ps, "zt", [64, NRW, 1])
        P.op("pool", lambda e: e.memset(zt[:], 0.0), writes=[zt.r])
        for tl_, n in ((S["rawp"], T), (S["rawc"], TC)):
            v = tl_.t.rearrange("(m p) t -> p m t", p=64)
            P.dma("sp", v[:, :, 0:1], zt[:], reads=[zt.r], writes=[tl_.r], allow_slow_non_contiguous=True)
            P.dma("sp", v[:, :, n + 1:n + 2], zt[:], reads=[zt.r], writes=[tl_.r], allow_slow_non_contiguous=True)

        groups = [("c", i) for i in range(TC // G)] + [("x", i) for i in range(T // G)] + [("o", i) for i in range(TQ // G)]
        pp = cx.psum
        for gi, (kind, i) in enumerate(groups):
            xg = xb[gi % 2]
            t0 = i * G
            if kind == "c":
                src = I["ctxT"].rearrange("(k p) t -> p k t", p=128)[:, :, t0:t0 + G]
                col = 1
            elif kind == "x":
                src = I["xT"].rearrange("(k p) t -> p k t", p=128)[:, :, t0:t0 + G]
                col = 0
            else:
                src = I["xoT"].rearrange("(k p) t -> p k t", p=128)[:, :, t0:t0 + G]
                col = 0
            for kh in range(2):
                P.dma("sp", xg[:, kh * 8:(kh + 1) * 8, :], src[:, kh * 8:(kh + 1) * 8, :], writes=[xg.r])
            act(cx, sq[:], xg[:], AF.Square, [xg.r], [sq.r])
            pss = pp[gi % 2]
            for k in range(16):
                mm(cx, pss[:, 0:G], cx.ones_bf[:], sq[:, k, :], k == 0, k == 15, [cx.ones_bf.r, sq.r], [pss.r])
            act(cx, rstd[:], pss[:, 0:G], AF.Ln, [pss.r, cx.epsb.r], [rstd.r], scale=1.0 / D, bias=cx.epsb[:, 0:1])
            act(cx, rstd[:], rstd[:], AF.Exp, [rstd.r], [rstd.r], scale=-0.5)
            for k in range(16):
                tm = tmp[k % 2]
                stt(cx, tm[:], xg[:, k, :], cx.dv[:, col, k:k + 1], rstd[:], ALU.mult, ALU.mult, [xg.r, cx.dv.r, rstd.r], [tm.r])
                if k % 2 == 0:
                    act(cx, hT[:, k, :], tm[:], AF.Identity, [tm.r, cx.dv.r], [hT.r], bias=cx.dv[:, 2 + col, k:k + 1], scale=1.0)
                else:
                    ts(cx, hT[:, k, :], tm[:], cx.dv[:, 2 + col, k:k + 1], None, ALU.add, None, [tm.r, cx.dv.r], [hT.r], eng="pool")
            if kind == "o":
                for m in range(4):
                    pc = pp[2 + m % 2]
                    for k in range(16):
                        mm(cx, pc[:, 0:G], w1p[:, k, m * 128:(m + 1) * 128], hT[:, k, :], k == 0, k == 15, [w1p.r, hT.r], [pc.r])
                    cp(cx, cqs[:, m, :], pc[:, 0:G], [pc.r], [cqs.r])
                act(cx, cqq[:], cqs[:], AF.Square, [cqs.r], [cqq.r])
                pq = pp[4]
                for m in range(4):
                    mm(cx, pq[:, 0:G], cx.ones_bf[:], cqq[:, m, :], m == 0, m == 3, [cx.ones_bf.r, cqq.r], [pq.r])
                act(cx, kvr[:], pq[:, 0:G], AF.Ln, [pq.r, cx.epsb.r], [kvr.r], scale=1.0 / 512, bias=cx.epsb[:, 0:1])
                act(cx, kvr[:], kvr[:], AF.Exp, [kvr.r], [kvr.r], scale=-0.5)
                co = cqo[i % 2]
                for m in range(4):
                    stt(cx, co[:, m, :], cqs[:, m, :], cx.pvec[:, PV_QNG + m:PV_QNG + m + 1], kvr[:], ALU.mult, ALU.mult,
                        [cqs.r, cx.pvec.r, kvr.r], [co.r])
                P.dma("sp", S["cqnT"].t.rearrange("(m p) t -> p m t", p=128)[:, :, t0:t0 + G], co[:], reads=[co.r], writes=[S["cqnT"].r])
                continue
            kv = kvs[gi % 2]
            for m in range(2):
                pc = pp[2 + m]
                c0 = 512 + m * 128
                for k in range(16):
                    mm(cx, pc[:, 0:G], w1p[:, k, c0:c0 + 128], hT[:, k, :], k == 0, k == 15, [w1p.r, hT.r], [pc.r])
                cp(cx, kv[:, m, :], pc[:, 0:G], [pc.r], [kv.r])
            act(cx, kvq[:], kv[:], AF.Square, [kv.r], [kvq.r])
            pq = pp[4]
            for m in range(2):
                mm(cx, pq[:, 0:G], cx.ones_bf[:], kvq[:, m, :], m == 0, m == 1, [cx.ones_bf.r, kvq.r], [pq.r])
            act(cx, kvr[:], pq[:, 0:G], AF.Ln, [pq.r, cx.epsb.r], [kvr.r], scale=1.0 / 256, bias=cx.epsb[:, 0:1])
            act(cx, kvr[:], kvr[:], AF.Exp, [kvr.r], [kvr.r], scale=-0.5)
            ko = kvo[gi % 2]
            for m in range(2):
                stt(cx, ko[:, m, :], kv[:, m, :], cx.pvec[:, PV_KVNG + m:PV_KVNG + m + 1], kvr[:], ALU.mult, ALU.mult,
                    [kv.r, cx.pvec.r, kvr.r], [ko.r])
            kvoff = T + t0 if kind == "c" else t0
            P.dma("sp", S["ckvnT"].t.rearrange("(m p) t -> p m t", p=128)[:, :, kvoff:kvoff + G], ko[:], reads=[ko.r], writes=[S["ckvnT"].r])
            kr_ = krs[gi % 2]
            pc = pp[5]
            for m in range(2):
                c0 = 768 + m * 64
                for k in range(16):
                    mm(cx, pc[0:64, m * G:(m + 1) * G], w1p[:, k, c0:c0 + 64], hT[:, k, :], k == 0, k == 15, [w1p.r, hT.r], [pc.r])
            o_ = kro[gi % 2]
            if kind == "c":
                cp(cx, o_[:], pc[0:64, 0:G], [pc.r], [o_.r])
            else:
                r_ = rp[gi % 2]
                P.dma("sp", r_[:], I["rope"][:, :, t0:t0 + G], writes=[r_.r])
                tt(cx, kr_[:], pc[0:64, 0:2 * G].rearrange("p (m t) -> p m t", m=2), r_[:], ALU.mult, [pc.r, r_.r], [kr_.r])
                tt(cx, o_[:], kr_[:, 0, :], kr_[:, 1, :], ALU.add, [kr_.r], [o_.r])
            P.dma("sp", S["krotT"].t[:, kvoff:kvoff + G], o_[:], reads=[o_.r], writes=[S["krotT"].r])
            sg_ = stg[gi % 2]
            for m, (nm, M, c0) in enumerate(PCH[8:]):
                pc = pp[6 + m % 2]
                for k in range(16):
                    mm(cx, pc[0:M, 0:G], w1p[:, k, c0:c0 + M], hT[:, k, :], k == 0, k == 15, [w1p.r, hT.r], [pc.r])
                if m % 2 == 0:
                    cp(cx, sg_[0:M, m, :], pc[0:M, 0:G], [pc.r], [sg_.r])
                else:
                    act(cx, sg_[0:M, m, :], pc[0:M, 0:G], AF.Copy, [pc.r], [sg_.r])
            dst = S["rawc"] if kind == "c" else S["rawp"]
            P.dma("sp", dst.t.rearrange("(m p) t -> p m t", p=64)[:, :, 1 + t0:1 + t0 + G], sg_[:], reads=[sg_.r], writes=[dst.r])


NCK = (TC + T) // CH
RV_W0, RV_A0, RV_KK, RV_KA, RV_RK, RV_LG, RV_LB, RV_N = 0, 8, 16, 20, 24, 28, 32, 36


def phase2(cx, ps_):
    nc, P, I, S = cx.nc, cx.P, cx.I, cx.S
    pp = cx.psum
    X = mybir.AxisListType.X
    rv = sb(cx, ps_, "rv", [64, RV_N])
    P.dma("sp", rv[:], I["rvec"][:, :], writes=[rv.r])
    nrv = sb(cx, ps_, "nrv", [64, 16])
    ts(cx, nrv[:], rv[:, 0:16], -1.0, None, ALU.mult, None, [rv.r], [nrv.r])
    omka = sb(cx, ps_, "omka", [64, 4])
    ts(cx, omka[:], rv[:, RV_KA:RV_KA + 4], -1.0, 1.0, ALU.mult, ALU.add, [rv.r], [omka.r])
    mux = sb(cx, ps_, "mux", [64, 3, NRW, 64])
    P.dma("sp", mux[:, 0:2], I["mux"][:, :, :, :], writes=[mux.r])
    tt(cx, mux[:, 2], mux[:, 0], mux[:, 1], ALU.add, [mux.r], [mux.r])
    ts(cx, mux[:, 2], mux[:, 2], -1.0, 1.0, ALU.mult, ALU.add, [mux.r], [mux.r])
    upw = sb(cx, ps_, "upw", [64, 2, 2, 256])
    P.dma("sp", upw[:], I["upw"][:, :, :, :], writes=[upw.r])
    gup = sb(cx, ps_, "gup", [64, 3, 256])
    P.dma("sp", gup[:], I["gup"][:, :, :], writes=[gup.r])
    m1mask = sb(cx, ps_, "m1mask", [128, 2, 128])
    P.dma("sp", m1mask[:], I["m1mask"][:, :, :], writes=[m1mask.r])
    p0mask = sb(cx, ps_, "p0mask", [64, 2, 64])
    P.dma("sp", p0mask[:], I["p0mask"][:, :, :], writes=[p0mask.r])
    zer = sb(cx, ps_, "zer", [64, 64])
    P.op("pool", lambda e: e.memset(zer[:], 0.0), writes=[zer.r])
    ones64 = cx.ones_f
    idn = cx.ident
    unit_v = S["unit"].t.rearrange("(g p) n -> g p n", p=64)
    st_v = S["states"].t.rearrange("(g p) n -> g p n", p=64)
    rest_v = S["rest"].t.rearrange("(g p) n -> g p n", p=64)

    with ExitStack() as st:
        Wn = [sb(cx, st, "Wn%d" % i, [64, NRW, 66]) for i in range(2)]
        sh = sb(cx, st, "sh", [64, NRW, 64])
        t17 = sb(cx, st, "t17", [64, NRW, 64])
        kk = sb(cx, st, "kk", [64, 4, 64])
        t4 = sb(cx, st, "t4", [64, 4, 64])
        kkn = sb(cx, st, "kkn", [64, 4, 64])
        th = sb(cx, st, "th", [64, 64])
        wd = sb(cx, st, "wd", [64, 8, 64])
        aa = sb(cx, st, "aa", [64, 8, 64])
        kd = sb(cx, st, "kd", [64, 8, 64])
        pref = sb(cx, st, "pref", [64, 8, 65])
        rp = sb(cx, st, "rp", [64, 8, 65])
        tot = sb(cx, st, "tot", [64, 8])
        rtot = sb(cx, st, "rtot", [64, 8])
        GIN = sb(cx, st, "GIN", [64, 8, 64])
        GEX = sb(cx, st, "GEX", [64, 8, 64])
        GINV = sb(cx, st, "GINV", [64, 8, 64])
        BKs = [sb(cx, st, "BK%d" % i, [64, 8, 2, 64]) for i in range(2)]
        ARs = [sb(cx, st, "AR%d" % i, [64, 8, 2, 64]) for i in range(2)]
        AVs = [sb(cx, st, "AV%d" % i, [64, 8, 2, 64]) for i in range(2)]
        BKgs = [sb(cx, st, "BKg%d" % i, [64, 8, 2, 64]) for i in range(2)]
        t8 = sb(cx, st, "t8", [64, 8, 64])
        M1m = [sb(cx, st, "M1m%d" % u, [128, 128]) for u in range(8)]
        Z = [sb(cx, st, "Z%d" % u, [128, 128]) for u in range(8)]
        T2s = [sb(cx, st, "T2s%d" % u, [128, 64]) for u in range(8)]
        PQ = [sb(cx, st, "PQ%d" % u, [64, 2, 2, 64]) for u in range(8)]
        dgs = [[sb(cx, st, "dg%d_%d" % (i, u), [64, 64]) for u in range(8)] for i in range(2)]
        Ou = [sb(cx, st, "Ou%d" % i, [64, 8, 4, 64]) for i in range(2)]
        rst = [sb(cx, st, "rst%d" % i, [64, 2, 4, 64]) for i in range(2)]
        sg = sb(cx, st, "sgl", [64, 3, 64])
        P.op("pool", lambda e: e.memset(pref[:], 1.0), writes=[pref.r])

        def prep_gen(gc):
            par = gc % 2
            BK_, AR_, AV_, BKg_, dg_ = BKs[par], ARs[par], AVs[par], BKgs[par], dgs[par]
            lat = gc >= 4
            ci = gc - 4 if lat else gc
            src = (S["rawp"] if lat else S["rawc"])
            W = Wn[gc % 2]
            P.dma("sp", W[:], src.t.rearrange("(m p) t -> p m t", p=64)[:, :, ci * 64:ci * 64 + 66], reads=[src.r], writes=[W.r])
            yield
            tt(cx, sh[:], W[:, :, 1:65], mux[:, 2], ALU.mult, [W.r, mux.r], [sh.r])
            tt(cx, t17[:], W[:, :, 0:64], mux[:, 0], ALU.mult, [W.r, mux.r], [t17.r])
            tt(cx, sh[:], sh[:], t17[:], ALU.add, [sh.r, t17.r], [sh.r])
            tt(cx, t17[:], W[:, :, 2:66], mux[:, 1], ALU.mult, [W.r, mux.r], [t17.r], eng="pool")
            tt(cx, sh[:], sh[:], t17[:], ALU.add, [sh.r, t17.r], [sh.r])
            rs, ks, vs = sh[:, 0:4], sh[:, 4:8], sh[:, 8:12]
            yield
            for h in range(4):
                ts(cx, kk[:, h, :], sh[:, 4 + h, :], rv[:, RV_KK + h:RV_KK + h + 1], None, ALU.mult, None, [sh.r, rv.r], [kk.r], eng="pool")
            tt(cx, t4[:], kk[:], kk[:], ALU.mult, [kk.r], [t4.r])
            pa = pp[7]
            mm(cx, pa[0:64, 0:256], ones64[0:64, 0:64], t4[:].rearrange("p a b -> p (a b)"), True, True, [ones64.r, t4.r], [pa.r])
            act(cx, t4[:].rearrange("p a b -> p (a b)"), pa[0:64, 0:256], AF.Ln, [pa.r, cx.epsb.r], [t4.r], bias=cx.epsb[0:64, 2:3], scale=1.0)
            act(cx, t4[:], t4[:], AF.Exp, [t4.r], [t4.r], scale=-0.5)
            tt(cx, kkn[:], kk[:], t4[:], ALU.mult, [kk.r, t4.r], [kkn.r])
            yield
            act(cx, th[:], sh[:, 12, :], AF.Exp, [sh.r], [th.r], scale=-2.0)
            ts(cx, th[:], th[:], 1.0, None, ALU.add, None, [th.r], [th.r])
            recip(cx, th[:], th[:], [th.r], [th.r])
            ts(cx, th[:], th[:], 2.0, -1.0, ALU.mult, ALU.add, [th.r], [th.r])
            pw, pq = pp[7], pp[6]
            for u in range(8):
                d, h = u // 4, u % 4
                mm(cx, pw[0:64, u * 64:(u + 1) * 64], upw[:, 0, d, h * 64:(h + 1) * 64], th[:], True, True, [upw.r, th.r], [pw.r])
                mm(cx, pq[0:64, u * 64:(u + 1) * 64], upw[:, 1, d, h * 64:(h + 1) * 64], sh[:, 13, :], True, True, [upw.r, sh.r], [pq.r])
            for u in range(8):
                act(cx, wd[:, u, :], pw[0:64, u * 64:(u + 1) * 64], AF.Exp, [pw.r, nrv.r], [wd.r], scale=-1.0, bias=nrv[:, u:u + 1])
                act(cx, aa[:, u, :], pq[0:64, u * 64:(u + 1) * 64], AF.Exp, [pq.r, nrv.r], [aa.r], scale=-1.0, bias=nrv[:, 8 + u:9 + u])
            for tl_ in (wd, aa):
                ts(cx, tl_[:], tl_[:], 1.0, None, ALU.add, None, [tl_.r], [tl_.r])
                recip(cx, tl_[:], tl_[:], [tl_.r], [tl_.r])
            act(cx, wd[:], wd[:], AF.Exp, [wd.r], [wd.r], scale=-0.6065306597126334)
            yield
            for u in range(8):
                h = u % 4
                ts(cx, kd[:, u, :], aa[:, u, :], rv[:, RV_KA + h:RV_KA + h + 1], omka[:, h:h + 1], ALU.mult, ALU.add, [aa.r, rv.r, omka.r], [kd.r], eng="pool")
            yield
            for d in range(2):
                tt(cx, kd[:, d * 4:(d + 1) * 4], kd[:, d * 4:(d + 1) * 4], ks, ALU.mult, [kd.r, sh.r], [kd.r])
            yield
            for u in range(8):
                P.op("dve", lambda e: e.tensor_tensor_scan(out=pref[:, u, 1:65], data0=wd[:, u, :], data1=zer[:], initial=1.0, op0=ALU.mult, op1=ALU.add),
                     reads=[wd.r, zer.r], writes=[pref.r])
            yield
            recip(cx, rp[:], pref[:], [pref.r], [rp.r])
            cp(cx, tot[:], pref[:, :, 64], [pref.r], [tot.r])
            cp(cx, rtot[:], rp[:, :, 64], [rp.r], [rtot.r])
            cp(cx, GIN[:, 0:4], pref[:, 0:4, 1:65], [pref.r], [GIN.r], eng="pool")
            cp(cx, GEX[:, 0:4], pref[:, 0:4, 0:64], [pref.r], [GEX.r], eng="pool")
            cp(cx, GINV[:, 0:4], rp[:, 0:4, 1:65], [rp.r], [GINV.r], eng="pool")
            for u in range(4, 8):
                ts(cx, GIN[:, u, :], rp[:, u, 0:64], tot[:, u:u + 1], None, ALU.mult, None, [rp.r, tot.r], [GIN.r], eng="pool")
                ts(cx, GEX[:, u, :], rp[:, u, 1:65], tot[:, u:u + 1], None, ALU.mult, None, [rp.r, tot.r], [GEX.r], eng="pool")
                ts(cx, GINV[:, u, :], pref[:, u, 0:64], rtot[:, u:u + 1], None, ALU.mult, None, [pref.r, rtot.r], [GINV.r], eng="pool")
            yield
            for d in range(2):
                sl = slice(d * 4, (d + 1) * 4)
                stt(cx, AR_[:, sl, 0, :], kkn[:], -1.0, GEX[:, sl], ALU.mult, ALU.mult, [kkn.r, GEX.r], [AR_.r])
                tt(cx, AR_[:, sl, 1, :], rs, GIN[:, sl], ALU.mult, [sh.r, GIN.r], [AR_.r])
                tt(cx, t8[:, sl], kkn[:], aa[:, sl], ALU.mult, [kkn.r, aa.r], [t8.r])
                cp(cx, AV_[:, sl, 1, :], vs, [sh.r], [AV_.r], eng="pool")
            yield
            tt(cx, BK_[:, :, 0, :], t8[:], GINV[:], ALU.mult, [t8.r, GINV.r], [BK_.r])
            tt(cx, BK_[:, :, 1, :], kd[:], GINV[:], ALU.mult, [kd.r, GINV.r], [BK_.r])
            cp(cx, AV_[:, :, 0, :], AR_[:, :, 0, :], [AR_.r], [AV_.r], eng="pool")
            for u in range(8):
                ts(cx, BKg_[:, u], BK_[:, u], tot[:, u:u + 1], None, ALU.mult, None, [BK_.r, tot.r], [BKg_.r], eng="pool")
                ts(cx, dg_[u][:], idn[0:64, 0:64], tot[:, u:u + 1], None, ALU.mult, None, [idn.r, tot.r], [dg_[u].r], eng="pool")
            yield
            if lat:
                R_ = rst[gc % 2]
                tt(cx, t4[:], kd[:, 0:4], kd[:, 4:8], ALU.add, [kd.r], [t4.r])
                tt(cx, t4[:], t4[:], rs, ALU.mult, [t4.r, sh.r], [t4.r])
                for h in range(4):
                    ts(cx, t4[:, h, :], t4[:, h, :], rv[:, RV_RK + h:RV_RK + h + 1], None, ALU.mult, None, [t4.r, rv.r], [t4.r])
                mm(cx, pa[0:64, 256:512], ones64[0:64, 0:64], t4[:].rearrange("p a b -> p (a b)"), True, True, [ones64.r, t4.r], [pa.r])
                tt(cx, R_[:, 0].rearrange("p a b -> p (a b)"), pa[0:64, 256:512], sh[:, 8:12].rearrange("p a b -> p (a b)"), ALU.mult, [pa.r, sh.r], [R_.r])
                act(cx, sg[:], sh[:, 14:17], AF.Exp, [sh.r], [sg.r], scale=-1.0)
                ts(cx, sg[:], sg[:], 1.0, None, ALU.add, None, [sg.r], [sg.r])
                recip(cx, sg[:], sg[:], [sg.r], [sg.r])
                pg = pp[7]
                for h in range(4):
                    for j, kj in enumerate((64, 64, 32)):
                        mm(cx, pg[0:64, h * 64:(h + 1) * 64], gup[0:kj, j, h * 64:(h + 1) * 64], sg[0:kj, j, :], j == 0, j == 2, [gup.r, sg.r], [pg.r])
                cp(cx, R_[:, 1].rearrange("p a b -> p (a b)"), pg[0:64, 0:256], [pg.r], [R_.r])
                P.dma("sp", rest_v[ci].rearrange("p (a n) -> p a n", a=2), R_[:].rearrange("p a h t -> p a (h t)"), reads=[R_.r], writes=[S["rest"].r])

            yield

        def units(gc, pg_):
            par = gc % 2
            BK_, AR_, AV_, BKg_, dg_ = BKs[par], ARs[par], AVs[par], BKgs[par], dgs[par]
            O = Ou[gc % 2]

            def tick():
                if pg_ is not None:
                    next(pg_, None)

            waves = (range(0, 6), range(6, 8))
            for wave in waves:
                tick()
                for u in wave:
                    pu = pp[u % 6]
                    mm(cx, pu[:, 0:128], BK_[:, u].rearrange("p a b -> p (a b)"), AR_[:, u].rearrange("p a b -> p (a b)"), True, True, [BK_.r, AR_.r], [pu.r])
                    mm(cx, pu[0:64, 128:192], AR_[:, u, 0, :], BK_[:, u, 0, :], True, True, [BK_.r, AR_.r], [pu.r])
                    P.op("pe", lambda e: e.transpose(pu[:, 192:256], AV_[:, u].rearrange("p a b -> p (a b)"), idn[0:64, 0:64]), reads=[AV_.r, idn.r], writes=[pu.r])
                    P.op("pe", lambda e: e.transpose(pu[:, 256:320], BKg_[:, u].rearrange("p a b -> p (a b)"), idn[0:64, 0:64]), reads=[BKg_.r, idn.r], writes=[pu.r])
                tick()
                for u in wave:
                    d = u // 4
                    pu = pp[u % 6]
                    tt(cx, M1m[u][:], pu[:, 0:128], m1mask[:, d, :], ALU.mult, [pu.r, m1mask.r], [M1m[u].r])
                    tt(cx, PQ[u][:, 0, 0, :], pu[0:64, 128:192], p0mask[:, d, :], ALU.mult, [pu.r, p0mask.r], [PQ[u].r])
                    cp(cx, Z[u][0:64, 0:64], pu[0:64, 192:256], [pu.r], [Z[u].r])
                    cp(cx, Z[u][64:128, 64:128], pu[64:128, 192:256], [pu.r], [Z[u].r])
                    cp(cx, T2s[u][:], pu[:, 256:320], [pu.r], [T2s[u].r])
                    cp(cx, PQ[u][:, 0, 1, :], M1m[u][0:64, 0:64], [M1m[u].r], [PQ[u].r], eng="pool")
            for wave in waves:
                tick()
                for u in wave:
                    pu = pp[u % 6]
                    mm(cx, pu[0:64, 320:384], M1m[u][64:128, 0:64], Z[u][64:128, 64:128], True, True, [M1m[u].r, Z[u].r], [pu.r])
                for u in wave:
                    pu = pp[u % 6]
                    cp(cx, Z[u][0:64, 64:128], pu[0:64, 320:384], [pu.r], [Z[u].r])
            for j in range(6):
                b0, b1 = j % 2, (j + 1) % 2
                for wave in waves:
                    tick()
                    for u in wave:
                        pu = pp[u % 6]
                        mm(cx, pu[0:64, 384:512], PQ[u][:, b0, 1, :], Z[u][0:64, :], True, True, [PQ[u].r, Z[u].r], [pu.r])
                        if j < 5:
                            mm(cx, pu[0:64, 128:192], PQ[u][:, b0, 1, :], PQ[u][:, b0, 0, :], True, True, [PQ[u].r], [pu.r])
                            mm(cx, pu[0:64, 192:256], PQ[u][:, b0, 0, :], PQ[u][:, b0, 1, :], True, True, [PQ[u].r], [pu.r])
                    for u in wave:
                        pu = pp[u % 6]
                        tt(cx, Z[u][0:64, :], Z[u][0:64, :], pu[0:64, 384:512], ALU.add, [Z[u].r, pu.r], [Z[u].r])
                        if j < 5:
                            cp(cx, PQ[u][:, b1].rearrange("p a b -> p (a b)"), pu[0:64, 128:256], [pu.r], [PQ[u].r])
            for wave in waves:
                tick()
                for u in wave:
                    pu = pp[u % 6]
                    mm(cx, pu[0:64, 0:64], Z[u][0:64, 0:64], M1m[u][0:64, 64:128], True, True, [Z[u].r, M1m[u].r], [pu.r])
                    mm(cx, pu[0:64, 64:128], Z[u][0:64, 0:64], T2s[u][0:64, :], True, True, [Z[u].r, T2s[u].r], [pu.r])
                    mm(cx, pu[0:64, 128:192], T2s[u][:], Z[u][:, 64:128], True, True, [Z[u].r, T2s[u].r], [pu.r])
                    mm(cx, pu[0:64, 192:256], Z[u][:, 64:128], M1m[u][:, 64:128], True, True, [Z[u].r, M1m[u].r], [pu.r])
                for u in wave:
                    pu = pp[u % 6]
                    tt(cx, O[:, u, 2, :], pu[0:64, 0:64], AR_[:, u, 1, :], ALU.add, [pu.r, AR_.r], [O.r])
                    tt(cx, O[:, u, 0, :], pu[0:64, 64:128], dg_[u][:], ALU.add, [pu.r, dg_[u].r], [O.r])
                    cp(cx, O[:, u, 1, :], pu[0:64, 128:192], [pu.r], [O.r])
                    cp(cx, O[:, u, 3, :], pu[0:64, 192:256], [pu.r], [O.r])
            P.dma("sp", unit_v[gc], O[:].rearrange("p u a t -> p (u a t)"), reads=[O.r], writes=[S["unit"].r])

        chunks = list(range(NCK) if "nchunks" not in cx.flags else cx.flags["nchunks"])
        g0 = prep_gen(chunks[0])
        for _ in g0:
            pass
        for ii, gc in enumerate(chunks):
            nxt = prep_gen(chunks[ii + 1]) if ii + 1 < len(chunks) else None
            units(gc, nxt)
            if nxt is not None:
                for _ in nxt:
                    pass
    P.barrier()
    if cx.flags.get("stopA"):
        return

    with ExitStack() as st:
        ST = [sb(cx, st, "ST%d" % i, [64, 8, 64]) for i in range(3)]
        GH = [sb(cx, st, "GH%d" % i, [64, 8, 2, 64]) for i in range(3)]
        P.op("pool", lambda e: e.memset(ST[0][:], 0.0), writes=[ST[0].r])
        order_f = list(range(NCK))
        order_b = [3, 2, 1, 0] + [4 + i for i in range(127, -1, -1)]
        uv = S["unit"].t.rearrange("(g p) (u a t) -> g p u a t", p=64, u=8, a=4)
        sv = S["states"].t.rearrange("(g p) (u t) -> g p u t", p=64, u=8)
        for s_ in range(NCK):
            gf, gb = order_f[s_], order_b[s_]
            cur, nxt = ST[s_ % 3], ST[(s_ + 1) % 3]
            g_ = GH[s_ % 3]
            P.dma("sp", g_[:, 0:4], uv[gf][:, 0:4, 0:2, :], reads=[S["unit"].r], writes=[g_.r])
            P.dma("sp", g_[:, 4:8], uv[gb][:, 4:8, 0:2, :], reads=[S["unit"].r], writes=[g_.r])
            P.dma("sp", sv[gf][:, 0:4, :], cur[:, 0:4, :], reads=[cur.r], writes=[S["states"].r])
            P.dma("sp", sv[gb][:, 4:8, :], cur[:, 4:8, :], reads=[cur.r], writes=[S["states"].r])
            pb_ = pp[s_ % 2]
            for u in range(8):
                mm(cx, pb_[0:64, u * 64:(u + 1) * 64], g_[:, u, 0, :], cur[:, u, :], True, True, [g_.r, cur.r], [pb_.r])
            tt(cx, nxt[:], pb_[0:64, 0:512].rearrange("p (u t) -> p u t", u=8), g_[:, :, 1, :], ALU.add, [pb_.r, g_.r], [nxt.r])
    P.barrier()
    if cx.flags.get("stopB"):
        return

    with ExitStack() as st:
        S0 = [sb(cx, st, "S0_%d" % i, [64, 8, 64]) for i in range(2)]
        RY = [sb(cx, st, "RY%d" % i, [64, 8, 2, 64]) for i in range(2)]
        RS = [sb(cx, st, "RS%d" % i, [64, 2, 4, 64]) for i in range(2)]
        y8 = sb(cx, st, "y8", [64, 8, 64])
        y = sb(cx, st, "y", [64, 4, 64])
        ysq = sb(cx, st, "ysq", [64, 4, 64])
        mu = sb(cx, st, "mu", [64, 4, 64])
        var = sb(cx, st, "var", [64, 4, 64])
        ob = [sb(cx, st, "ob%d" % i, [64, 4, 512], BF16) for i in range(2)]
        uv = S["unit"].t.rearrange("(g p) (u a t) -> g p u a t", p=64, u=8, a=4)
        sv = S["states"].t.rearrange("(g p) (u t) -> g p u t", p=64, u=8)
        rv4 = S["rest"].t.rearrange("(g p) (a h t) -> g p a h t", p=64, a=2, h=4)
        for ci in range(cx.flags.get("cchunks", T // CH)):
            gc = 4 + ci
            s0, ry, rs_ = S0[ci % 2], RY[ci % 2], RS[ci % 2]
            P.dma("sp", s0[:], sv[gc], reads=[S["states"].r], writes=[s0.r])
            P.dma("sp", ry[:], uv[gc][:, :, 2:4, :], reads=[S["unit"].r], writes=[ry.r])
            P.dma("sp", rs_[:], rv4[ci], reads=[S["rest"].r], writes=[rs_.r])
            pc_ = pp[ci % 2]
            for u in range(8):
                mm(cx, pc_[0:64, u * 64:(u + 1) * 64], s0[:, u, :], ry[:, u, 0, :], True, True, [s0.r, ry.r], [pc_.r])
            tt(cx, y8[:], pc_[0:64, 0:512].rearrange("p (u t) -> p u t", u=8), ry[:, :, 1, :], ALU.add, [pc_.r, ry.r], [y8.r])
            tt(cx, y[:], y8[:, 0:4], y8[:, 4:8], ALU.add, [y8.r], [y.r])
            if cx.flags.get("ccut") == 1:
                continue
            tt(cx, ysq[:], y[:], y[:], ALU.mult, [y.r], [ysq.r], eng="pool")
            pm_ = pp[2 + ci % 2]
            mm(cx, pm_[0:64, 0:256], ones64[0:64, 0:64], y[:].rearrange("p a b -> p (a b)"), True, True, [ones64.r, y.r], [pm_.r])
            mm(cx, pm_[0:64, 256:512], ones64[0:64, 0:64], ysq[:].rearrange("p a b -> p (a b)"), True, True, [ones64.r, ysq.r], [pm_.r])
            if cx.flags.get("ccut") == 2:
                continue
            muf, varf = mu[:].rearrange("p a b -> p (a b)"), var[:].rearrange("p a b -> p (a b)")
            ts(cx, muf, pm_[0:64, 0:256], 1.0 / 64, None, ALU.mult, None, [pm_.r], [mu.r])
            tt(cx, ysq[:], mu[:], mu[:], ALU.mult, [mu.r], [ysq.r])
            stt(cx, varf, pm_[0:64, 256:512], 1.0 / 64, ysq[:].rearrange("p a b -> p (a b)"), ALU.mult, ALU.subtract, [pm_.r, ysq.r], [var.r])
            act(cx, varf, varf, AF.Ln, [var.r, cx.epsb.r], [var.r], bias=cx.epsb[0:64, 1:2], scale=1.0)
            act(cx, varf, varf, AF.Exp, [var.r], [var.r], scale=-0.5)
            if cx.flags.get("ccut") == 3:
                continue
            tt(cx, y[:], y[:], mu[:], ALU.subtract, [y.r, mu.r], [y.r])
            tt(cx, y[:], y[:], var[:], ALU.mult, [y.r, var.r], [y.r])
            for h in range(4):
                ts(cx, y[:, h, :], y[:, h, :], rv[:, RV_LG + h:RV_LG + h + 1], rv[:, RV_LB + h:RV_LB + h + 1], ALU.mult, ALU.add, [y.r, rv.r], [y.r])
            tt(cx, y[:], y[:], rs_[:, 0], ALU.add, [y.r, rs_.r], [y.r])
            if cx.flags.get("ccut") == 4:
                continue
            o_ = ob[(ci // 8) % 2]
            tt(cx, o_[:, :, (ci % 8) * 64:(ci % 8 + 1) * 64], y[:], rs_[:, 1], ALU.mult, [y.r, rs_.r], [o_.r])
            if cx.flags.get("ccut") == 5:
                continue
            if ci % 8 == 7:
                t0 = (ci // 8) * 512
                rin = S["rwkv_in%d" % (t0 // TQ)]
                P.dma("pool", rin.t.rearrange("(h p) t -> p h t", p=64)[:, :, t0 % TQ:t0 % TQ + 512], o_[:], reads=[o_.r], writes=[rin.r])
                if "rwkv_dbg" in S:
                    P.dma("pool", S["rwkv_dbg"].t.rearrange("(h p) t -> p h t", p=64)[:, :, t0:t0 + 512], o_[:], reads=[o_.r], writes=[S["rwkv_dbg"].r])
    P.barrier()
    if cx.flags.get("stopC"):
        return
    for j in range(4):
        rin, ra = S["rwkv_in%d" % j], S["rwkv_all%d" % j].r
        P._deps("pool", [rin.r], [ra])
        inst = nc.gpsimd.collective_compute("AllGather", ALU.bypass, replica_groups=[[0, 1, 2, 3], [4, 5, 6, 7]],
                                            ins=[rin.t], outs=[S["rwkv_all%d" % j].t])
        if ra.sem is None:
            ra.sem = nc.alloc_semaphore("d_rwkv_all%d" % j)
            P.all_dma.append(ra)
        ra.dcount += 1
        inst.then_inc(ra.sem, 1)
        P.ninst += 1
        P._post((ra.sem, ra.dcount), [rin.r], [ra])


def phase3(cx, ps_):
    nc, P, I, S = cx.nc, cx.P, cx.I, cx.S
    pp = cx.psum
    NT = NKV // 128
    ckv = sb(cx, ps_, "a_ckv", [128, 2, NKV], BF16)
    krot = sb(cx, ps_, "a_krot", [64, NKV], BF16)
    cqn = sb(cx, ps_, "a_cqn", [128, 4, TQ], BF16)
    wq = sb(cx, ps_, "a_wq", [128, 4, 8 * 256], BF16)
    wk = sb(cx, ps_, "a_wk", [128, 2, 8 * 128], BF16)
    wv = sb(cx, ps_, "a_wv", [128, 2, 8 * 128], BF16)
    rqs = [sb(cx, ps_, "a_rq%d" % i, [64, 2, 512]) for i in range(2)]
    otl = [sb(cx, ps_, "a_ot%d" % i, [128, 512], BF16) for i in range(2)]
    knT = sb(cx, ps_, "a_knT", [128, NKV], BF16)
    vh = sb(cx, ps_, "a_vh", [128, NT, 128], BF16)
    qnT = sb(cx, ps_, "a_qnT", [128, TQ], BF16)
    qrT = sb(cx, ps_, "a_qrT", [64, TQ], BF16)
    qtmp = sb(cx, ps_, "a_qtmp", [64, 2, 512])
    pT = [sb(cx, ps_, "a_pT%d" % i, [128, 512], BF16) for i in range(3)]
    rden = sb(cx, ps_, "a_rden", [128, 512])
    P.dma("sp", ckv[:], S["ckvnT"].t.rearrange("(m p) t -> p m t", p=128), reads=[S["ckvnT"].r], writes=[ckv.r])
    P.dma("sp", krot[:], S["krotT"].t[:, :], reads=[S["krotT"].r], writes=[krot.r])
    P.dma("sp", cqn[:], S["cqnT"].t.rearrange("(m p) t -> p m t", p=128), reads=[S["cqnT"].r], writes=[cqn.r])
    for k in range(4):
        P.dma("pool", wq[:, k, :], I["wq"].rearrange("(k p) n -> p k n", p=128)[:, k, :], writes=[wq.r])
    for k in range(2):
        P.dma("pool", wk[:, k, :], I["wk"].rearrange("(k p) n -> p k n", p=128)[:, k, :], writes=[wk.r])
        P.dma("pool", wv[:, k, :], I["wv"].rearrange("(k p) n -> p k n", p=128)[:, k, :], writes=[wv.r])
    ev = 0
    for h in range(8):
        for g in range((NKV + 511) // 512):
            t0 = g * 512
            n = min(512, NKV - t0)
            pc = pp[g % 2]
            for k in range(2):
                mm(cx, pc[:, 0:n], wk[:, k, h * 128:(h + 1) * 128], ckv[:, k, t0:t0 + n], k == 0, k == 1, [wk.r, ckv.r], [pc.r])
            if g % 2 == 0:
                cp(cx, knT[:, t0:t0 + n], pc[:, 0:n], [pc.r], [knT.r])
            else:
                act(cx, knT[:, t0:t0 + n], pc[:, 0:n], AF.Copy, [pc.r], [knT.r])
        for g in range((NT + 3) // 4):
            nt = min(4, NT - g * 4)
            pc = pp[2 + g % 2]
            for j in range(nt):
                t0 = (g * 4 + j) * 128
                for k in range(2):
                    mm(cx, pc[:, j * 128:(j + 1) * 128], ckv[:, k, t0:t0 + 128], wv[:, k, h * 128:(h + 1) * 128], k == 0, k == 1,
                       [wv.r, ckv.r], [pc.r])
            dst = vh[:, g * 4:g * 4 + nt, :].rearrange("p a b -> p (a b)")
            if g % 2 == 0:
                act(cx, dst, pc[:, 0:nt * 128], AF.Copy, [pc.r], [vh.r])
            else:
                cp(cx, dst, pc[:, 0:nt * 128], [pc.r], [vh.r])
        for g in range(4):
            t0 = g * 512
            pc = pp[g % 2]
            for k in range(4):
                mm(cx, pc[:, :], wq[:, k, h * 256:h * 256 + 128], cqn[:, k, t0:t0 + 512], k == 0, k == 3, [wq.r, cqn.r], [pc.r])
            act(cx, qnT[:, t0:t0 + 512], pc[:, :], AF.Copy, [pc.r], [qnT.r], scale=MLA_SCALE)
            pa, pb = pp[4], pp[5]
            for m, pdst in ((0, pa), (1, pb)):
                c0 = h * 256 + 128 + m * 64
                for k in range(4):
                    mm(cx, pdst[0:64, :], wq[:, k, c0:c0 + 64], cqn[:, k, t0:t0 + 512], k == 0, k == 3, [wq.r, cqn.r], [pdst.r])
            rq = rqs[g % 2]
            P.dma("sp", rq[:], I["ropeq"][:, :, t0:t0 + 512], writes=[rq.r])
            tt(cx, qtmp[:, 0, :], pa[0:64, :], rq[:, 0, :], ALU.mult, [pa.r, rq.r], [qtmp.r])
            tt(cx, qtmp[:, 1, :], pb[0:64, :], rq[:, 1, :], ALU.mult, [pb.r, rq.r], [qtmp.r])
            tt(cx, qtmp[:, 0, :], qtmp[:, 0, :], qtmp[:, 1, :], ALU.add, [qtmp.r], [qtmp.r])
            ts(cx, qrT[:, t0:t0 + 512], qtmp[:, 0, :], MLA_SCALE, None, ALU.mult, None, [qtmp.r], [qrT.r])
        for g in range(4):
            t0 = g * 512
            po, pd = pp[6], pp[7]
            def qk(j):
                k0 = j * 128
                pc = pp[j % 4]
                mm(cx, pc[:, :], knT[:, k0:k0 + 128], qnT[:, t0:t0 + 512], True, False, [knT.r, qnT.r], [pc.r])
                mm(cx, pc[:, :], krot[:, k0:k0 + 128], qrT[:, t0:t0 + 512], False, True, [krot.r, qrT.r], [pc.r])

            qk(0)
            qk(1)
            for j in range(NT):
                if j + 2 < NT:
                    qk(j + 2)
                pc = pp[j % 4]
                pt = pT[j % 3]
                act(cx, pt[:], pc[:, :], AF.Exp, [pc.r], [pt.r])
                mm(cx, po[:, :], vh[:, j, :], pt[:], j == 0, j == NT - 1, [vh.r, pt.r], [po.r], sig=True)
                mm(cx, pd[:, :], cx.ones_bf[:], pt[:], j == 0, j == NT - 1, [cx.ones_bf.r, pt.r], [pd.r], sig=True)
            recip(cx, rden[:], pd[:, :], [pd.r], [rden.r])
            ot_ = otl[g % 2]
            tt(cx, ot_[:], po[:, :], rden[:], ALU.mult, [po.r, rden.r], [ot_.r])
            P.dma("sp", S["catA"].t[h * 128:(h + 1) * 128, t0:t0 + 512], ot_[:], reads=[ot_.r], writes=[S["catA"].r])


def phase4(cx, ps_):
    nc, P, I, S = cx.nc, cx.P, cx.I, cx.S
    pp = cx.psum
    dv, pv = cx.dv, cx.pvec
    x2T = S["x2T"]
    x2v = x2T.t.rearrange("(m p) t -> p m t", p=128)
    xov = I["xoT"].rearrange("(m p) t -> p m t", p=128)
    x2n = sb(cx, ps_, "x2n", [128, 16, TQ], BF16)
    gateT = sb(cx, ps_, "gateT", [32, TQ])
    with ExitStack() as st:
        rstd2 = sb(cx, st, "rstd2", [128, TQ])
        cx.catT = sb(cx, st, "catT", [128, 16, TQ], BF16)
        P.dma("sp", cx.catT[:, 0:8, :], S["catA"].t.rearrange("(m p) t -> p m t", p=128), reads=[S["catA"].r], writes=[cx.catT.r])
        if cx.have_rwkv:
            selq = sb(cx, st, "selq", [128, 4])
            P.dma("sp", selq[:], I["selq"][:, :], writes=[selq.r])
            stg_ = sb(cx, st, "rstage", [128, 4, TQ], BF16)
            for mh in range(2):
                dst = cx.catT[:, 8 + mh * 4:12 + mh * 4, :]
                for j in range(4):
                    raj = S["rwkv_all%d" % j]
                    P.dma("sp", stg_[:], raj.t.rearrange("(m p) t -> p m t", p=128)[:, mh * 4:(mh + 1) * 4, :], reads=[raj.r], writes=[stg_.r])
                    if j == 0:
                        ts(cx, dst, stg_[:], selq[:, 0:1], None, ALU.mult, None, [stg_.r, selq.r], [cx.catT.r])
                    else:
                        stt(cx, dst, stg_[:], selq[:, j:j + 1], dst, ALU.mult, ALU.add, [stg_.r, selq.r, cx.catT.r], [cx.catT.r])
        else:
            P.op("pool", lambda e: e.memset(cx.catT[:, 8:16, :], 0.0), writes=[cx.catT.r])
        wo = [sb(cx, st, "wo%d" % i, [128, 16, 128], BF16) for i in range(2)]
        xt = [sb(cx, st, "xt%d" % i, [128, 512]) for i in range(3)]
        x2t = [sb(cx, st, "x2t%d" % i, [128, 512]) for i in range(3)]
        sqt = [sb(cx, st, "sqt%d" % i, [128, 512], BF16) for i in range(2)]
        wsrc = I["w_out"].rearrange("(k p) n -> p k n", p=128)
        it = 0
        for m in range(16):
            w = wo[m % 2]
            for kh in range(4):
                P.dma("pool", w[:, kh * 4:(kh + 1) * 4, :], wsrc[:, kh * 4:(kh + 1) * 4, m * 128:(m + 1) * 128], writes=[w.r])
            for g in range(4):
                t0 = g * 512
                pc = pp[it % 2]
                for k in range(16):
                    mm(cx, pc[:, :], w[:, k, :], cx.catT[:, k, t0:t0 + 512], k == 0, k == 15, [w.r, cx.catT.r], [pc.r])
                x_ = xt[it % 3]
                P.dma("sp", x_[:], xov[:, m, t0:t0 + 512], writes=[x_.r])
                o_ = x2t[it % 3]
                stt(cx, o_[:], pc[:, :], dv[:, 4, m:m + 1], x_[:], ALU.mult, ALU.add, [pc.r, dv.r, x_.r], [o_.r])
                P.dma("sp", x2v[:, m, t0:t0 + 512], o_[:], reads=[o_.r], writes=[x2T.r])
                q_ = sqt[it % 2]
                act(cx, q_[:], o_[:], AF.Square, [o_.r], [q_.r])
                mm(cx, pp[4 + g][:, :], cx.ones_bf[:], q_[:], m == 0, m == 15, [cx.ones_bf.r, q_.r], [pp[4 + g].r], sig=True)
                it += 1
        for g in range(4):
            act(cx, rstd2[:, g * 512:(g + 1) * 512], pp[4 + g][:, :], AF.Ln, [pp[4 + g].r, cx.epsb.r], [rstd2.r], scale=1.0 / D, bias=cx.epsb[:, 0:1])
        act(cx, rstd2[:], rstd2[:], AF.Exp, [rstd2.r], [rstd2.r], scale=-0.5)
        wr = sb(cx, st, "wr", [128, 16, 36])
        P.dma("sp", wr[:], I["wr"].rearrange("(k p) n -> p k n", p=128), writes=[wr.r])
        brt = sb(cx, st, "brt", [36, 1])
        P.dma("sp", brt[:], I["br"][:, :], writes=[brt.r])
        xnf = [sb(cx, st, "xnf%d" % i, [128, 512]) for i in range(2)]
        it = 0
        for m in range(16):
            for g in range(4):
                t0 = g * 512
                x_ = xt[it % 3]
                P.dma("sp", x_[:], x2v[:, m, t0:t0 + 512], reads=[x2T.r], writes=[x_.r])
                f_ = xnf[it % 2]
                stt(cx, f_[:], x_[:], dv[:, 5, m:m + 1], rstd2[:, t0:t0 + 512], ALU.mult, ALU.mult, [x_.r, dv.r, rstd2.r], [f_.r])
                ts(cx, f_[:], f_[:], dv[:, 6, m:m + 1], None, ALU.add, None, [f_.r, dv.r], [f_.r])
                act(cx, x2n[:, m, t0:t0 + 512], f_[:], AF.Copy, [f_.r], [x2n.r])
                mm(cx, pp[4 + g][0:36, :], wr[:, m, :], f_[:], m == 0, m == 15, [wr.r, f_.r], [pp[4 + g].r], sig=True)
                it += 1
        lgT = sb(cx, st, "lgT", [36, TQ])
        for g in range(4):
            ts(cx, lgT[:, g * 512:(g + 1) * 512], pp[4 + g][0:36, :], brt[:, 0:1], None, ALU.add, None, [pp[4 + g].r, brt.r], [lgT.r])
        L = sb(cx, st, "L", [128, 36])
        sc = sb(cx, st, "rsc", [128, 16])
        ohg = sb(cx, st, "ohg", [128, 4])
        eg = sb(cx, st, "eg", [128, 4])
        wi = sb(cx, st, "wi", [128, 8])
        oh1 = sb(cx, st, "oh1", [128, 8])
        oh2 = sb(cx, st, "oh2", [128, 8])
        msk = sb(cx, st, "msk", [128, 8])
        gw = sb(cx, st, "gw", [128, 8])
        g32 = sb(cx, st, "g32", [128, 32])
        for tt_ in range(16):
            pl = pp[tt_ % 2]
            P.op("pe", lambda e: e.transpose(pl[:, 0:36], lgT[:, tt_ * 128:(tt_ + 1) * 128], cx.ident[0:36, 0:36]), reads=[lgT.r, cx.ident.r], writes=[pl.r])
            cp(cx, L[:], pl[:, 0:36], [pl.r], [L.r])
            R_, W_ = [L.r, sc.r, ohg.r, eg.r, wi.r, oh1.r, oh2.r, msk.r, gw.r], None
            P.op("dve", lambda e: e.tensor_reduce(out=sc[:, 0:1], in_=L[:, 0:4], axis=mybir.AxisListType.X, op=ALU.max), reads=[L.r], writes=[sc.r])
            ts(cx, ohg[:], L[:, 0:4], sc[:, 0:1], None, ALU.is_ge, None, [L.r, sc.r], [ohg.r])
            ts(cx, sc[:, 1:2], sc[:, 0:1], -1.0, None, ALU.mult, None, [sc.r], [sc.r])
            act(cx, eg[:], L[:, 0:4], AF.Exp, [L.r, sc.r], [eg.r], bias=sc[:, 1:2], scale=1.0)
            P.op("dve", lambda e: e.tensor_reduce(out=sc[:, 2:3], in_=eg[:], axis=mybir.AxisListType.X, op=ALU.add), reads=[eg.r], writes=[sc.r])
            recip(cx, sc[:, 3:4], sc[:, 2:3], [sc.r], [sc.r])
            ts(cx, wi[:], L[:, 4:12], ohg[:, 0:1], None, ALU.mult, None, [L.r, ohg.r], [wi.r])
            for gi in range(1, 4):
                stt(cx, wi[:], L[:, 4 + gi * 8:12 + gi * 8], ohg[:, gi:gi + 1], wi[:], ALU.mult, ALU.add, [L.r, ohg.r, wi.r], [wi.r])
            P.op("dve", lambda e: e.tensor_reduce(out=sc[:, 4:5], in_=wi[:], axis=mybir.AxisListType.X, op=ALU.max), reads=[wi.r], writes=[sc.r])
            ts(cx, oh1[:], wi[:], sc[:, 4:5], None, ALU.is_ge, None, [wi.r, sc.r], [oh1.r])
            stt(cx, msk[:], oh1[:], -1e30, wi[:], ALU.mult, ALU.add, [oh1.r, wi.r], [msk.r])
            P.op("dve", lambda e: e.tensor_reduce(out=sc[:, 5:6], in_=msk[:], axis=mybir.AxisListType.X, op=ALU.max), reads=[msk.r], writes=[sc.r])
            ts(cx, oh2[:], msk[:], sc[:, 5:6], None, ALU.is_ge, None, [msk.r, sc.r], [oh2.r])
            tt(cx, sc[:, 6:7], sc[:, 5:6], sc[:, 4:5], ALU.subtract, [sc.r], [sc.r])
            act(cx, sc[:, 7:8], sc[:, 6:7], AF.Exp, [sc.r], [sc.r])
            ts(cx, sc[:, 8:9], sc[:, 7:8], 1.0, None, ALU.add, None, [sc.r], [sc.r])
            recip(cx, sc[:, 9:10], sc[:, 8:9], [sc.r], [sc.r])
            tt(cx, sc[:, 10:11], sc[:, 9:10], sc[:, 3:4], ALU.mult, [sc.r], [sc.r])
            tt(cx, sc[:, 11:12], sc[:, 10:11], sc[:, 7:8], ALU.mult, [sc.r], [sc.r])
            ts(cx, gw[:], oh1[:], sc[:, 10:11], None, ALU.mult, None, [oh1.r, sc.r], [gw.r])
            stt(cx, gw[:], oh2[:], sc[:, 11:12], gw[:], ALU.mult, ALU.add, [oh2.r, sc.r, gw.r], [gw.r])
            for gi in range(4):
                ts(cx, g32[:, gi * 8:(gi + 1) * 8], gw[:], ohg[:, gi:gi + 1], None, ALU.mult, None, [gw.r, ohg.r], [g32.r])
            pg = pp[2 + tt_ % 2]
            P.op("pe", lambda e: e.transpose(pg[0:32, 0:128], g32[:], cx.ident[:]), reads=[g32.r, cx.ident.r], writes=[pg.r])
            cp(cx, gateT[:, tt_ * 128:(tt_ + 1) * 128], pg[0:32, 0:128], [pg.r], [gateT.r])
    if "gateT" in cx.debug:
        P.dma("sp", S["gateT"].t[:, :], gateT[:], reads=[gateT.r], writes=[S["gateT"].r])
    P.barrier()
    with ExitStack() as st:
        HT = TQ // 2
        yacc = sb(cx, st, "yacc", [128, 16, HT])
        w1e = sb(cx, st, "w1e", [128, 16, 512], BF16)
        w3e = sb(cx, st, "w3e", [128, 16, 512], BF16)
        w2e = sb(cx, st, "w2e", [128, 4, D // 2], BF16)
        selr = [sb(cx, st, "selr%d" % i, [32, 128]) for i in range(2)]
        wstg = [sb(cx, st, "wstg%d" % i, [128, 512]) for i in range(3)]
        wsi = 0
        actg = sb(cx, st, "actg", [128, 4, HT], BF16)
        e1 = [sb(cx, st, "e1_%d" % i, [128, 512]) for i in range(2)]
        xt = [sb(cx, st, "mxt%d" % i, [128, 512]) for i in range(2)]
        sqt = [sb(cx, st, "msq%d" % i, [128, 512], BF16) for i in range(1)] * 2
        rstdf = sb(cx, st, "rstdf", [128, HT])
        for hf in range(2):
            h0 = hf * HT
            P.op("pool", lambda e: e.memset(yacc[:], 0.0), writes=[yacc.r])
            for ex in range(NEXP):
                w1s = I["w1"][ex].rearrange("(k p) n -> p k n", p=128)
                w3s = I["w3"][ex].rearrange("(k p) n -> p k n", p=128)
                w2s = I["w2"][ex].rearrange("(j p) n -> p j n", p=128)
                for kh in range(4):
                    P.dma("pool", w1e[:, kh * 4:(kh + 1) * 4, :], w1s[:, kh * 4:(kh + 1) * 4, :], writes=[w1e.r])
                for kh in range(16):
                    sg_ = wstg[wsi % len(wstg)]
                    P.dma("sp", sg_[:], w3s[:, kh, :], writes=[sg_.r])
                    act(cx, w3e[:, kh, :], sg_[:], AF.Copy, [sg_.r], [w3e.r])
                    wsi += 1
                it = 0
                sel = selr[ex % 2]
                P.op("pool", lambda e: e.memset(sel[:], 1.0), writes=[sel.r])
                P.op("pool", lambda e: e.affine_select(out=sel[:], in_=sel[:], pattern=[[0, 128]], compare_op=ALU.is_equal, fill=0.0,
                                                       base=-ex, channel_multiplier=1), reads=[sel.r], writes=[sel.r])
                for tg in range(HT // 512):
                    t0 = h0 + tg * 512
                    pgb = pp[6 + tg % 2]
                    mm(cx, pgb[:, :], sel[:], gateT[:, t0:t0 + 512], True, True, [sel.r, gateT.r], [pgb.r])
                    for j in range(4):
                        p1, p3 = pp[(it % 2) * 2], pp[(it % 2) * 2 + 1]
                        for k in range(16):
                            mm(cx, p1[:, :], w1e[:, k, j * 128:(j + 1) * 128], x2n[:, k, t0:t0 + 512], k == 0, k == 15, [w1e.r, x2n.r], [p1.r])
                        for k in range(16):
                            mm(cx, p3[:, :], w3e[:, k, j * 128:(j + 1) * 128], x2n[:, k, t0:t0 + 512], k == 0, k == 15, [w3e.r, x2n.r], [p3.r])
                        e_ = e1[it % 2]
                        act(cx, e_[:], p1[:, :], AF.Silu, [p1.r], [e_.r])
                        tt(cx, e_[:], e_[:], p3[:, :], ALU.mult, [e_.r, p3.r], [e_.r])
                        tt(cx, actg[:, j, tg * 512:(tg + 1) * 512], e_[:], pgb[:, :], ALU.mult, [e_.r, pgb.r], [actg.r])
                        it += 1
                it = 0
                for m in range(16):
                    if m % 8 == 0:
                        for j in range(4):
                            P.dma("pool", w2e[:, j, :], w2s[:, j, (m // 8) * 1024:(m // 8 + 1) * 1024], writes=[w2e.r])
                    for tg in range(HT // 512):
                        py = pp[(4, 5, 2, 3)[it % 4]]
                        for j in range(4):
                            mm(cx, py[:, :], w2e[:, j, (m % 8) * 128:(m % 8 + 1) * 128], actg[:, j, tg * 512:(tg + 1) * 512], j == 0, j == 3, [w2e.r, actg.r], [py.r])
                        tt(cx, yacc[:, m, tg * 512:(tg + 1) * 512], yacc[:, m, tg * 512:(tg + 1) * 512], py[:, :], ALU.add, [yacc.r, py.r], [yacc.r])
                        it += 1
            it = 0
            for m in range(16):
                for tg in range(HT // 512):
                    t0 = h0 + tg * 512
                    x_ = xt[it % 2]
                    P.dma("sp", x_[:], x2v[:, m, t0:t0 + 512], reads=[x2T.r], writes=[x_.r])
                    o_ = e1[it % 2]
                    stt(cx, o_[:], yacc[:, m, tg * 512:(tg + 1) * 512], dv[:, 7, m:m + 1], x_[:], ALU.mult, ALU.add, [yacc.r, dv.r, x_.r], [o_.r])
                    P.dma("sp", x2v[:, m, t0:t0 + 512], o_[:], reads=[o_.r], writes=[x2T.r])
                    q_ = sqt[it % 2]
                    act(cx, q_[:], o_[:], AF.Square, [o_.r], [q_.r])
                    mm(cx, pp[6 + tg][:, :], cx.ones_bf[:], q_[:], m == 0, m == 15, [cx.ones_bf.r, q_.r], [pp[6 + tg].r], sig=True)
                    it += 1
            for tg in range(HT // 512):
                act(cx, rstdf[:, tg * 512:(tg + 1) * 512], pp[6 + tg][:, :], AF.Ln, [pp[6 + tg].r, cx.epsb.r], [rstdf.r], scale=1.0 / D, bias=cx.epsb[:, 0:1])
            act(cx, rstdf[:], rstdf[:], AF.Exp, [rstdf.r], [rstdf.r], scale=-0.5)
            ov = cx.outT.t.rearrange("(m p) t -> p m t", p=128)
            it = 0
            for m in range(16):
                for tg in range(HT // 512):
                    t0 = h0 + tg * 512
                    x_ = xt[it % 2]
                    P.dma("sp", x_[:], x2v[:, m, t0:t0 + 512], reads=[x2T.r], writes=[x_.r])
                    o_ = e1[it % 2]
                    stt(cx, o_[:], x_[:], pv[:, PV_GFIN + m:PV_GFIN + m + 1], rstdf[:, tg * 512:(tg + 1) * 512], ALU.mult, ALU.mult,
                        [x_.r, pv.r, rstdf.r], [o_.r])
                    P.dma("sp", ov[:, m, t0:t0 + 512], o_[:], reads=[o_.r], writes=[cx.outT.r])
                    it += 1


def build_nc(debug=(), upto=99, rwkv=True, flags=None):
    nc = bass.Bass("TRN2", target_bir_lowering=False)
    cx = Ctx()
    cx.nc = nc
    cx.P = Prog(nc)
    cx.I = {}
    cx.S = {}
    cx.debug = debug
    cx.flags = flags or {}

    def inp(name, shape, dt=F32):
        cx.I[name] = nc.dram_tensor(name, list(shape), dt, kind="ExternalInput").ap()

    inp("xT", [D, T]); inp("xoT", [D, TQ]); inp("ctxT", [D, TC]); inp("cT", [128, 16, 2]); inp("w_mod", [D, 6 * D])
    inp("pvec", [128, PV_N]); inp("w1p", [D, NP1]); inp("rope", [64, 2, T]); inp("ropeq", [64, 2, TQ])
    inp("ident", [128, 128])
    if upto >= 3:
        inp("wq", [512, 8 * 256]); inp("wk", [256, 8 * 128]); inp("wv", [256, 8 * 128])
    if upto >= 4:
        inp("w_out", [D, D]); inp("wr", [D, 36]); inp("br", [36, 1])
    inp("rvec", [64, RV_N]); inp("mux", [64, 2, NRW, 64]); inp("upw", [64, 2, 2, 256]); inp("gup", [64, 3, 256])
    inp("m1mask", [128, 2, 128]); inp("p0mask", [64, 2, 64]); inp("selq", [128, 4])
    if upto >= 4:
        inp("w1", [NEXP, D, DEXP]); inp("w3", [NEXP, D, DEXP]); inp("w2", [NEXP, DEXP, D])

    def scr(name, shape, dt):
        kind = "ExternalOutput" if name in debug else "Internal"
        cx.S[name] = dram(cx, name, shape, dt, kind=kind)

    scr("rawp", [NRW * 64, T + 2], F32)
    scr("rawc", [NRW * 64, TC + 2], F32)
    scr("ckvnT", [256, NKV], BF16)
    scr("krotT", [64, NKV], BF16)
    scr("cqnT", [512, TQ], BF16)
    scr("x2T", [D, TQ], F32)
    scr("catA", [1024, TQ], BF16)
    scr("unit", [NCK * 64, 8 * 4 * 64], F32)
    scr("states", [NCK * 64, 8 * 64], F32)
    scr("rest", [(T // CH) * 64, 2 * 4 * 64], F32)
    for j in range(4):
        cx.S["rwkv_in%d" % j] = dram(cx, "rwkv_in%d" % j, [256, TQ], BF16, kind="Internal")
        cx.S["rwkv_all%d" % j] = dram(cx, "rwkv_all%d" % j, [1024, TQ], BF16, kind="Internal", addr_space="Local")
    if "rwkv_dbg" in debug:
        scr("rwkv_dbg", [256, T], BF16)
    scr("gateT", [32, TQ], F32)
    cx.outT = dram(cx, "outT", [D, TQ], F32, kind="ExternalOutput")

    with ExitStack() as gstack:
        cx.gstack = gstack
        cx.psum = []
        for i in range(8):
            t = gstack.enter_context(nc.psum_tensor("ps%d" % i, [128, 512], F32))
            cx.psum.append(Tl(cx.P, t, "ps%d" % i))
        cx.q0 = None
        phase0(cx)
        if "modv" in debug:
            cx.S["modv"] = dram(cx, "modv", [128, 192], F32, kind="ExternalOutput")
            cx.P.dma("sp", cx.S["modv"].t[:, :], cx.modv[:].rearrange("p j c -> p (j c)"), reads=[cx.modv.r], writes=[cx.S["modv"].r])
        cx.P.barrier()
        if upto >= 1 and not cx.flags.get("skip1"):
            phase1(cx)
            cx.P.barrier()
        cx.have_rwkv = False
        if upto >= 2 and rwkv:
            with ExitStack() as st2:
                phase2(cx, st2)
            cx.P.barrier()
            cx.have_rwkv = True
        if upto >= 3:
            with ExitStack() as st3:
                phase3(cx, st3)
            cx.P.barrier()
            if upto >= 4:
                with ExitStack() as st4:
                    phase4(cx, st4)
                cx.P.barrier()
        finals = [cx.outT] + [cx.S[n] for n in debug if n in cx.S]
        cx.P.wait_all("sp", [f.r for f in finals])
    cx.nc_ninst = cx.P.ninst
    return nc, cx


def rope_tables():
    rows = T // GRID_W
    row, col = np.meshgrid(np.arange(rows), np.arange(GRID_W), indexing="ij")
    inv_freq = (10000.0 ** (-np.arange(0, 32, 2, dtype=np.float32) / 32)).astype(np.float32)
    ang_r = row.reshape(-1)[:, None].astype(np.float32) * inv_freq
    ang_c = col.reshape(-1)[:, None].astype(np.float32) * inv_freq
    Ct = np.concatenate([np.cos(ang_r), np.cos(ang_r), np.cos(ang_c), np.cos(ang_c)], axis=1).T
    St = np.concatenate([-np.sin(ang_r), np.sin(ang_r), -np.sin(ang_c), np.sin(ang_c)], axis=1).T
    return np.ascontiguousarray(np.stack([Ct, St], axis=1).astype(np.float32))


ROPE_PERM = np.concatenate([np.arange(16, 32), np.arange(0, 16), np.arange(48, 64), np.arange(32, 48)])


def fm(v, p=128):
    return np.ascontiguousarray(v.reshape(-1, p).T)


def prep_core(inp, c, shared):
    b, q = c // 4, c % 4
    m = {}
    m["xT"] = shared["xT"][b]
    m["xoT"] = np.ascontiguousarray(shared["xT"][b][:, q * TQ:(q + 1) * TQ])
    m["ctxT"] = shared["ctxT"][b]
    m["cT"] = np.ascontiguousarray(np.stack([fm(inp["c"][b]), fm(inp["c_ctx"])], axis=-1))
    m["w_mod"] = shared["w_mod"]
    m["pvec"] = shared["pvec"]
    w_in = inp["w_in"][0]
    cols = list(range(0, 832)) + list(768 + ROPE_PERM)
    for base in (832, 1856, 2880):
        for i in range(4):
            h = 4 * q + i
            cols += list(range(base + h * 64, base + (h + 1) * 64))
    cols += list(range(3904, 4192))
    m["w1p"] = np.ascontiguousarray(w_in[:, cols])
    assert m["w1p"].shape[1] == NP1
    m["rope"] = shared["rope"]
    m["ropeq"] = np.ascontiguousarray(shared["rope"][:, :, q * TQ:(q + 1) * TQ])
    m["ident"] = shared["ident"]
    for k in ("wq", "wk", "wv", "w_out", "wr", "br", "w1", "w2", "w3", "m1mask", "p0mask"):
        m[k] = shared[k]
    hs = [4 * q + i for i in range(4)]
    rvv = np.zeros((64, RV_N), np.float32)
    for i, h in enumerate(hs):
        cs = slice(h * 64, (h + 1) * 64)
        for d in range(2):
            rvv[:, RV_W0 + d * 4 + i] = inp["decay_w0"][0][d][cs]
            rvv[:, RV_A0 + d * 4 + i] = inp["iclr_a0"][0][d][cs]
        rvv[:, RV_KK + i] = inp["key_k"][0][cs]
        rvv[:, RV_KA + i] = inp["key_a"][0][cs]
        rvv[:, RV_RK + i] = inp["bonus_r_k"][0][h]
        rvv[:, RV_LG + i] = inp["lnx_g"][0][cs]
        rvv[:, RV_LB + i] = inp["lnx_b"][0][cs]
    m["rvec"] = rvv
    smu = inp["shift_mu"][0]
    mux = np.zeros((64, 2, NRW, 64), np.float32)
    mi = 0
    for base in (0, 1024, 2048):
        for h in hs:
            for a in range(2):
                mux[:, a, mi, :] = smu[a][base + h * 64: base + (h + 1) * 64][:, None]
            mi += 1
    for c0, n in ((3072, 64), (3136, 64), (3200, 64), (3264, 64), (3328, 32)):
        for a in range(2):
            mux[:n, a, mi, :] = smu[a][c0:c0 + n][:, None]
        mi += 1
    m["mux"] = mux
    cols = slice(4 * q * 64, 4 * q * 64 + 256)
    upw = np.zeros((64, 2, 2, 256), np.float32)
    for d in range(2):
        upw[:, 0, d, :] = inp["decay_up"][0][d][:, cols]
        upw[:, 1, d, :] = inp["iclr_up"][0][d][:, cols]
    m["upw"] = upw
    gup = np.zeros((64, 3, 256), np.float32)
    gu = inp["gate_up"][0]
    gup[:, 0, :] = gu[0:64, cols]; gup[:, 1, :] = gu[64:128, cols]; gup[:32, 2, :] = gu[128:160, cols]
    m["gup"] = gup
    sq_ = np.zeros((128, 4), np.float32); sq_[:, q] = 1.0
    m["selq"] = sq_
    return m


def prep_shared(inp):
    sh = {}
    sh["xT"] = [np.ascontiguousarray(inp["x"][b].T) for b in range(2)]
    sh["ctxT"] = [np.ascontiguousarray(inp["ctx"][b].T) for b in range(2)]
    sh["w_mod"] = np.ascontiguousarray(inp["w_mod"][0])
    pv = np.zeros((128, PV_N), np.float32)
    pv[:, PV_BMOD:PV_BMOD + 96] = fm(inp["b_mod"][0])
    pv[:, PV_GATTN:PV_GATTN + 16] = fm(inp["norm_attn_g"][0])
    pv[:, PV_GFFN:PV_GFFN + 16] = fm(inp["norm_ffn_g"][0])
    pv[:, PV_GFIN:PV_GFIN + 16] = fm(inp["final_norm_g"])
    pv[:, PV_QNG:PV_QNG + 4] = fm(inp["q_norm_g"][0])
    pv[:, PV_KVNG:PV_KVNG + 2] = fm(inp["kv_norm_g"][0])
    sh["pvec"] = pv
    sh["rope"] = rope_tables()
    sh["ident"] = np.eye(128, dtype=np.float32)
    wuq = inp["w_uq"][0].reshape(512, 8, 192)
    sh["wq"] = np.ascontiguousarray(np.concatenate([wuq, wuq[:, :, 128 + ROPE_PERM]], axis=2).reshape(512, 8 * 256))
    wukv = inp["w_ukv"][0].reshape(256, 8, 256)
    sh["wk"] = np.ascontiguousarray(wukv[:, :, :128].reshape(256, 1024))
    sh["wv"] = np.ascontiguousarray(wukv[:, :, 128:].reshape(256, 1024))
    sh["w_out"] = np.ascontiguousarray(inp["w_out"][0])
    sh["wr"] = np.ascontiguousarray(np.concatenate([inp["w_grp"][0], inp["w_exp"][0]], axis=1))
    sh["br"] = np.ascontiguousarray(np.concatenate([inp["b_grp"][0], inp["b_exp"][0]])[:, None])
    lo_s = np.tril(np.ones((64, 64), np.float32), -1)
    lo_i = np.tril(np.ones((64, 64), np.float32), 0)
    m1 = np.zeros((128, 2, 128), np.float32)
    p0 = np.zeros((64, 2, 64), np.float32)
    for d, (ts_, ti_) in enumerate(((lo_s, lo_i), (lo_s.T, lo_i.T))):
        blk = np.block([[ts_.T, ti_.T], [ts_.T, ti_.T]])
        m1[:, d, :] = blk
        p0[:, d, :] = ts_
    sh["m1mask"] = m1; sh["p0mask"] = p0
    sh["w1"] = np.ascontiguousarray(inp["w1"][0]); sh["w3"] = np.ascontiguousarray(inp["w3"][0]); sh["w2"] = np.ascontiguousarray(inp["w2"][0])
    return sh


def kernel(**inputs):
    inp = {k: np.asarray(v) for k, v in inputs.items()}
    shared = prep_shared(inp)
    nc, cx = build_nc()
    in_maps = [prep_core(inp, c, shared) for c in range(8)]
    res = run_bass_kernel_spmd(nc, in_maps, core_ids=list(range(8)))
    out = np.zeros((2, T, D), np.float32)
    for c in range(8):
        b, q = c // 4, c % 4
        out[b, q * TQ:(q + 1) * TQ, :] = res.results[c]["outT"].T
    return out
```

```python
from contextlib import ExitStack
import numpy as np
import ml_dtypes
import concourse.bass as bass
import concourse.mybir as mybir
from concourse.bass_utils import run_bass_kernel_spmd

F32 = mybir.dt.float32
BF16 = mybir.dt.bfloat16
AF = mybir.ActivationFunctionType
ALU = mybir.AluOpType

D = 2048
T = 8192
TC = 256
TQ = 2048
NKV = T + TC
GRID_W = 64
NEXP = 32
DEXP = 512
EPS = 1e-6
LNX_EPS = 64e-5
MLA_SCALE = 192.0 ** -0.5
CH = 64

PCH = []
_o = 0
for _n, _m in ([("cq%d" % i, 128) for i in range(4)] + [("ckv0", 128), ("ckv1", 128), ("kr", 64), ("krsw", 64)]
               + [("r%d" % i, 64) for i in range(4)] + [("k%d" % i, 64) for i in range(4)]
               + [("v%d" % i, 64) for i in range(4)] + [("wl", 64), ("al", 64), ("gl0", 64), ("gl1", 64), ("gl2", 32)]):
    PCH.append((_n, _m, _o))
    _o += _m
NP1 = _o
RW_NAMES = [n for n, _, _ in PCH[8:]]
NRW = len(RW_NAMES)

PV_BMOD, PV_GATTN, PV_GFFN, PV_GFIN, PV_QNG, PV_KVNG, PV_N = 0, 96, 112, 128, 144, 148, 150


class Res:
    __slots__ = ("name", "lw", "rd", "sem", "dcount")

    def __init__(self, name):
        self.name = name
        self.lw = None
        self.rd = {}
        self.sem = None
        self.dcount = 0


class Prog:
    def __init__(self, nc):
        self.nc = nc
        self.engs = {"pe": nc.tensor, "act": nc.scalar, "dve": nc.vector, "pool": nc.gpsimd, "sp": nc.sync}
        self.sem = {}
        self.cnt = {}
        self.known = {}
        for e in self.engs:
            self.sem[e] = nc.alloc_semaphore("s_" + e)
            self.cnt[e] = 0
            self.known[e] = {}
        self.semown = {id(self.sem[e]): e for e in self.engs}
        self.ninst = 0
        self.all_dma = []
        self.retired = []

    def res(self, name):
        return Res(name)

    def _deps(self, e, reads, writes):
        deps = {}

        def add(tok):
            s, v = tok
            k = id(s)
            if k not in deps or deps[k][1] < v:
                deps[k] = (s, v)

        for r in reads:
            if r.lw is not None:
                add(r.lw)
        for w in writes:
            if w.lw is not None:
                add(w.lw)
            for t in w.rd.values():
                add(t)
        eng = self.engs[e]
        for k, (s, v) in deps.items():
            if e == "pe" and self.semown.get(k) == "pe":
                continue
            if self.known[e].get(k, 0) < v:
                eng.wait_ge(s, v)
                self.known[e][k] = v
                self.ninst += 1

    def _post(self, tok, reads, writes):
        k = id(tok[0])
        for r in reads:
            if k not in r.rd or r.rd[k][1] < tok[1]:
                r.rd[k] = tok
        for w in writes:
            w.lw = tok
            w.rd = {}

    def op(self, e, fn, reads=(), writes=()):
        self._deps(e, reads, writes)
        inst = fn(self.engs[e])
        self.cnt[e] += 1
        inst.then_inc(self.sem[e], 1)
        self.ninst += 1
        self._post((self.sem[e], self.cnt[e]), reads, writes)

    def dma(self, q, out, in_, reads=(), writes=(), **kw):
        self._deps(q, reads, writes)
        w = writes[0]
        if w.sem is None:
            w.sem = self.nc.alloc_semaphore("d_" + w.name)
            self.all_dma.append(w)
        inst = self.engs[q].dma_start(out=out, in_=in_, **kw)
        w.dcount += 16
        inst.then_inc(w.sem, 16)
        self.ninst += 1
        self._post((w.sem, w.dcount), reads, writes)

    def barrier(self):
        for e in self.engs:
            eng = self.engs[e]
            for f in self.engs:
                if f == e:
                    continue
                k = id(self.sem[f])
                if self.cnt[f] > 0 and self.known[e].get(k, 0) < self.cnt[f]:
                    eng.wait_ge(self.sem[f], self.cnt[f])
                    self.known[e][k] = self.cnt[f]
                    self.ninst += 1
            for r in self.all_dma:
                k = id(r.sem)
                if self.known[e].get(k, 0) < r.dcount:
                    eng.wait_ge(r.sem, r.dcount)
                    self.known[e][k] = r.dcount
                    self.ninst += 1
        for f in self.engs:
            if self.cnt[f] > 20000:
                self.retired.append(self.sem[f])
                self.sem[f] = self.nc.alloc_semaphore("s_%s_%d" % (f, len(self.retired)))
                self.semown[id(self.sem[f])] = f
                self.cnt[f] = 0

    def wait_all(self, e, resources):
        self._deps(e, resources, [])


class Tl:
    def __init__(self, P, t, name):
        self.t = t
        self.r = P.res(name)

    def __getitem__(self, idx):
        return self.t[idx]


class Ctx:
    pass


def sb(cx, stack, name, shape, dt=F32):
    t = stack.enter_context(cx.nc.sbuf_tensor("sb_" + name, shape, dt))
    return Tl(cx.P, t, name)


def dram(cx, name, shape, dt, kind="Internal", **kw):
    t = cx.nc.dram_tensor(name, shape, dt, kind=kind, **kw).ap()
    tl = Tl(cx.P, t, name)
    return tl


def mm(cx, out, lhsT, rhs, start, stop, reads, writes):
    cx.P.op("pe", lambda e: e.matmul(out, lhsT=lhsT, rhs=rhs, start=start, stop=stop), reads=reads, writes=writes)


def act(cx, out, in_, func, reads, writes, eng="act", **kw):
    cx.P.op("act", lambda e: e.activation(out=out, in_=in_, func=func, **kw), reads=reads, writes=writes)


def tt(cx, out, in0, in1, op, reads, writes, eng="dve"):
    cx.P.op(eng, lambda e: e.tensor_tensor(out=out, in0=in0, in1=in1, op=op), reads=reads, writes=writes)


def ts(cx, out, in0, s1, s2, op0, op1, reads, writes, eng="dve"):
    if op1 is None and eng == "pool":
        op1, s2 = (ALU.mult, 1.0) if op0 == ALU.add else (ALU.add, 0.0)
    if op1 is None:
        cx.P.op(eng, lambda e: e.tensor_scalar(out=out, in0=in0, scalar1=s1, scalar2=None, op0=op0), reads=reads, writes=writes)
    else:
        cx.P.op(eng, lambda e: e.tensor_scalar(out=out, in0=in0, scalar1=s1, scalar2=s2, op0=op0, op1=op1), reads=reads, writes=writes)


def stt(cx, out, in0, scalar, in1, op0, op1, reads, writes):
    cx.P.op("dve", lambda e: e.scalar_tensor_tensor(out=out, in0=in0, scalar=scalar, in1=in1, op0=op0, op1=op1), reads=reads, writes=writes)


def cp(cx, out, in_, reads, writes, eng="dve"):
    cx.P.op(eng, lambda e: e.tensor_copy(out=out, in_=in_), reads=reads, writes=writes)


def recip(cx, out, in_, reads, writes):
    cx.P.op("dve", lambda e: e.reciprocal(out=out, in_=in_), reads=reads, writes=writes)


def rsqrt_inplace(cx, tl, ap, scale, bias):
    act(cx, ap, ap, AF.Ln, [tl.r, cx.epsb.r], [tl.r], scale=scale, bias=bias)
    act(cx, ap, ap, AF.Exp, [tl.r], [tl.r], scale=-0.5)


def phase0(cx):
    nc, P, I = cx.nc, cx.P, cx.I
    st = cx.gstack
    cx.ident = sb(cx, st, "ident", [128, 128])
    P.dma("sp", cx.ident[:], I["ident"][:, :], writes=[cx.ident.r])
    cx.ones_bf = sb(cx, st, "ones_bf", [128, 128], BF16)
    P.op("pool", lambda e: e.memset(cx.ones_bf[:], 1.0), writes=[cx.ones_bf.r])
    cx.ones_f = sb(cx, st, "ones_f", [128, 128])
    P.op("pool", lambda e: e.memset(cx.ones_f[:], 1.0), writes=[cx.ones_f.r])
    cx.epsb = sb(cx, st, "epsb", [128, 4])
    P.op("pool", lambda e: e.memset(cx.epsb[:, 0:1], EPS), writes=[cx.epsb.r])
    P.op("pool", lambda e: e.memset(cx.epsb[:, 1:2], LNX_EPS), writes=[cx.epsb.r])
    P.op("pool", lambda e: e.memset(cx.epsb[:, 2:3], 1e-12), writes=[cx.epsb.r])
    P.op("pool", lambda e: e.memset(cx.epsb[:, 3:4], 0.0), writes=[cx.epsb.r])
    cx.pvec = sb(cx, st, "pvec", [128, PV_N])
    P.dma("sp", cx.pvec[:], I["pvec"][:, :], writes=[cx.pvec.r])
    cx.modv = sb(cx, st, "modv", [128, 96, 2])
    cx.dv = sb(cx, st, "dv", [128, 8, 16])

    with ExitStack() as ps:
        cT = sb(cx, ps, "cT", [128, 16, 2])
        sg = sb(cx, ps, "sg", [128, 16, 2])
        P.dma("sp", cT[:], I["cT"][:, :, :], writes=[cT.r])
        act(cx, sg[:], cT[:], AF.Exp, [cT.r], [sg.r], scale=-1.0)
        ts(cx, sg[:], sg[:], 1.0, None, ALU.add, None, [sg.r], [sg.r])
        recip(cx, sg[:], sg[:], [sg.r], [sg.r])
        tt(cx, sg[:], sg[:], cT[:], ALU.mult, [sg.r, cT.r], [sg.r])
        wbuf = [sb(cx, ps, "wmod%d" % i, [128, 16, 512]) for i in range(2)]
        pm = cx.psum[0]
        wsrc = I["w_mod"].rearrange("(k p) n -> p k n", p=128)
        for blk in range(0 if not cx.flags.get("fast0") else 24, 24):
            wb = wbuf[blk % 2]
            for kh in range(4):
                P.dma("sp", wb[:, kh * 4:(kh + 1) * 4, :], wsrc[:, kh * 4:(kh + 1) * 4, blk * 512:(blk + 1) * 512], writes=[wb.r])
            for jj in range(4):
                j = blk * 4 + jj
                for k in range(16):
                    mm(cx, pm[:, j * 2:j * 2 + 2], wb[:, k, jj * 128:(jj + 1) * 128], sg[:, k, :], k == 0, k == 15,
                       [wb.r, sg.r], [pm.r])
        for col in range(2):
            tt(cx, cx.modv[:, :, col], pm[:, 0:192].rearrange("p (j c) -> p j c", c=2)[:, :, col], cx.pvec[:, PV_BMOD:PV_BMOD + 96],
               ALU.add, [pm.r, cx.pvec.r], [cx.modv.r])
    dv, mv, pv = cx.dv, cx.modv, cx.pvec
    for col in range(2):
        stt(cx, dv[:, col, :], mv[:, 16:32, col], 1.0, pv[:, PV_GATTN:PV_GATTN + 16], ALU.add, ALU.mult, [mv.r, pv.r], [dv.r])
        cp(cx, dv[:, 2 + col, :], mv[:, 0:16, col], [mv.r], [dv.r])
    cp(cx, dv[:, 4, :], mv[:, 32:48, 0], [mv.r], [dv.r])
    stt(cx, dv[:, 5, :], mv[:, 64:80, 0], 1.0, pv[:, PV_GFFN:PV_GFFN + 16], ALU.add, ALU.mult, [mv.r, pv.r], [dv.r])
    cp(cx, dv[:, 6, :], mv[:, 48:64, 0], [mv.r], [dv.r])
    cp(cx, dv[:, 7, :], mv[:, 80:96, 0], [mv.r], [dv.r])


def phase1(cx):
    nc, P, I, S = cx.nc, cx.P, cx.I, cx.S
    G = 256
    q0 = cx.q0
    with ExitStack() as ps:
        w1p = sb(cx, ps, "w1p", [128, 16, NP1], BF16)
        wsrc = I["w1p"].rearrange("(k p) n -> p k n", p=128)
        for k in range(16):
            P.dma("pool", w1p[:, k, :], wsrc[:, k, :], writes=[w1p.r])
        xb = [sb(cx, ps, "xg%d" % i, [128, 16, G]) for i in range(2)]
        sq = sb(cx, ps, "sq", [128, 16, G], BF16)
        hT = sb(cx, ps, "hT", [128, 16, G], BF16)
        rstd = sb(cx, ps, "rstd", [128, G])
        tmp = [sb(cx, ps, "tmp%d" % i, [128, G]) for i in range(2)]
        stg = [sb(cx, ps, "stg%d" % i, [64, NRW, G]) for i in range(2)]
        kvs = [sb(cx, ps, "kvs%d" % i, [128, 2, G]) for i in range(2)]
        kvq = sb(cx, ps, "kvq", [128, 2, G], BF16)
        kvr = sb(cx, ps, "kvr", [128, G])
        kvo = [sb(cx, ps, "kvo%d" % i, [128, 2, G], BF16) for i in range(2)]
        krs = [sb(cx, ps, "krs%d" % i, [64, 2, G]) for i in range(2)]
        kro = [sb(cx, ps, "kro%d" % i, [64, G], BF16) for i in range(2)]
        rp = [sb(cx, ps, "rp%d" % i, [64, 2, G]) for i in range(2)]
        cqs = sb(cx, ps, "cqs", [128, 4, G])
        cqq = sb(cx, ps, "cqq", [128, 4, G], BF16)
        cqo = [sb(cx, ps, "cqo%d" % i, [128, 4, G], BF16) for i in range(2)]
        zt = sb(cx, ps, "zt", [64, NRW, 1])
        P.op("pool", lambda e: e.memset(zt[:], 0.0), writes=[zt.r])
        for tl_, n in ((S["rawp"], T), (S["rawc"], TC)):
            v = tl_.t.rearrange("(m p) t -> p m t", p=64)
            P.dma("sp", v[:, :, 0:1], zt[:], reads=[zt.r], writes=[tl_.r], allow_slow_non_contiguous=True)
            P.dma("sp", v[:, :, n + 1:n + 2], zt[:], reads=[zt.r], writes=[tl_.r], allow_slow_non_contiguous=True)

        groups = [("c", i) for i in range(TC // G)] + [("x", i) for i in range(T // G)] + [("o", i) for i in range(TQ // G)]
        pp = cx.psum
        for gi, (kind, i) in enumerate(groups):
            xg = xb[gi % 2]
            t0 = i * G
            if kind == "c":
                src = I["ctxT"].rearrange("(k p) t -> p k t", p=128)[:, :, t0:t0 + G]
                col = 1
            elif kind == "x":
                src = I["xT"].rearrange("(k p) t -> p k t", p=128)[:, :, t0:t0 + G]
                col = 0
            else:
                src = I["xoT"].rearrange("(k p) t -> p k t", p=128)[:, :, t0:t0 + G]
                col = 0
            for kh in range(2):
                P.dma("sp", xg[:, kh * 8:(kh + 1) * 8, :], src[:, kh * 8:(kh + 1) * 8, :], writes=[xg.r])
            act(cx, sq[:], xg[:], AF.Square, [xg.r], [sq.r])
            pss = pp[gi % 2]
            for k in range(16):
                mm(cx, pss[:, 0:G], cx.ones_bf[:], sq[:, k, :], k == 0, k == 15, [cx.ones_bf.r, sq.r], [pss.r])
            act(cx, rstd[:], pss[:, 0:G], AF.Ln, [pss.r, cx.epsb.r], [rstd.r], scale=1.0 / D, bias=cx.epsb[:, 0:1])
            act(cx, rstd[:], rstd[:], AF.Exp, [rstd.r], [rstd.r], scale=-0.5)
            for k in range(16):
                tm = tmp[k % 2]
                stt(cx, tm[:], xg[:, k, :], cx.dv[:, col, k:k + 1], rstd[:], ALU.mult, ALU.mult, [xg.r, cx.dv.r, rstd.r], [tm.r])
                if k % 2 == 0:
                    act(cx, hT[:, k, :], tm[:], AF.Identity, [tm.r, cx.dv.r], [hT.r], bias=cx.dv[:, 2 + col, k:k + 1], scale=1.0)
                else:
                    ts(cx, hT[:, k, :], tm[:], cx.dv[:, 2 + col, k:k + 1], None, ALU.add, None, [tm.r, cx.dv.r], [hT.r], eng="pool")
            if kind == "o":
                for m in range(4):
                    pc = pp[2 + m % 2]
                    for k in range(16):
                        mm(cx, pc[:, 0:G], w1p[:, k, m * 128:(m + 1) * 128], hT[:, k, :], k == 0, k == 15, [w1p.r, hT.r], [pc.r])
                    cp(cx, cqs[:, m, :], pc[:, 0:G], [pc.r], [cqs.r])
                act(cx, cqq[:], cqs[:], AF.Square, [cqs.r], [cqq.r])
                pq = pp[4]
                for m in range(4):
                    mm(cx, pq[:, 0:G], cx.ones_bf[:], cqq[:, m, :], m == 0, m == 3, [cx.ones_bf.r, cqq.r], [pq.r])
                act(cx, kvr[:], pq[:, 0:G], AF.Ln, [pq.r, cx.epsb.r], [kvr.r], scale=1.0 / 512, bias=cx.epsb[:, 0:1])
                act(cx, kvr[:], kvr[:], AF.Exp, [kvr.r], [kvr.r], scale=-0.5)
                co = cqo[i % 2]
                for m in range(4):
                    stt(cx, co[:, m, :], cqs[:, m, :], cx.pvec[:, PV_QNG + m:PV_QNG + m + 1], kvr[:], ALU.mult, ALU.mult,
                        [cqs.r, cx.pvec.r, kvr.r], [co.r])
                P.dma("sp", S["cqnT"].t.rearrange("(m p) t -> p m t", p=128)[:, :, t0:t0 + G], co[:], reads=[co.r], writes=[S["cqnT"].r])
                continue
            kv = kvs[gi % 2]
            for m in range(2):
                pc = pp[2 + m]
                c0 = 512 + m * 128
                for k in range(16):
                    mm(cx, pc[:, 0:G], w1p[:, k, c0:c0 + 128], hT[:, k, :], k == 0, k == 15, [w1p.r, hT.r], [pc.r])
                cp(cx, kv[:, m, :], pc[:, 0:G], [pc.r], [kv.r])
            act(cx, kvq[:], kv[:], AF.Square, [kv.r], [kvq.r])
            pq = pp[4]
            for m in range(2):
                mm(cx, pq[:, 0:G], cx.ones_bf[:], kvq[:, m, :], m == 0, m == 1, [cx.ones_bf.r, kvq.r], [pq.r])
            act(cx, kvr[:], pq[:, 0:G], AF.Ln, [pq.r, cx.epsb.r], [kvr.r], scale=1.0 / 256, bias=cx.epsb[:, 0:1])
            act(cx, kvr[:], kvr[:], AF.Exp, [kvr.r], [kvr.r], scale=-0.5)
            ko = kvo[gi % 2]
            for m in range(2):
                stt(cx, ko[:, m, :], kv[:, m, :], cx.pvec[:, PV_KVNG + m:PV_KVNG + m + 1], kvr[:], ALU.mult, ALU.mult,
                    [kv.r, cx.pvec.r, kvr.r], [ko.r])
            kvoff = T + t0 if kind == "c" else t0
            P.dma("sp", S["ckvnT"].t.rearrange("(m p) t -> p m t", p=128)[:, :, kvoff:kvoff + G], ko[:], reads=[ko.r], writes=[S["ckvnT"].r])
            kr_ = krs[gi % 2]
            pc = pp[5]
            for m in range(2):
                c0 = 768 + m * 64
                for k in range(16):
                    mm(cx, pc[0:64, m * G:(m + 1) * G], w1p[:, k, c0:c0 + 64], hT[:, k, :], k == 0, k == 15, [w1p.r, hT.r], [pc.r])
            o_ = kro[gi % 2]
            if kind == "c":
                cp(cx, o_[:], pc[0:64, 0:G], [pc.r], [o_.r])
            else:
                r_ = rp[gi % 2]
                P.dma("sp", r_[:], I["rope"][:, :, t0:t0 + G], writes=[r_.r])
                tt(cx, kr_[:], pc[0:64, 0:2 * G].rearrange("p (m t) -> p m t", m=2), r_[:], ALU.mult, [pc.r, r_.r], [kr_.r])
                tt(cx, o_[:], kr_[:, 0, :], kr_[:, 1, :], ALU.add, [kr_.r], [o_.r])
            P.dma("sp", S["krotT"].t[:, kvoff:kvoff + G], o_[:], reads=[o_.r], writes=[S["krotT"].r])
            sg_ = stg[gi % 2]
            for m, (nm, M, c0) in enumerate(PCH[8:]):
                pc = pp[6 + m % 2]
                for k in range(16):
                    mm(cx, pc[0:M, 0:G], w1p[:, k, c0:c0 + M], hT[:, k, :], k == 0, k == 15, [w1p.r, hT.r], [pc.r])
                if m % 2 == 0:
                    cp(cx, sg_[0:M, m, :], pc[0:M, 0:G], [pc.r], [sg_.r])
                else:
                    act(cx, sg_[0:M, m, :], pc[0:M, 0:G], AF.Copy, [pc.r], [sg_.r])
            dst = S["rawc"] if kind == "c" else S["rawp"]
            P.dma("sp", dst.t.rearrange("(m p) t -> p m t", p=64)[:, :, 1 + t0:1 + t0 + G], sg_[:], reads=[sg_.r], writes=[dst.r])


NCK = (TC + T) // CH
RV_W0, RV_A0, RV_KK, RV_KA, RV_RK, RV_LG, RV_LB, RV_N = 0, 8, 16, 20, 24, 28, 32, 36


def phase2(cx, ps_):
    nc, P, I, S = cx.nc, cx.P, cx.I, cx.S
    pp = cx.psum
    X = mybir.AxisListType.X
    rv = sb(cx, ps_, "rv", [64, RV_N])
    P.dma("sp", rv[:], I["rvec"][:, :], writes=[rv.r])
    nrv = sb(cx, ps_, "nrv", [64, 16])
    ts(cx, nrv[:], rv[:, 0:16], -1.0, None, ALU.mult, None, [rv.r], [nrv.r])
    omka = sb(cx, ps_, "omka", [64, 4])
    ts(cx, omka[:], rv[:, RV_KA:RV_KA + 4], -1.0, 1.0, ALU.mult, ALU.add, [rv.r], [omka.r])
    mux = sb(cx, ps_, "mux", [64, 3, NRW, 64])
    P.dma("sp", mux[:, 0:2], I["mux"][:, :, :, :], writes=[mux.r])
    tt(cx, mux[:, 2], mux[:, 0], mux[:, 1], ALU.add, [mux.r], [mux.r])
    ts(cx, mux[:, 2], mux[:, 2], -1.0, 1.0, ALU.mult, ALU.add, [mux.r], [mux.r])
    upw = sb(cx, ps_, "upw", [64, 2, 2, 256])
    P.dma("sp", upw[:], I["upw"][:, :, :, :], writes=[upw.r])
    gup = sb(cx, ps_, "gup", [64, 3, 256])
    P.dma("sp", gup[:], I["gup"][:, :, :], writes=[gup.r])
    m1mask = sb(cx, ps_, "m1mask", [128, 2, 128])
    P.dma("sp", m1mask[:], I["m1mask"][:, :, :], writes=[m1mask.r])
    p0mask = sb(cx, ps_, "p0mask", [64, 2, 64])
    P.dma("sp", p0mask[:], I["p0mask"][:, :, :], writes=[p0mask.r])
    zer = sb(cx, ps_, "zer", [64, 64])
    P.op("pool", lambda e: e.memset(zer[:], 0.0), writes=[zer.r])
    ones64 = cx.ones_f
    idn = cx.ident
    unit_v = S["unit"].t.rearrange("(g p) n -> g p n", p=64)
    st_v = S["states"].t.rearrange("(g p) n -> g p n", p=64)
    rest_v = S["rest"].t.rearrange("(g p) n -> g p n", p=64)

    with ExitStack() as st:
        Wn = [sb(cx, st, "Wn%d" % i, [64, NRW, 66]) for i in range(2)]
        sh = sb(cx, st, "sh", [64, NRW, 64])
        t17 = sb(cx, st, "t17", [64, NRW, 64])
        kk = sb(cx, st, "kk", [64, 4, 64])
        t4 = sb(cx, st, "t4", [64, 4, 64])
        kkn = sb(cx, st, "kkn", [64, 4, 64])
        th = sb(cx, st, "th", [64, 64])
        wd = sb(cx, st, "wd", [64, 8, 64])
        aa = sb(cx, st, "aa", [64, 8, 64])
        kd = sb(cx, st, "kd", [64, 8, 64])
        pref = sb(cx, st, "pref", [64, 8, 65])
        rp = sb(cx, st, "rp", [64, 8, 65])
        tot = sb(cx, st, "tot", [64, 8])
        rtot = sb(cx, st, "rtot", [64, 8])
        GIN = sb(cx, st, "GIN", [64, 8, 64])
        GEX = sb(cx, st, "GEX", [64, 8, 64])
        GINV = sb(cx, st, "GINV", [64, 8, 64])
        BKs = [sb(cx, st, "BK%d" % i, [64, 8, 2, 64]) for i in range(2)]
        ARs = [sb(cx, st, "AR%d" % i, [64, 8, 2, 64]) for i in range(2)]
        AVs = [sb(cx, st, "AV%d" % i, [64, 8, 2, 64]) for i in range(2)]
        BKgs = [sb(cx, st, "BKg%d" % i, [64, 8, 2, 64]) for i in range(2)]
        t8 = sb(cx, st, "t8", [64, 8, 64])
        M1m = [sb(cx, st, "M1m%d" % u, [128, 128], BF16) for u in range(8)]
        Z = [sb(cx, st, "Z%d" % u, [128, 128], BF16) for u in range(8)]
        T2s = [sb(cx, st, "T2s%d" % u, [128, 64], BF16) for u in range(8)]
        PQ = [sb(cx, st, "PQ%d" % u, [64, 2, 2, 64], BF16) for u in range(8)]
        dgs = [[sb(cx, st, "dg%d_%d" % (i, u), [64, 64]) for u in range(8)] for i in range(2)]
        Ou = [sb(cx, st, "Ou%d" % i, [64, 8, 4, 64]) for i in range(2)]
        rst = [sb(cx, st, "rst%d" % i, [64, 2, 4, 64]) for i in range(2)]
        sg = sb(cx, st, "sgl", [64, 3, 64])
        P.op("pool", lambda e: e.memset(pref[:], 1.0), writes=[pref.r])

        def prep_gen(gc):
            par = gc % 2
            BK_, AR_, AV_, BKg_, dg_ = BKs[par], ARs[par], AVs[par], BKgs[par], dgs[par]
            lat = gc >= 4
            ci = gc - 4 if lat else gc
            src = (S["rawp"] if lat else S["rawc"])
            W = Wn[gc % 2]
            P.dma("sp", W[:], src.t.rearrange("(m p) t -> p m t", p=64)[:, :, ci * 64:ci * 64 + 66], reads=[src.r], writes=[W.r])
            yield
            tt(cx, sh[:], W[:, :, 1:65], mux[:, 2], ALU.mult, [W.r, mux.r], [sh.r])
            tt(cx, t17[:], W[:, :, 0:64], mux[:, 0], ALU.mult, [W.r, mux.r], [t17.r])
            tt(cx, sh[:], sh[:], t17[:], ALU.add, [sh.r, t17.r], [sh.r])
            tt(cx, t17[:], W[:, :, 2:66], mux[:, 1], ALU.mult, [W.r, mux.r], [t17.r], eng="pool")
            tt(cx, sh[:], sh[:], t17[:], ALU.add, [sh.r, t17.r], [sh.r])
            rs, ks, vs = sh[:, 0:4], sh[:, 4:8], sh[:, 8:12]
            yield
            for h in range(4):
                ts(cx, kk[:, h, :], sh[:, 4 + h, :], rv[:, RV_KK + h:RV_KK + h + 1], None, ALU.mult, None, [sh.r, rv.r], [kk.r], eng="pool")
            tt(cx, t4[:], kk[:], kk[:], ALU.mult, [kk.r], [t4.r])
            pa = pp[7]
            mm(cx, pa[0:64, 0:256], ones64[0:64, 0:64], t4[:].rearrange("p a b -> p (a b)"), True, True, [ones64.r, t4.r], [pa.r])
            act(cx, t4[:].rearrange("p a b -> p (a b)"), pa[0:64, 0:256], AF.Ln, [pa.r, cx.epsb.r], [t4.r], bias=cx.epsb[0:64, 2:3], scale=1.0)
            act(cx, t4[:], t4[:], AF.Exp, [t4.r], [t4.r], scale=-0.5)
            tt(cx, kkn[:], kk[:], t4[:], ALU.mult, [kk.r, t4.r], [kkn.r])
            yield
            act(cx, th[:], sh[:, 12, :], AF.Exp, [sh.r], [th.r], scale=-2.0)
            ts(cx, th[:], th[:], 1.0, None, ALU.add, None, [th.r], [th.r])
            recip(cx, th[:], th[:], [th.r], [th.r])
            ts(cx, th[:], th[:], 2.0, -1.0, ALU.mult, ALU.add, [th.r], [th.r])
            pw, pq = pp[7], pp[6]
            for u in range(8):
                d, h = u // 4, u % 4
                mm(cx, pw[0:64, u * 64:(u + 1) * 64], upw[:, 0, d, h * 64:(h + 1) * 64], th[:], True, True, [upw.r, th.r], [pw.r])
                mm(cx, pq[0:64, u * 64:(u + 1) * 64], upw[:, 1, d, h * 64:(h + 1) * 64], sh[:, 13, :], True, True, [upw.r, sh.r], [pq.r])
            for u in range(8):
                act(cx, wd[:, u, :], pw[0:64, u * 64:(u + 1) * 64], AF.Exp, [pw.r, nrv.r], [wd.r], scale=-1.0, bias=nrv[:, u:u + 1])
                act(cx, aa[:, u, :], pq[0:64, u * 64:(u + 1) * 64], AF.Exp, [pq.r, nrv.r], [aa.r], scale=-1.0, bias=nrv[:, 8 + u:9 + u])
            for tl_ in (wd, aa):
                ts(cx, tl_[:], tl_[:], 1.0, None, ALU.add, None, [tl_.r], [tl_.r])
                recip(cx, tl_[:], tl_[:], [tl_.r], [tl_.r])
            act(cx, wd[:], wd[:], AF.Exp, [wd.r], [wd.r], scale=-0.6065306597126334)
            yield
            for u in range(8):
                h = u % 4
                ts(cx, kd[:, u, :], aa[:, u, :], rv[:, RV_KA + h:RV_KA + h + 1], omka[:, h:h + 1], ALU.mult, ALU.add, [aa.r, rv.r, omka.r], [kd.r], eng="pool")
            yield
            for d in range(2):
                tt(cx, kd[:, d * 4:(d + 1) * 4], kd[:, d * 4:(d + 1) * 4], ks, ALU.mult, [kd.r, sh.r], [kd.r])
            yield
            for u in range(8):
                P.op("dve", lambda e: e.tensor_tensor_scan(out=pref[:, u, 1:65], data0=wd[:, u, :], data1=zer[:], initial=1.0, op0=ALU.mult, op1=ALU.add),
                     reads=[wd.r, zer.r], writes=[pref.r])
            yield
            recip(cx, rp[:], pref[:], [pref.r], [rp.r])
            cp(cx, tot[:], pref[:, :, 64], [pref.r], [tot.r])
            cp(cx, rtot[:], rp[:, :, 64], [rp.r], [rtot.r])
            cp(cx, GIN[:, 0:4], pref[:, 0:4, 1:65], [pref.r], [GIN.r], eng="pool")
            cp(cx, GEX[:, 0:4], pref[:, 0:4, 0:64], [pref.r], [GEX.r], eng="pool")
            cp(cx, GINV[:, 0:4], rp[:, 0:4, 1:65], [rp.r], [GINV.r], eng="pool")
            for u in range(4, 8):
                ts(cx, GIN[:, u, :], rp[:, u, 0:64], tot[:, u:u + 1], None, ALU.mult, None, [rp.r, tot.r], [GIN.r], eng="pool")
                ts(cx, GEX[:, u, :], rp[:, u, 1:65], tot[:, u:u + 1], None, ALU.mult, None, [rp.r, tot.r], [GEX.r], eng="pool")
                ts(cx, GINV[:, u, :], pref[:, u, 0:64], rtot[:, u:u + 1], None, ALU.mult, None, [pref.r, rtot.r], [GINV.r], eng="pool")
            yield
            for d in range(2):
                sl = slice(d * 4, (d + 1) * 4)
                stt(cx, AR_[:, sl, 0, :], kkn[:], -1.0, GEX[:, sl], ALU.mult, ALU.mult, [kkn.r, GEX.r], [AR_.r])
                tt(cx, AR_[:, sl, 1, :], rs, GIN[:, sl], ALU.mult, [sh.r, GIN.r], [AR_.r])
                tt(cx, t8[:, sl], kkn[:], aa[:, sl], ALU.mult, [kkn.r, aa.r], [t8.r])
                cp(cx, AV_[:, sl, 1, :], vs, [sh.r], [AV_.r], eng="pool")
            yield
            tt(cx, BK_[:, :, 0, :], t8[:], GINV[:], ALU.mult, [t8.r, GINV.r], [BK_.r])
            tt(cx, BK_[:, :, 1, :], kd[:], GINV[:], ALU.mult, [kd.r, GINV.r], [BK_.r])
            cp(cx, AV_[:, :, 0, :], AR_[:, :, 0, :], [AR_.r], [AV_.r], eng="pool")
            for u in range(8):
                ts(cx, BKg_[:, u], BK_[:, u], tot[:, u:u + 1], None, ALU.mult, None, [BK_.r, tot.r], [BKg_.r], eng="pool")
                ts(cx, dg_[u][:], idn[0:64, 0:64], tot[:, u:u + 1], None, ALU.mult, None, [idn.r, tot.r], [dg_[u].r], eng="pool")
            yield
            if lat:
                R_ = rst[gc % 2]
                tt(cx, t4[:], kd[:, 0:4], kd[:, 4:8], ALU.add, [kd.r], [t4.r])
                tt(cx, t4[:], t4[:], rs, ALU.mult, [t4.r, sh.r], [t4.r])
                for h in range(4):
                    ts(cx, t4[:, h, :], t4[:, h, :], rv[:, RV_RK + h:RV_RK + h + 1], None, ALU.mult, None, [t4.r, rv.r], [t4.r])
                mm(cx, pa[0:64, 256:512], ones64[0:64, 0:64], t4[:].rearrange("p a b -> p (a b)"), True, True, [ones64.r, t4.r], [pa.r])
                tt(cx, R_[:, 0].rearrange("p a b -> p (a b)"), pa[0:64, 256:512], sh[:, 8:12].rearrange("p a b -> p (a b)"), ALU.mult, [pa.r, sh.r], [R_.r])
                act(cx, sg[:], sh[:, 14:17], AF.Exp, [sh.r], [sg.r], scale=-1.0)
                ts(cx, sg[:], sg[:], 1.0, None, ALU.add, None, [sg.r], [sg.r])
                recip(cx, sg[:], sg[:], [sg.r], [sg.r])
                pg = pp[7]
                for h in range(4):
                    for j, kj in enumerate((64, 64, 32)):
                        mm(cx, pg[0:64, h * 64:(h + 1) * 64], gup[0:kj, j, h * 64:(h + 1) * 64], sg[0:kj, j, :], j == 0, j == 2, [gup.r, sg.r], [pg.r])
                cp(cx, R_[:, 1].rearrange("p a b -> p (a b)"), pg[0:64, 0:256], [pg.r], [R_.r])
                P.dma("sp", rest_v[ci].rearrange("p (a n) -> p a n", a=2), R_[:].rearrange("p a h t -> p a (h t)"), reads=[R_.r], writes=[S["rest"].r])

            yield

        def units(gc, pg_):
            par = gc % 2
            BK_, AR_, AV_, BKg_, dg_ = BKs[par], ARs[par], AVs[par], BKgs[par], dgs[par]
            O = Ou[gc % 2]

            def tick():
                if pg_ is not None:
                    next(pg_, None)

            waves = (range(0, 6), range(6, 8))
            for wave in waves:
                tick()
                for u in wave:
                    pu = pp[u % 6]
                    mm(cx, pu[:, 0:128], BK_[:, u].rearrange("p a b -> p (a b)"), AR_[:, u].rearrange("p a b -> p (a b)"), True, True, [BK_.r, AR_.r], [pu.r])
                    mm(cx, pu[0:64, 128:192], AR_[:, u, 0, :], BK_[:, u, 0, :], True, True, [BK_.r, AR_.r], [pu.r])
                    P.op("pe", lambda e: e.transpose(pu[:, 192:256], AV_[:, u].rearrange("p a b -> p (a b)"), idn[0:64, 0:64]), reads=[AV_.r, idn.r], writes=[pu.r])
                    P.op("pe", lambda e: e.transpose(pu[:, 256:320], BKg_[:, u].rearrange("p a b -> p (a b)"), idn[0:64, 0:64]), reads=[BKg_.r, idn.r], writes=[pu.r])
                tick()
                for u in wave:
                    d = u // 4
                    pu = pp[u % 6]
                    tt(cx, M1m[u][:], pu[:, 0:128], m1mask[:, d, :], ALU.mult, [pu.r, m1mask.r], [M1m[u].r])
                    tt(cx, PQ[u][:, 0, 0, :], pu[0:64, 128:192], p0mask[:, d, :], ALU.mult, [pu.r, p0mask.r], [PQ[u].r])
                    cp(cx, Z[u][0:64, 0:64], pu[0:64, 192:256], [pu.r], [Z[u].r])
                    cp(cx, Z[u][64:128, 64:128], pu[64:128, 192:256], [pu.r], [Z[u].r])
                    cp(cx, T2s[u][:], pu[:, 256:320], [pu.r], [T2s[u].r])
                    cp(cx, PQ[u][:, 0, 1, :], M1m[u][0:64, 0:64], [M1m[u].r], [PQ[u].r], eng="pool")
            for wave in waves:
                tick()
                for u in wave:
                    pu = pp[u % 6]
                    mm(cx, pu[0:64, 320:384], M1m[u][64:128, 0:64], Z[u][64:128, 64:128], True, True, [M1m[u].r, Z[u].r], [pu.r])
                for u in wave:
                    pu = pp[u % 6]
                    cp(cx, Z[u][0:64, 64:128], pu[0:64, 320:384], [pu.r], [Z[u].r])
            for j in range(6):
                b0, b1 = j % 2, (j + 1) % 2
                for wave in waves:
                    tick()
                    for u in wave:
                        pu = pp[u % 6]
                        mm(cx, pu[0:64, 384:512], PQ[u][:, b0, 1, :], Z[u][0:64, :], True, True, [PQ[u].r, Z[u].r], [pu.r])
                        if j < 5:
                            mm(cx, pu[0:64, 128:192], PQ[u][:, b0, 1, :], PQ[u][:, b0, 0, :], True, True, [PQ[u].r], [pu.r])
                            mm(cx, pu[0:64, 192:256], PQ[u][:, b0, 0, :], PQ[u][:, b0, 1, :], True, True, [PQ[u].r], [pu.r])
                    for u in wave:
                        pu = pp[u % 6]
                        tt(cx, Z[u][0:64, :], Z[u][0:64, :], pu[0:64, 384:512], ALU.add, [Z[u].r, pu.r], [Z[u].r])
                        if j < 5:
                            cp(cx, PQ[u][:, b1].rearrange("p a b -> p (a b)"), pu[0:64, 128:256], [pu.r], [PQ[u].r])
            for wave in waves:
                tick()
                for u in wave:
                    pu = pp[u % 6]
                    mm(cx, pu[0:64, 0:64], Z[u][0:64, 0:64], M1m[u][0:64, 64:128], True, True, [Z[u].r, M1m[u].r], [pu.r])
                    mm(cx, pu[0:64, 64:128], Z[u][0:64, 0:64], T2s[u][0:64, :], True, True, [Z[u].r, T2s[u].r], [pu.r])
                    mm(cx, pu[0:64, 128:192], T2s[u][:], Z[u][:, 64:128], True, True, [Z[u].r, T2s[u].r], [pu.r])
                    mm(cx, pu[0:64, 192:256], Z[u][:, 64:128], M1m[u][:, 64:128], True, True, [Z[u].r, M1m[u].r], [pu.r])
                for u in wave:
                    pu = pp[u % 6]
                    tt(cx, O[:, u, 2, :], pu[0:64, 0:64], AR_[:, u, 1, :], ALU.add, [pu.r, AR_.r], [O.r])
                    tt(cx, O[:, u, 0, :], pu[0:64, 64:128], dg_[u][:], ALU.add, [pu.r, dg_[u].r], [O.r])
                    cp(cx, O[:, u, 1, :], pu[0:64, 128:192], [pu.r], [O.r])
                    cp(cx, O[:, u, 3, :], pu[0:64, 192:256], [pu.r], [O.r])
            P.dma("sp", unit_v[gc], O[:].rearrange("p u a t -> p (u a t)"), reads=[O.r], writes=[S["unit"].r])

        chunks = list(range(NCK) if "nchunks" not in cx.flags else cx.flags["nchunks"])
        g0 = prep_gen(chunks[0])
        for _ in g0:
            pass
        for ii, gc in enumerate(chunks):
            nxt = prep_gen(chunks[ii + 1]) if ii + 1 < len(chunks) else None
            units(gc, nxt)
            if nxt is not None:
                for _ in nxt:
                    pass
    P.barrier()
    if cx.flags.get("stopA"):
        return

    with ExitStack() as st:
        ST = [sb(cx, st, "ST%d" % i, [64, 8, 64]) for i in range(3)]
        GH = [sb(cx, st, "GH%d" % i, [64, 8, 2, 64]) for i in range(3)]
        P.op("pool", lambda e: e.memset(ST[0][:], 0.0), writes=[ST[0].r])
        order_f = list(range(NCK))
        order_b = [3, 2, 1, 0] + [4 + i for i in range(127, -1, -1)]
        uv = S["unit"].t.rearrange("(g p) (u a t) -> g p u a t", p=64, u=8, a=4)
        sv = S["states"].t.rearrange("(g p) (u t) -> g p u t", p=64, u=8)
        for s_ in range(NCK):
            gf, gb = order_f[s_], order_b[s_]
            cur, nxt = ST[s_ % 3], ST[(s_ + 1) % 3]
            g_ = GH[s_ % 3]
            P.dma("sp", g_[:, 0:4], uv[gf][:, 0:4, 0:2, :], reads=[S["unit"].r], writes=[g_.r])
            P.dma("sp", g_[:, 4:8], uv[gb][:, 4:8, 0:2, :], reads=[S["unit"].r], writes=[g_.r])
            P.dma("sp", sv[gf][:, 0:4, :], cur[:, 0:4, :], reads=[cur.r], writes=[S["states"].r])
            P.dma("sp", sv[gb][:, 4:8, :], cur[:, 4:8, :], reads=[cur.r], writes=[S["states"].r])
            pb_ = pp[s_ % 2]
            for u in range(8):
                mm(cx, pb_[0:64, u * 64:(u + 1) * 64], g_[:, u, 0, :], cur[:, u, :], True, True, [g_.r, cur.r], [pb_.r])
            tt(cx, nxt[:], pb_[0:64, 0:512].rearrange("p (u t) -> p u t", u=8), g_[:, :, 1, :], ALU.add, [pb_.r, g_.r], [nxt.r])
    P.barrier()
    if cx.flags.get("stopB"):
        return

    with ExitStack() as st:
        S0 = [sb(cx, st, "S0_%d" % i, [64, 8, 64]) for i in range(2)]
        RY = [sb(cx, st, "RY%d" % i, [64, 8, 2, 64]) for i in range(2)]
        RS = [sb(cx, st, "RS%d" % i, [64, 2, 4, 64]) for i in range(2)]
        y8 = sb(cx, st, "y8", [64, 8, 64])
        y = sb(cx, st, "y", [64, 4, 64])
        ysq = sb(cx, st, "ysq", [64, 4, 64])
        mu = sb(cx, st, "mu", [64, 4, 64])
        var = sb(cx, st, "var", [64, 4, 64])
        ob = [sb(cx, st, "ob%d" % i, [64, 4, 512], BF16) for i in range(2)]
        uv = S["unit"].t.rearrange("(g p) (u a t) -> g p u a t", p=64, u=8, a=4)
        sv = S["states"].t.rearrange("(g p) (u t) -> g p u t", p=64, u=8)
        rv4 = S["rest"].t.rearrange("(g p) (a h t) -> g p a h t", p=64, a=2, h=4)
        for ci in range(cx.flags.get("cchunks", T // CH)):
            gc = 4 + ci
            s0, ry, rs_ = S0[ci % 2], RY[ci % 2], RS[ci % 2]
            P.dma("sp", s0[:], sv[gc], reads=[S["states"].r], writes=[s0.r])
            P.dma("sp", ry[:], uv[gc][:, :, 2:4, :], reads=[S["unit"].r], writes=[ry.r])
            P.dma("sp", rs_[:], rv4[ci], reads=[S["rest"].r], writes=[rs_.r])
            pc_ = pp[ci % 2]
            for u in range(8):
                mm(cx, pc_[0:64, u * 64:(u + 1) * 64], s0[:, u, :], ry[:, u, 0, :], True, True, [s0.r, ry.r], [pc_.r])
            tt(cx, y8[:], pc_[0:64, 0:512].rearrange("p (u t) -> p u t", u=8), ry[:, :, 1, :], ALU.add, [pc_.r, ry.r], [y8.r])
            tt(cx, y[:], y8[:, 0:4], y8[:, 4:8], ALU.add, [y8.r], [y.r])
            if cx.flags.get("ccut") == 1:
                continue
            tt(cx, ysq[:], y[:], y[:], ALU.mult, [y.r], [ysq.r], eng="pool")
            pm_ = pp[2 + ci % 2]
            mm(cx, pm_[0:64, 0:256], ones64[0:64, 0:64], y[:].rearrange("p a b -> p (a b)"), True, True, [ones64.r, y.r], [pm_.r])
            mm(cx, pm_[0:64, 256:512], ones64[0:64, 0:64], ysq[:].rearrange("p a b -> p (a b)"), True, True, [ones64.r, ysq.r], [pm_.r])
            if cx.flags.get("ccut") == 2:
                continue
            muf, varf = mu[:].rearrange("p a b -> p (a b)"), var[:].rearrange("p a b -> p (a b)")
            ts(cx, muf, pm_[0:64, 0:256], 1.0 / 64, None, ALU.mult, None, [pm_.r], [mu.r])
            tt(cx, ysq[:], mu[:], mu[:], ALU.mult, [mu.r], [ysq.r])
            stt(cx, varf, pm_[0:64, 256:512], 1.0 / 64, ysq[:].rearrange("p a b -> p (a b)"), ALU.mult, ALU.subtract, [pm_.r, ysq.r], [var.r])
            act(cx, varf, varf, AF.Ln, [var.r, cx.epsb.r], [var.r], bias=cx.epsb[0:64, 1:2], scale=1.0)
            act(cx, varf, varf, AF.Exp, [var.r], [var.r], scale=-0.5)
            if cx.flags.get("ccut") == 3:
                continue
            tt(cx, y[:], y[:], mu[:], ALU.subtract, [y.r, mu.r], [y.r])
            tt(cx, y[:], y[:], var[:], ALU.mult, [y.r, var.r], [y.r])
            for h in range(4):
                ts(cx, y[:, h, :], y[:, h, :], rv[:, RV_LG + h:RV_LG + h + 1], rv[:, RV_LB + h:RV_LB + h + 1], ALU.mult, ALU.add, [y.r, rv.r], [y.r])
            tt(cx, y[:], y[:], rs_[:, 0], ALU.add, [y.r, rs_.r], [y.r])
            if cx.flags.get("ccut") == 4:
                continue
            o_ = ob[(ci // 8) % 2]
            tt(cx, o_[:, :, (ci % 8) * 64:(ci % 8 + 1) * 64], y[:], rs_[:, 1], ALU.mult, [y.r, rs_.r], [o_.r])
            if cx.flags.get("ccut") == 5:
                continue
            if ci % 8 == 7:
                t0 = (ci // 8) * 512
                rin = S["rwkv_in%d" % (t0 // TQ)]
                P.dma("pool", rin.t.rearrange("(h p) t -> p h t", p=64)[:, :, t0 % TQ:t0 % TQ + 512], o_[:], reads=[o_.r], writes=[rin.r])
                if "rwkv_dbg" in S:
                    P.dma("pool", S["rwkv_dbg"].t.rearrange("(h p) t -> p h t", p=64)[:, :, t0:t0 + 512], o_[:], reads=[o_.r], writes=[S["rwkv_dbg"].r])
    P.barrier()
    if cx.flags.get("stopC"):
        return
    for j in range(4):
        rin, ra = S["rwkv_in%d" % j], S["rwkv_all%d" % j].r
        P._deps("pool", [rin.r], [ra])
        inst = nc.gpsimd.collective_compute("AllGather", ALU.bypass, replica_groups=[[0, 1, 2, 3], [4, 5, 6, 7]],
                                            ins=[rin.t], outs=[S["rwkv_all%d" % j].t])
        if ra.sem is None:
            ra.sem = nc.alloc_semaphore("d_rwkv_all%d" % j)
            P.all_dma.append(ra)
        ra.dcount += 1
        inst.then_inc(ra.sem, 1)
        P.ninst += 1
        P._post((ra.sem, ra.dcount), [rin.r], [ra])


def phase3(cx, ps_):
    nc, P, I, S = cx.nc, cx.P, cx.I, cx.S
    pp = cx.psum
    NT = NKV // 128
    ckv = sb(cx, ps_, "a_ckv", [128, 2, NKV], BF16)
    krot = sb(cx, ps_, "a_krot", [64, NKV], BF16)
    cqn = sb(cx, ps_, "a_cqn", [128, 4, TQ], BF16)
    wq = sb(cx, ps_, "a_wq", [128, 4, 8 * 256], BF16)
    wk = sb(cx, ps_, "a_wk", [128, 2, 8 * 128], BF16)
    wv = sb(cx, ps_, "a_wv", [128, 2, 8 * 128], BF16)
    rqs = [sb(cx, ps_, "a_rq%d" % i, [64, 2, 512]) for i in range(2)]
    otl = [sb(cx, ps_, "a_ot%d" % i, [128, 512], BF16) for i in range(2)]
    knT = sb(cx, ps_, "a_knT", [128, NKV], BF16)
    vh = sb(cx, ps_, "a_vh", [128, NT, 128], BF16)
    qnT = sb(cx, ps_, "a_qnT", [128, TQ], BF16)
    qrT = sb(cx, ps_, "a_qrT", [64, TQ], BF16)
    qtmp = sb(cx, ps_, "a_qtmp", [64, 2, 512])
    pT = [sb(cx, ps_, "a_pT%d" % i, [128, 512], BF16) for i in range(3)]
    rden = sb(cx, ps_, "a_rden", [128, 512])
    P.dma("sp", ckv[:], S["ckvnT"].t.rearrange("(m p) t -> p m t", p=128), reads=[S["ckvnT"].r], writes=[ckv.r])
    P.dma("sp", krot[:], S["krotT"].t[:, :], reads=[S["krotT"].r], writes=[krot.r])
    P.dma("sp", cqn[:], S["cqnT"].t.rearrange("(m p) t -> p m t", p=128), reads=[S["cqnT"].r], writes=[cqn.r])
    for k in range(4):
        P.dma("pool", wq[:, k, :], I["wq"].rearrange("(k p) n -> p k n", p=128)[:, k, :], writes=[wq.r])
    for k in range(2):
        P.dma("pool", wk[:, k, :], I["wk"].rearrange("(k p) n -> p k n", p=128)[:, k, :], writes=[wk.r])
        P.dma("pool", wv[:, k, :], I["wv"].rearrange("(k p) n -> p k n", p=128)[:, k, :], writes=[wv.r])
    ev = 0
    for h in range(8):
        for g in range((NKV + 511) // 512):
            t0 = g * 512
            n = min(512, NKV - t0)
            pc = pp[g % 2]
            for k in range(2):
                mm(cx, pc[:, 0:n], wk[:, k, h * 128:(h + 1) * 128], ckv[:, k, t0:t0 + n], k == 0, k == 1, [wk.r, ckv.r], [pc.r])
            if g % 2 == 0:
                cp(cx, knT[:, t0:t0 + n], pc[:, 0:n], [pc.r], [knT.r])
            else:
                act(cx, knT[:, t0:t0 + n], pc[:, 0:n], AF.Copy, [pc.r], [knT.r])
        for g in range((NT + 3) // 4):
            nt = min(4, NT - g * 4)
            pc = pp[2 + g % 2]
            for j in range(nt):
                t0 = (g * 4 + j) * 128
                for k in range(2):
                    mm(cx, pc[:, j * 128:(j + 1) * 128], ckv[:, k, t0:t0 + 128], wv[:, k, h * 128:(h + 1) * 128], k == 0, k == 1,
                       [wv.r, ckv.r], [pc.r])
            dst = vh[:, g * 4:g * 4 + nt, :].rearrange("p a b -> p (a b)")
            if g % 2 == 0:
                act(cx, dst, pc[:, 0:nt * 128], AF.Copy, [pc.r], [vh.r])
            else:
                cp(cx, dst, pc[:, 0:nt * 128], [pc.r], [vh.r])
        for g in range(4):
            t0 = g * 512
            pc = pp[g % 2]
            for k in range(4):
                mm(cx, pc[:, :], wq[:, k, h * 256:h * 256 + 128], cqn[:, k, t0:t0 + 512], k == 0, k == 3, [wq.r, cqn.r], [pc.r])
            act(cx, qnT[:, t0:t0 + 512], pc[:, :], AF.Copy, [pc.r], [qnT.r], scale=MLA_SCALE)
            pa, pb = pp[4], pp[5]
            for m, pdst in ((0, pa), (1, pb)):
                c0 = h * 256 + 128 + m * 64
                for k in range(4):
                    mm(cx, pdst[0:64, :], wq[:, k, c0:c0 + 64], cqn[:, k, t0:t0 + 512], k == 0, k == 3, [wq.r, cqn.r], [pdst.r])
            rq = rqs[g % 2]
            P.dma("sp", rq[:], I["ropeq"][:, :, t0:t0 + 512], writes=[rq.r])
            tt(cx, qtmp[:, 0, :], pa[0:64, :], rq[:, 0, :], ALU.mult, [pa.r, rq.r], [qtmp.r])
            tt(cx, qtmp[:, 1, :], pb[0:64, :], rq[:, 1, :], ALU.mult, [pb.r, rq.r], [qtmp.r])
            tt(cx, qtmp[:, 0, :], qtmp[:, 0, :], qtmp[:, 1, :], ALU.add, [qtmp.r], [qtmp.r])
            ts(cx, qrT[:, t0:t0 + 512], qtmp[:, 0, :], MLA_SCALE, None, ALU.mult, None, [qtmp.r], [qrT.r])
        for g in range(4):
            t0 = g * 512
            po, pd = pp[6], pp[7]
            def qk(j):
                k0 = j * 128
                pc = pp[j % 4]
                mm(cx, pc[:, :], knT[:, k0:k0 + 128], qnT[:, t0:t0 + 512], True, False, [knT.r, qnT.r], [pc.r])
                mm(cx, pc[:, :], krot[:, k0:k0 + 128], qrT[:, t0:t0 + 512], False, True, [krot.r, qrT.r], [pc.r])

            qk(0)
            qk(1)
            for j in range(NT):
                if j + 2 < NT:
                    qk(j + 2)
                pc = pp[j % 4]
                pt = pT[j % 3]
                act(cx, pt[:], pc[:, :], AF.Exp, [pc.r], [pt.r])
                mm(cx, po[:, :], vh[:, j, :], pt[:], j == 0, j == NT - 1, [vh.r, pt.r], [po.r])
                mm(cx, pd[:, :], cx.ones_bf[:], pt[:], j == 0, j == NT - 1, [cx.ones_bf.r, pt.r], [pd.r])
            recip(cx, rden[:], pd[:, :], [pd.r], [rden.r])
            ot_ = otl[g % 2]
            tt(cx, ot_[:], po[:, :], rden[:], ALU.mult, [po.r, rden.r], [ot_.r])
            P.dma("sp", S["catA"].t[h * 128:(h + 1) * 128, t0:t0 + 512], ot_[:], reads=[ot_.r], writes=[S["catA"].r])


def phase4(cx, ps_):
    nc, P, I, S = cx.nc, cx.P, cx.I, cx.S
    pp = cx.psum
    dv, pv = cx.dv, cx.pvec
    x2T = S["x2T"]
    x2v = x2T.t.rearrange("(m p) t -> p m t", p=128)
    xov = I["xoT"].rearrange("(m p) t -> p m t", p=128)
    x2n = sb(cx, ps_, "x2n", [128, 16, TQ], BF16)
    gateT = sb(cx, ps_, "gateT", [32, TQ])
    with ExitStack() as st:
        rstd2 = sb(cx, st, "rstd2", [128, TQ])
        cx.catT = sb(cx, st, "catT", [128, 16, TQ], BF16)
        P.dma("sp", cx.catT[:, 0:8, :], S["catA"].t.rearrange("(m p) t -> p m t", p=128), reads=[S["catA"].r], writes=[cx.catT.r])
        if cx.have_rwkv:
            selq = sb(cx, st, "selq", [128, 4])
            P.dma("sp", selq[:], I["selq"][:, :], writes=[selq.r])
            stg_ = sb(cx, st, "rstage", [128, 4, TQ], BF16)
            for mh in range(2):
                dst = cx.catT[:, 8 + mh * 4:12 + mh * 4, :]
                for j in range(4):
                    raj = S["rwkv_all%d" % j]
                    P.dma("sp", stg_[:], raj.t.rearrange("(m p) t -> p m t", p=128)[:, mh * 4:(mh + 1) * 4, :], reads=[raj.r], writes=[stg_.r])
                    if j == 0:
                        ts(cx, dst, stg_[:], selq[:, 0:1], None, ALU.mult, None, [stg_.r, selq.r], [cx.catT.r])
                    else:
                        stt(cx, dst, stg_[:], selq[:, j:j + 1], dst, ALU.mult, ALU.add, [stg_.r, selq.r, cx.catT.r], [cx.catT.r])
        else:
            P.op("pool", lambda e: e.memset(cx.catT[:, 8:16, :], 0.0), writes=[cx.catT.r])
        wo = [sb(cx, st, "wo%d" % i, [128, 16, 128], BF16) for i in range(2)]
        xt = [sb(cx, st, "xt%d" % i, [128, 512]) for i in range(3)]
        x2t = [sb(cx, st, "x2t%d" % i, [128, 512]) for i in range(3)]
        sqt = [sb(cx, st, "sqt%d" % i, [128, 512], BF16) for i in range(2)]
        wsrc = I["w_out"].rearrange("(k p) n -> p k n", p=128)
        it = 0
        for m in range(16):
            w = wo[m % 2]
            for kh in range(4):
                P.dma("pool", w[:, kh * 4:(kh + 1) * 4, :], wsrc[:, kh * 4:(kh + 1) * 4, m * 128:(m + 1) * 128], writes=[w.r])
            for g in range(4):
                t0 = g * 512
                pc = pp[it % 2]
                for k in range(16):
                    mm(cx, pc[:, :], w[:, k, :], cx.catT[:, k, t0:t0 + 512], k == 0, k == 15, [w.r, cx.catT.r], [pc.r])
                x_ = xt[it % 3]
                P.dma("sp", x_[:], xov[:, m, t0:t0 + 512], writes=[x_.r])
                o_ = x2t[it % 3]
                stt(cx, o_[:], pc[:, :], dv[:, 4, m:m + 1], x_[:], ALU.mult, ALU.add, [pc.r, dv.r, x_.r], [o_.r])
                P.dma("sp", x2v[:, m, t0:t0 + 512], o_[:], reads=[o_.r], writes=[x2T.r])
                q_ = sqt[it % 2]
                act(cx, q_[:], o_[:], AF.Square, [o_.r], [q_.r])
                mm(cx, pp[4 + g][:, :], cx.ones_bf[:], q_[:], m == 0, m == 15, [cx.ones_bf.r, q_.r], [pp[4 + g].r])
                it += 1
        for g in range(4):
            act(cx, rstd2[:, g * 512:(g + 1) * 512], pp[4 + g][:, :], AF.Ln, [pp[4 + g].r, cx.epsb.r], [rstd2.r], scale=1.0 / D, bias=cx.epsb[:, 0:1])
        act(cx, rstd2[:], rstd2[:], AF.Exp, [rstd2.r], [rstd2.r], scale=-0.5)
        wr = sb(cx, st, "wr", [128, 16, 36])
        P.dma("sp", wr[:], I["wr"].rearrange("(k p) n -> p k n", p=128), writes=[wr.r])
        brt = sb(cx, st, "brt", [36, 1])
        P.dma("sp", brt[:], I["br"][:, :], writes=[brt.r])
        xnf = [sb(cx, st, "xnf%d" % i, [128, 512]) for i in range(2)]
        it = 0
        for m in range(16):
            for g in range(4):
                t0 = g * 512
                x_ = xt[it % 3]
                P.dma("sp", x_[:], x2v[:, m, t0:t0 + 512], reads=[x2T.r], writes=[x_.r])
                f_ = xnf[it % 2]
                stt(cx, f_[:], x_[:], dv[:, 5, m:m + 1], rstd2[:, t0:t0 + 512], ALU.mult, ALU.mult, [x_.r, dv.r, rstd2.r], [f_.r])
                ts(cx, f_[:], f_[:], dv[:, 6, m:m + 1], None, ALU.add, None, [f_.r, dv.r], [f_.r])
                act(cx, x2n[:, m, t0:t0 + 512], f_[:], AF.Copy, [f_.r], [x2n.r])
                mm(cx, pp[4 + g][0:36, :], wr[:, m, :], f_[:], m == 0, m == 15, [wr.r, f_.r], [pp[4 + g].r])
                it += 1
        lgT = sb(cx, st, "lgT", [36, TQ])
        for g in range(4):
            ts(cx, lgT[:, g * 512:(g + 1) * 512], pp[4 + g][0:36, :], brt[:, 0:1], None, ALU.add, None, [pp[4 + g].r, brt.r], [lgT.r])
        L = sb(cx, st, "L", [128, 36])
        sc = sb(cx, st, "rsc", [128, 16])
        ohg = sb(cx, st, "ohg", [128, 4])
        eg = sb(cx, st, "eg", [128, 4])
        wi = sb(cx, st, "wi", [128, 8])
        oh1 = sb(cx, st, "oh1", [128, 8])
        oh2 = sb(cx, st, "oh2", [128, 8])
        msk = sb(cx, st, "msk", [128, 8])
        gw = sb(cx, st, "gw", [128, 8])
        g32 = sb(cx, st, "g32", [128, 32])
        for tt_ in range(16):
            pl = pp[tt_ % 2]
            P.op("pe", lambda e: e.transpose(pl[:, 0:36], lgT[:, tt_ * 128:(tt_ + 1) * 128], cx.ident[0:36, 0:36]), reads=[lgT.r, cx.ident.r], writes=[pl.r])
            cp(cx, L[:], pl[:, 0:36], [pl.r], [L.r])
            R_, W_ = [L.r, sc.r, ohg.r, eg.r, wi.r, oh1.r, oh2.r, msk.r, gw.r], None
            P.op("dve", lambda e: e.tensor_reduce(out=sc[:, 0:1], in_=L[:, 0:4], axis=mybir.AxisListType.X, op=ALU.max), reads=[L.r], writes=[sc.r])
            ts(cx, ohg[:], L[:, 0:4], sc[:, 0:1], None, ALU.is_ge, None, [L.r, sc.r], [ohg.r])
            ts(cx, sc[:, 1:2], sc[:, 0:1], -1.0, None, ALU.mult, None, [sc.r], [sc.r])
            act(cx, eg[:], L[:, 0:4], AF.Exp, [L.r, sc.r], [eg.r], bias=sc[:, 1:2], scale=1.0)
            P.op("dve", lambda e: e.tensor_reduce(out=sc[:, 2:3], in_=eg[:], axis=mybir.AxisListType.X, op=ALU.add), reads=[eg.r], writes=[sc.r])
            recip(cx, sc[:, 3:4], sc[:, 2:3], [sc.r], [sc.r])
            ts(cx, wi[:], L[:, 4:12], ohg[:, 0:1], None, ALU.mult, None, [L.r, ohg.r], [wi.r])
            for gi in range(1, 4):
                stt(cx, wi[:], L[:, 4 + gi * 8:12 + gi * 8], ohg[:, gi:gi + 1], wi[:], ALU.mult, ALU.add, [L.r, ohg.r, wi.r], [wi.r])
            P.op("dve", lambda e: e.tensor_reduce(out=sc[:, 4:5], in_=wi[:], axis=mybir.AxisListType.X, op=ALU.max), reads=[wi.r], writes=[sc.r])
            ts(cx, oh1[:], wi[:], sc[:, 4:5], None, ALU.is_ge, None, [wi.r, sc.r], [oh1.r])
            stt(cx, msk[:], oh1[:], -1e30, wi[:], ALU.mult, ALU.add, [oh1.r, wi.r], [msk.r])
            P.op("dve", lambda e: e.tensor_reduce(out=sc[:, 5:6], in_=msk[:], axis=mybir.AxisListType.X, op=ALU.max), reads=[msk.r], writes=[sc.r])
            ts(cx, oh2[:], msk[:], sc[:, 5:6], None, ALU.is_ge, None, [msk.r, sc.r], [oh2.r])
            tt(cx, sc[:, 6:7], sc[:, 5:6], sc[:, 4:5], ALU.subtract, [sc.r], [sc.r])
            act(cx, sc[:, 7:8], sc[:, 6:7], AF.Exp, [sc.r], [sc.r])
            ts(cx, sc[:, 8:9], sc[:, 7:8], 1.0, None, ALU.add, None, [sc.r], [sc.r])
            recip(cx, sc[:, 9:10], sc[:, 8:9], [sc.r], [sc.r])
            tt(cx, sc[:, 10:11], sc[:, 9:10], sc[:, 3:4], ALU.mult, [sc.r], [sc.r])
            tt(cx, sc[:, 11:12], sc[:, 10:11], sc[:, 7:8], ALU.mult, [sc.r], [sc.r])
            ts(cx, gw[:], oh1[:], sc[:, 10:11], None, ALU.mult, None, [oh1.r, sc.r], [gw.r])
            stt(cx, gw[:], oh2[:], sc[:, 11:12], gw[:], ALU.mult, ALU.add, [oh2.r, sc.r, gw.r], [gw.r])
            for gi in range(4):
                ts(cx, g32[:, gi * 8:(gi + 1) * 8], gw[:], ohg[:, gi:gi + 1], None, ALU.mult, None, [gw.r, ohg.r], [g32.r])
            pg = pp[2 + tt_ % 2]
            P.op("pe", lambda e: e.transpose(pg[0:32, 0:128], g32[:], cx.ident[:]), reads=[g32.r, cx.ident.r], writes=[pg.r])
            cp(cx, gateT[:, tt_ * 128:(tt_ + 1) * 128], pg[0:32, 0:128], [pg.r], [gateT.r])
    if "gateT" in cx.debug:
        P.dma("sp", S["gateT"].t[:, :], gateT[:], reads=[gateT.r], writes=[S["gateT"].r])
    P.barrier()
    with ExitStack() as st:
        HT = TQ // 2
        yacc = sb(cx, st, "yacc", [128, 16, HT])
        w1e = sb(cx, st, "w1e", [128, 16, 512], BF16)
        w3e = sb(cx, st, "w3e", [128, 16, 512], BF16)
        w2e = sb(cx, st, "w2e", [128, 4, D // 2], BF16)
        selr = [sb(cx, st, "selr%d" % i, [32, 128]) for i in range(2)]
        wstg = [sb(cx, st, "wstg%d" % i, [128, 512]) for i in range(3)]
        wsi = 0
        actg = sb(cx, st, "actg", [128, 4, HT], BF16)
        e1 = [sb(cx, st, "e1_%d" % i, [128, 512]) for i in range(2)]
        xt = [sb(cx, st, "mxt%d" % i, [128, 512]) for i in range(2)]
        sqt = [sb(cx, st, "msq%d" % i, [128, 512], BF16) for i in range(1)] * 2
        rstdf = sb(cx, st, "rstdf", [128, HT])
        for hf in range(2):
            h0 = hf * HT
            P.op("pool", lambda e: e.memset(yacc[:], 0.0), writes=[yacc.r])
            for ex in range(NEXP):
                w1s = I["w1"][ex].rearrange("(k p) n -> p k n", p=128)
                w3s = I["w3"][ex].rearrange("(k p) n -> p k n", p=128)
                w2s = I["w2"][ex].rearrange("(j p) n -> p j n", p=128)
                for kh in range(4):
                    P.dma("pool", w1e[:, kh * 4:(kh + 1) * 4, :], w1s[:, kh * 4:(kh + 1) * 4, :], writes=[w1e.r])
                for kh in range(16):
                    sg_ = wstg[wsi % len(wstg)]
                    P.dma("sp", sg_[:], w3s[:, kh, :], writes=[sg_.r])
                    act(cx, w3e[:, kh, :], sg_[:], AF.Copy, [sg_.r], [w3e.r])
                    wsi += 1
                it = 0
                sel = selr[ex % 2]
                P.op("pool", lambda e: e.memset(sel[:], 1.0), writes=[sel.r])
                P.op("pool", lambda e: e.affine_select(out=sel[:], in_=sel[:], pattern=[[0, 128]], compare_op=ALU.is_equal, fill=0.0,
                                                       base=-ex, channel_multiplier=1), reads=[sel.r], writes=[sel.r])
                for tg in range(HT // 512):
                    t0 = h0 + tg * 512
                    pgb = pp[6 + tg % 2]
                    mm(cx, pgb[:, :], sel[:], gateT[:, t0:t0 + 512], True, True, [sel.r, gateT.r], [pgb.r])
                    for j in range(4):
                        p1, p3 = pp[(it % 2) * 2], pp[(it % 2) * 2 + 1]
                        for k in range(16):
                            mm(cx, p1[:, :], w1e[:, k, j * 128:(j + 1) * 128], x2n[:, k, t0:t0 + 512], k == 0, k == 15, [w1e.r, x2n.r], [p1.r])
                        for k in range(16):
                            mm(cx, p3[:, :], w3e[:, k, j * 128:(j + 1) * 128], x2n[:, k, t0:t0 + 512], k == 0, k == 15, [w3e.r, x2n.r], [p3.r])
                        e_ = e1[it % 2]
                        act(cx, e_[:], p1[:, :], AF.Silu, [p1.r], [e_.r])
                        tt(cx, e_[:], e_[:], p3[:, :], ALU.mult, [e_.r, p3.r], [e_.r])
                        tt(cx, actg[:, j, tg * 512:(tg + 1) * 512], e_[:], pgb[:, :], ALU.mult, [e_.r, pgb.r], [actg.r])
                        it += 1
                it = 0
                for m in range(16):
                    if m % 8 == 0:
                        for j in range(4):
                            P.dma("pool", w2e[:, j, :], w2s[:, j, (m // 8) * 1024:(m // 8 + 1) * 1024], writes=[w2e.r])
                    for tg in range(HT // 512):
                        py = pp[4 + it % 2]
                        for j in range(4):
                            mm(cx, py[:, :], w2e[:, j, (m % 8) * 128:(m % 8 + 1) * 128], actg[:, j, tg * 512:(tg + 1) * 512], j == 0, j == 3, [w2e.r, actg.r], [py.r])
                        tt(cx, yacc[:, m, tg * 512:(tg + 1) * 512], yacc[:, m, tg * 512:(tg + 1) * 512], py[:, :], ALU.add, [yacc.r, py.r], [yacc.r])
                        it += 1
            it = 0
            for m in range(16):
                for tg in range(HT // 512):
                    t0 = h0 + tg * 512
                    x_ = xt[it % 2]
                    P.dma("sp", x_[:], x2v[:, m, t0:t0 + 512], reads=[x2T.r], writes=[x_.r])
                    o_ = e1[it % 2]
                    stt(cx, o_[:], yacc[:, m, tg * 512:(tg + 1) * 512], dv[:, 7, m:m + 1], x_[:], ALU.mult, ALU.add, [yacc.r, dv.r, x_.r], [o_.r])
                    P.dma("sp", x2v[:, m, t0:t0 + 512], o_[:], reads=[o_.r], writes=[x2T.r])
                    q_ = sqt[it % 2]
                    act(cx, q_[:], o_[:], AF.Square, [o_.r], [q_.r])
                    mm(cx, pp[6 + tg][:, :], cx.ones_bf[:], q_[:], m == 0, m == 15, [cx.ones_bf.r, q_.r], [pp[6 + tg].r])
                    it += 1
            for tg in range(HT // 512):
                act(cx, rstdf[:, tg * 512:(tg + 1) * 512], pp[6 + tg][:, :], AF.Ln, [pp[6 + tg].r, cx.epsb.r], [rstdf.r], scale=1.0 / D, bias=cx.epsb[:, 0:1])
            act(cx, rstdf[:], rstdf[:], AF.Exp, [rstdf.r], [rstdf.r], scale=-0.5)
            ov = cx.outT.t.rearrange("(m p) t -> p m t", p=128)
            it = 0
            for m in range(16):
                for tg in range(HT // 512):
                    t0 = h0 + tg * 512
                    x_ = xt[it % 2]
                    P.dma("sp", x_[:], x2v[:, m, t0:t0 + 512], reads=[x2T.r], writes=[x_.r])
                    o_ = e1[it % 2]
                    stt(cx, o_[:], x_[:], pv[:, PV_GFIN + m:PV_GFIN + m + 1], rstdf[:, tg * 512:(tg + 1) * 512], ALU.mult, ALU.mult,
                        [x_.r, pv.r, rstdf.r], [o_.r])
                    P.dma("sp", ov[:, m, t0:t0 + 512], o_[:], reads=[o_.r], writes=[cx.outT.r])
                    it += 1


def build_nc(debug=(), upto=99, rwkv=True, flags=None):
    nc = bass.Bass("TRN2", target_bir_lowering=False)
    cx = Ctx()
    cx.nc = nc
    cx.P = Prog(nc)
    cx.I = {}
    cx.S = {}
    cx.debug = debug
    cx.flags = flags or {}

    def inp(name, shape, dt=F32):
        cx.I[name] = nc.dram_tensor(name, list(shape), dt, kind="ExternalInput").ap()

    inp("xT", [D, T]); inp("xoT", [D, TQ]); inp("ctxT", [D, TC]); inp("cT", [128, 16, 2]); inp("w_mod", [D, 6 * D])
    inp("pvec", [128, PV_N]); inp("w1p", [D, NP1]); inp("rope", [64, 2, T]); inp("ropeq", [64, 2, TQ])
    inp("ident", [128, 128])
    if upto >= 3:
        inp("wq", [512, 8 * 256]); inp("wk", [256, 8 * 128]); inp("wv", [256, 8 * 128])
    if upto >= 4:
        inp("w_out", [D, D]); inp("wr", [D, 36]); inp("br", [36, 1])
    inp("rvec", [64, RV_N]); inp("mux", [64, 2, NRW, 64]); inp("upw", [64, 2, 2, 256]); inp("gup", [64, 3, 256])
    inp("m1mask", [128, 2, 128]); inp("p0mask", [64, 2, 64]); inp("selq", [128, 4])
    if upto >= 4:
        inp("w1", [NEXP, D, DEXP]); inp("w3", [NEXP, D, DEXP]); inp("w2", [NEXP, DEXP, D])

    def scr(name, shape, dt):
        kind = "ExternalOutput" if name in debug else "Internal"
        cx.S[name] = dram(cx, name, shape, dt, kind=kind)

    scr("rawp", [NRW * 64, T + 2], F32)
    scr("rawc", [NRW * 64, TC + 2], F32)
    scr("ckvnT", [256, NKV], BF16)
    scr("krotT", [64, NKV], BF16)
    scr("cqnT", [512, TQ], BF16)
    scr("x2T", [D, TQ], F32)
    scr("catA", [1024, TQ], BF16)
    scr("unit", [NCK * 64, 8 * 4 * 64], F32)
    scr("states", [NCK * 64, 8 * 64], F32)
    scr("rest", [(T // CH) * 64, 2 * 4 * 64], F32)
    for j in range(4):
        cx.S["rwkv_in%d" % j] = dram(cx, "rwkv_in%d" % j, [256, TQ], BF16, kind="Internal")
        cx.S["rwkv_all%d" % j] = dram(cx, "rwkv_all%d" % j, [1024, TQ], BF16, kind="Internal", addr_space="Local")
    if "rwkv_dbg" in debug:
        scr("rwkv_dbg", [256, T], BF16)
    scr("gateT", [32, TQ], F32)
    cx.outT = dram(cx, "outT", [D, TQ], F32, kind="ExternalOutput")

    with ExitStack() as gstack:
        cx.gstack = gstack
        cx.psum = []
        for i in range(8):
            t = gstack.enter_context(nc.psum_tensor("ps%d" % i, [128, 512], F32))
            cx.psum.append(Tl(cx.P, t, "ps%d" % i))
        cx.q0 = None
        phase0(cx)
        if "modv" in debug:
            cx.S["modv"] = dram(cx, "modv", [128, 192], F32, kind="ExternalOutput")
            cx.P.dma("sp", cx.S["modv"].t[:, :], cx.modv[:].rearrange("p j c -> p (j c)"), reads=[cx.modv.r], writes=[cx.S["modv"].r])
        cx.P.barrier()
        if upto >= 1 and not cx.flags.get("skip1"):
            phase1(cx)
            cx.P.barrier()
        cx.have_rwkv = False
        if upto >= 2 and rwkv:
            with ExitStack() as st2:
                phase2(cx, st2)
            cx.P.barrier()
            cx.have_rwkv = True
        if upto >= 3:
            with ExitStack() as st3:
                phase3(cx, st3)
            cx.P.barrier()
            if upto >= 4:
                with ExitStack() as st4:
                    phase4(cx, st4)
                cx.P.barrier()
        finals = [cx.outT] + [cx.S[n] for n in debug if n in cx.S]
        cx.P.wait_all("sp", [f.r for f in finals])
    cx.nc_ninst = cx.P.ninst
    return nc, cx


def rope_tables():
    rows = T // GRID_W
    row, col = np.meshgrid(np.arange(rows), np.arange(GRID_W), indexing="ij")
    inv_freq = (10000.0 ** (-np.arange(0, 32, 2, dtype=np.float32) / 32)).astype(np.float32)
    ang_r = row.reshape(-1)[:, None].astype(np.float32) * inv_freq
    ang_c = col.reshape(-1)[:, None].astype(np.float32) * inv_freq
    Ct = np.concatenate([np.cos(ang_r), np.cos(ang_r), np.cos(ang_c), np.cos(ang_c)], axis=1).T
    St = np.concatenate([-np.sin(ang_r), np.sin(ang_r), -np.sin(ang_c), np.sin(ang_c)], axis=1).T
    return np.ascontiguousarray(np.stack([Ct, St], axis=1).astype(np.float32))


ROPE_PERM = np.concatenate([np.arange(16, 32), np.arange(0, 16), np.arange(48, 64), np.arange(32, 48)])


def fm(v, p=128):
    return np.ascontiguousarray(v.reshape(-1, p).T)


def prep_core(inp, c, shared):
    b, q = c // 4, c % 4
    m = {}
    m["xT"] = shared["xT"][b]
    m["xoT"] = np.ascontiguousarray(shared["xT"][b][:, q * TQ:(q + 1) * TQ])
    m["ctxT"] = shared["ctxT"][b]
    m["cT"] = np.ascontiguousarray(np.stack([fm(inp["c"][b]), fm(inp["c_ctx"])], axis=-1))
    m["w_mod"] = shared["w_mod"]
    m["pvec"] = shared["pvec"]
    w_in = inp["w_in"][0]
    cols = list(range(0, 832)) + list(768 + ROPE_PERM)
    for base in (832, 1856, 2880):
        for i in range(4):
            h = 4 * q + i
            cols += list(range(base + h * 64, base + (h + 1) * 64))
    cols += list(range(3904, 4192))
    m["w1p"] = np.ascontiguousarray(w_in[:, cols])
    assert m["w1p"].shape[1] == NP1
    m["rope"] = shared["rope"]
    m["ropeq"] = np.ascontiguousarray(shared["rope"][:, :, q * TQ:(q + 1) * TQ])
    m["ident"] = shared["ident"]
    for k in ("wq", "wk", "wv", "w_out", "wr", "br", "w1", "w2", "w3", "m1mask", "p0mask"):
        m[k] = shared[k]
    hs = [4 * q + i for i in range(4)]
    rvv = np.zeros((64, RV_N), np.float32)
    for i, h in enumerate(hs):
        cs = slice(h * 64, (h + 1) * 64)
        for d in range(2):
            rvv[:, RV_W0 + d * 4 + i] = inp["decay_w0"][0][d][cs]
            rvv[:, RV_A0 + d * 4 + i] = inp["iclr_a0"][0][d][cs]
        rvv[:, RV_KK + i] = inp["key_k"][0][cs]
        rvv[:, RV_KA + i] = inp["key_a"][0][cs]
        rvv[:, RV_RK + i] = inp["bonus_r_k"][0][h]
        rvv[:, RV_LG + i] = inp["lnx_g"][0][cs]
        rvv[:, RV_LB + i] = inp["lnx_b"][0][cs]
    m["rvec"] = rvv
    smu = inp["shift_mu"][0]
    mux = np.zeros((64, 2, NRW, 64), np.float32)
    mi = 0
    for base in (0, 1024, 2048):
        for h in hs:
            for a in range(2):
                mux[:, a, mi, :] = smu[a][base + h * 64: base + (h + 1) * 64][:, None]
            mi += 1
    for c0, n in ((3072, 64), (3136, 64), (3200, 64), (3264, 64), (3328, 32)):
        for a in range(2):
            mux[:n, a, mi, :] = smu[a][c0:c0 + n][:, None]
        mi += 1
    m["mux"] = mux
    cols = slice(4 * q * 64, 4 * q * 64 + 256)
    upw = np.zeros((64, 2, 2, 256), np.float32)
    for d in range(2):
        upw[:, 0, d, :] = inp["decay_up"][0][d][:, cols]
        upw[:, 1, d, :] = inp["iclr_up"][0][d][:, cols]
    m["upw"] = upw
    gup = np.zeros((64, 3, 256), np.float32)
    gu = inp["gate_up"][0]
    gup[:, 0, :] = gu[0:64, cols]; gup[:, 1, :] = gu[64:128, cols]; gup[:32, 2, :] = gu[128:160, cols]
    m["gup"] = gup
    sq_ = np.zeros((128, 4), np.float32); sq_[:, q] = 1.0
    m["selq"] = sq_
    return m


def prep_shared(inp):
    sh = {}
    sh["xT"] = [np.ascontiguousarray(inp["x"][b].T) for b in range(2)]
    sh["ctxT"] = [np.ascontiguousarray(inp["ctx"][b].T) for b in range(2)]
    sh["w_mod"] = np.ascontiguousarray(inp["w_mod"][0])
    pv = np.zeros((128, PV_N), np.float32)
    pv[:, PV_BMOD:PV_BMOD + 96] = fm(inp["b_mod"][0])
    pv[:, PV_GATTN:PV_GATTN + 16] = fm(inp["norm_attn_g"][0])
    pv[:, PV_GFFN:PV_GFFN + 16] = fm(inp["norm_ffn_g"][0])
    pv[:, PV_GFIN:PV_GFIN + 16] = fm(inp["final_norm_g"])
    pv[:, PV_QNG:PV_QNG + 4] = fm(inp["q_norm_g"][0])
    pv[:, PV_KVNG:PV_KVNG + 2] = fm(inp["kv_norm_g"][0])
    sh["pvec"] = pv
    sh["rope"] = rope_tables()
    sh["ident"] = np.eye(128, dtype=np.float32)
    wuq = inp["w_uq"][0].reshape(512, 8, 192)
    sh["wq"] = np.ascontiguousarray(np.concatenate([wuq, wuq[:, :, 128 + ROPE_PERM]], axis=2).reshape(512, 8 * 256))
    wukv = inp["w_ukv"][0].reshape(256, 8, 256)
    sh["wk"] = np.ascontiguousarray(wukv[:, :, :128].reshape(256, 1024))
    sh["wv"] = np.ascontiguousarray(wukv[:, :, 128:].reshape(256, 1024))
    sh["w_out"] = np.ascontiguousarray(inp["w_out"][0])
    sh["wr"] = np.ascontiguousarray(np.concatenate([inp["w_grp"][0], inp["w_exp"][0]], axis=1))
    sh["br"] = np.ascontiguousarray(np.concatenate([inp["b_grp"][0], inp["b_exp"][0]])[:, None])
    lo_s = np.tril(np.ones((64, 64), np.float32), -1)
    lo_i = np.tril(np.ones((64, 64), np.float32), 0)
    m1 = np.zeros((128, 2, 128), np.float32)
    p0 = np.zeros((64, 2, 64), np.float32)
    for d, (ts_, ti_) in enumerate(((lo_s, lo_i), (lo_s.T, lo_i.T))):
        blk = np.block([[ts_.T, ti_.T], [ts_.T, ti_.T]])
        m1[:, d, :] = blk
        p0[:, d, :] = ts_
    sh["m1mask"] = m1; sh["p0mask"] = p0
    sh["w1"] = np.ascontiguousarray(inp["w1"][0]); sh["w3"] = np.ascontiguousarray(inp["w3"][0]); sh["w2"] = np.ascontiguousarray(inp["w2"][0])
    return sh


def kernel(**inputs):
    inp = {k: np.asarray(v) for k, v in inputs.items()}
    shared = prep_shared(inp)
    nc, cx = build_nc()
    in_maps = [prep_core(inp, c, shared) for c in range(8)]
    res = run_bass_kernel_spmd(nc, in_maps, core_ids=list(range(8)))
    out = np.zeros((2, T, D), np.float32)
    for c in range(8):
        b, q = c // 4, c % 4
        out[b, q * TQ:(q + 1) * TQ, :] = res.results[c]["outT"].T
    return out
```
